# Optimizing a Trainium2 kernel written in Bass

```python
import jax, jax.numpy as jnp
from jax import lax
import numpy as np

D_MODEL = 1024
BATCH = 2
SEQ = 8192
DEPTH = 1

EPS = 1e-6
NEG_INF = -1e30
FORCE_SCORE = 1e9
ROPE_THETA = 500000.0
Q_BLOCK = 128

A_HEADS = 8
A_KV_HEADS = 2
A_HEAD_DIM = 64
A_ROPE_DIM = A_HEAD_DIM // 4
CMP_LEN = 32
CMP_STRIDE = 16
CMP_HIDDEN = 256
SLC_LEN = 64
SLC_TOPK = 16
WINDOW = 512
A_WIDTH = A_HEADS * A_HEAD_DIM
A_KV_WIDTH = A_KV_HEADS * A_HEAD_DIM

B_HEADS = 8
Q_LORA = 256
KV_LORA = 128
B_NOPE = 64
B_ROPE = 32
B_V = 64
B_QK = B_NOPE + B_ROPE
B_WIDTH = B_HEADS * B_V

MIX_WIDTH = A_WIDTH + B_WIDTH
IN_SIZES = (A_WIDTH, A_KV_WIDTH, A_KV_WIDTH, A_KV_WIDTH, A_KV_WIDTH, A_KV_WIDTH, A_KV_WIDTH,
            3 * A_HEADS, Q_LORA, KV_LORA, B_ROPE)
IN_WIDTH = sum(IN_SIZES)

P_HEADS = 8
N_KEYS = 128
N_EXPERTS = N_KEYS * N_KEYS
P_KEY_DIM = 256
P_TOPK = 16
P_CHUNK = 128

kernel_name = 'hybrid_nsa_mla_peer_layer'


def _rms(x):
    xf = x.astype(jnp.float32)
    return xf * lax.rsqrt(jnp.mean(xf * xf, axis=-1, keepdims=True) + EPS)


def rmsnorm(x, gain):
    return (_rms(x) * gain.astype(jnp.float32)).astype(x.dtype)


def rope_tables(pos, rot_dim):
    inv_freq = ROPE_THETA ** (-jnp.arange(0, rot_dim, 2, dtype=jnp.float32) / rot_dim)
    ang = pos.astype(jnp.float32)[..., None] * inv_freq
    return jnp.cos(ang), jnp.sin(ang)


def apply_rope(x, cos, sin):
    half = cos.shape[-1]
    xf = x.astype(jnp.float32)
    x1, x2, rest = xf[..., :half], xf[..., half:2 * half], xf[..., 2 * half:]
    out = jnp.concatenate([x1 * cos - x2 * sin, x2 * cos + x1 * sin, rest], axis=-1)
    return out.astype(x.dtype)


def masked_softmax(s, mask):
    s = jnp.where(mask, s.astype(jnp.float32), NEG_INF)
    p = jax.nn.softmax(s, axis=-1)
    return jnp.where(mask, p, 0.0)


def nsa_mixer(q, k_cmp, v_cmp, k_slc, v_slc, k_win, v_win, gates, positions,
              q_gain, kc_gain, ks_gain, kw_gain, cmp_pos, cmp_k_w1, cmp_k_w2, cmp_v_w1, cmp_v_w2):
    B, S = q.shape[:2]
    G, R, dk = A_KV_HEADS, A_HEADS // A_KV_HEADS, A_HEAD_DIM
    scale = dk ** -0.5
    cos, sin = rope_tables(positions, A_ROPE_DIM)
    cos_h, sin_h = cos[:, :, None, :], sin[:, :, None, :]
    q = apply_rope(rmsnorm(q, q_gain), cos_h, sin_h)
    k_slc = apply_rope(rmsnorm(k_slc, ks_gain), cos_h, sin_h)
    k_win = apply_rope(rmsnorm(k_win, kw_gain), cos_h, sin_h)

    n_cmp = (S - CMP_LEN) // CMP_STRIDE + 1
    cmp_idx = jnp.arange(n_cmp)[:, None] * CMP_STRIDE + jnp.arange(CMP_LEN)[None, :]
    cmp_start, cmp_end = cmp_idx[:, 0], cmp_idx[:, -1]

    def compress(t, w1, w2):
        blk = t[:, cmp_idx] + cmp_pos[:, None, :]
        blk = blk.transpose(0, 1, 3, 2, 4).reshape(B, n_cmp, G, CMP_LEN * dk)
        return jax.nn.gelu(blk @ w1) @ w2

    kc = compress(k_cmp, cmp_k_w1, cmp_k_w2)
    vc = compress(v_cmp, cmp_v_w1, cmp_v_w2)
    cos_c, sin_c = rope_tables(positions[:, cmp_end], A_ROPE_DIM)
    kc = apply_rope(rmsnorm(kc, kc_gain), cos_c[:, :, None, :], sin_c[:, :, None, :])

    n_slc = S // SLC_LEN
    topk = min(SLC_TOPK, n_slc)
    slc_start = jnp.arange(n_slc) * SLC_LEN
    overlap = ((cmp_start[:, None] < slc_start[None, :] + SLC_LEN)
               & (cmp_end[:, None] >= slc_start[None, :])).astype(jnp.float32)
    ks_blocks = k_slc.reshape(B, n_slc, SLC_LEN, G, dk).transpose(0, 3, 1, 2, 4)
    vs_blocks = v_slc.reshape(B, n_slc, SLC_LEN, G, dk).transpose(0, 3, 1, 2, 4)

    pad = ((0, 0), (WINDOW, 0), (0, 0), (0, 0))
    kw_pad, vw_pad = jnp.pad(k_win, pad), jnp.pad(v_win, pad)

    def block(qb):
        q0 = qb * Q_BLOCK
        t = q0 + jnp.arange(Q_BLOCK)
        qblk = lax.dynamic_slice_in_dim(q, q0, Q_BLOCK, axis=1).reshape(B, Q_BLOCK, G, R, dk)
        gblk = lax.dynamic_slice_in_dim(gates, q0, Q_BLOCK, axis=1).reshape(B, Q_BLOCK, G, R, 3)

        s_c = jnp.einsum('bqgrd,bngd->bgrqn', qblk, kc) * scale
        p_c = masked_softmax(s_c, cmp_end[None, :] <= t[:, None])
        o_c = jnp.einsum('bgrqn,bngd->bqgrd', p_c.astype(vc.dtype), vc)

        imp = jnp.einsum('bgrqn,nj->bgqj', p_c, overlap)
        j = jnp.arange(n_slc)
        forced = (j[None, :] == (t // SLC_LEN)[:, None]) | (j[None, :] == 0)
        causal_blk = slc_start[None, :] <= t[:, None]
        imp = jnp.where(forced, FORCE_SCORE, jnp.where(causal_blk, imp, NEG_INF))
        _, sel = lax.top_k(imp, topk)
        flat = sel.reshape(B, G, Q_BLOCK * topk)[..., None, None]
        ks = jnp.take_along_axis(ks_blocks, flat, axis=2).reshape(B, G, Q_BLOCK, topk * SLC_LEN, dk)
        vs = jnp.take_along_axis(vs_blocks, flat, axis=2).reshape(B, G, Q_BLOCK, topk * SLC_LEN, dk)
        tok = (sel[..., None] * SLC_LEN + jnp.arange(SLC_LEN)).reshape(B, G, Q_BLOCK, topk * SLC_LEN)
        mask_s = (tok <= t[:, None])[:, :, None]
        s_s = jnp.einsum('bqgrd,bgqsd->bgrqs', qblk, ks) * scale
        p_s = masked_softmax(s_s, mask_s)
        o_s = jnp.einsum('bgrqs,bgqsd->bqgrd', p_s.astype(vs.dtype), vs)

        kw = lax.dynamic_slice_in_dim(kw_pad, q0, Q_BLOCK + WINDOW, axis=1)
        vw = lax.dynamic_slice_in_dim(vw_pad, q0, Q_BLOCK + WINDOW, axis=1)
        s_pos = q0 - WINDOW + jnp.arange(Q_BLOCK + WINDOW)
        mask_w = ((s_pos[None, :] <= t[:, None]) & (s_pos[None, :] > t[:, None] - WINDOW)
                  & (s_pos[None, :] >= 0))
        s_w = jnp.einsum('bqgrd,bsgd->bgrqs', qblk, kw) * scale
        p_w = masked_softmax(s_w, mask_w)
        o_w = jnp.einsum('bgrqs,bsgd->bqgrd', p_w.astype(vw.dtype), vw)

        return gblk[..., 0:1] * o_c + gblk[..., 1:2] * o_s + gblk[..., 2:3] * o_w

    out = lax.map(block, jnp.arange(S // Q_BLOCK))
    return out.transpose(1, 0, 2, 3, 4, 5).reshape(B, S, A_WIDTH)


def mla_mixer(c_q, c_kv, k_rope, positions, q_lora_gain, w_uq, kv_lora_gain, w_ukv, q_gain, k_gain):
    B, S = c_q.shape[:2]
    H = B_HEADS
    cos, sin = rope_tables(positions, B_ROPE)
    cos_h, sin_h = cos[:, :, None, :], sin[:, :, None, :]
    q = (rmsnorm(c_q, q_lora_gain) @ w_uq).reshape(B, S, H, B_QK)
    kv = (rmsnorm(c_kv, kv_lora_gain) @ w_ukv).reshape(B, S, H, B_NOPE + B_V)
    k_nope, v = kv[..., :B_NOPE], kv[..., B_NOPE:]
    k = jnp.concatenate([k_nope, jnp.broadcast_to(k_rope[:, :, None, :], (B, S, H, B_ROPE))], axis=-1)
    q, k = rmsnorm(q, q_gain), rmsnorm(k, k_gain)
    q = jnp.concatenate([q[..., :B_NOPE], apply_rope(q[..., B_NOPE:], cos_h, sin_h)], axis=-1)
    k = jnp.concatenate([k[..., :B_NOPE], apply_rope(k[..., B_NOPE:], cos_h, sin_h)], axis=-1)
    scale = B_QK ** -0.5
    key_idx = jnp.arange(S)

    def block(qb):
        q0 = qb * Q_BLOCK
        t = q0 + jnp.arange(Q_BLOCK)
        qblk = lax.dynamic_slice_in_dim(q, q0, Q_BLOCK, axis=1)
        s = jnp.einsum('bqhd,bshd->bhqs', qblk, k) * scale
        p = masked_softmax(s, key_idx[None, :] <= t[:, None])
        return jnp.einsum('bhqs,bshd->bqhd', p.astype(v.dtype), v)

    out = lax.map(block, jnp.arange(S // Q_BLOCK))
    return out.transpose(1, 0, 2, 3, 4).reshape(B, S, B_WIDTH)


def peer_ffn(x, w_q, sub_keys, expert_u, expert_v):
    B, S, D = x.shape
    T = B * S
    xt = x.reshape(T, D)
    q = _rms((xt @ w_q).reshape(T, P_HEADS, P_KEY_DIM)).astype(x.dtype)
    half = P_KEY_DIM // 2
    s1 = jnp.einsum('thd,hnd->thn', q[..., :half], sub_keys[:, 0]).astype(jnp.float32)
    s2 = jnp.einsum('thd,hnd->thn', q[..., half:], sub_keys[:, 1]).astype(jnp.float32)
    v1, i1 = lax.top_k(s1, P_TOPK)
    v2, i2 = lax.top_k(s2, P_TOPK)
    cand = (v1[..., :, None] + v2[..., None, :]).reshape(T, P_HEADS, P_TOPK * P_TOPK)
    vals, ci = lax.top_k(cand, P_TOPK)
    e = (jnp.take_along_axis(i1, ci // P_TOPK, axis=-1) * N_KEYS
         + jnp.take_along_axis(i2, ci % P_TOPK, axis=-1))
    g = jax.nn.softmax(vals, axis=-1)
    n_chunks = T // P_CHUNK

    def chunk(args):
        xc, ec, gc = args
        a = jnp.einsum('cd,ckd->ck', xc, expert_u[ec])
        w = (gc * jax.nn.gelu(a.astype(jnp.float32))).astype(x.dtype)
        return jnp.einsum('ck,ckd->cd', w, expert_v[ec])

    y = lax.map(chunk, (xt.reshape(n_chunks, P_CHUNK, D),
                        e.reshape(n_chunks, P_CHUNK, P_HEADS * P_TOPK),
                        g.reshape(n_chunks, P_CHUNK, P_HEADS * P_TOPK)))
    return y.reshape(B, S, D)


def hybrid_layer(x, positions, norm1_gain, w_in,
                 nsa_q_gain, nsa_kc_gain, nsa_ks_gain, nsa_kw_gain,
                 cmp_pos, cmp_k_w1, cmp_k_w2, cmp_v_w1, cmp_v_w2,
                 mla_q_lora_gain, mla_w_uq, mla_kv_lora_gain, mla_w_ukv, mla_q_gain, mla_k_gain,
                 out_gain_a, out_gain_b, w_out,
                 norm2_gain, peer_w_q, peer_sub_keys, peer_u, peer_v):
    B, S, _ = x.shape
    proj = rmsnorm(x, norm1_gain) @ w_in
    pts = [int(p) for p in np.cumsum(IN_SIZES)[:-1]]
    (a_q, a_kc, a_vc, a_ks, a_vs, a_kw, a_vw, a_gate, b_cq, b_ckv, b_kr) = jnp.split(proj, pts, axis=-1)
    kvs = lambda t: t.reshape(B, S, A_KV_HEADS, A_HEAD_DIM)
    gates = jax.nn.sigmoid(a_gate).reshape(B, S, A_HEADS, 3)
    o_a = nsa_mixer(a_q.reshape(B, S, A_HEADS, A_HEAD_DIM), kvs(a_kc), kvs(a_vc), kvs(a_ks), kvs(a_vs),
                    kvs(a_kw), kvs(a_vw), gates, positions,
                    nsa_q_gain, nsa_kc_gain, nsa_ks_gain, nsa_kw_gain,
                    cmp_pos, cmp_k_w1, cmp_k_w2, cmp_v_w1, cmp_v_w2)
    o_b = mla_mixer(b_cq, b_ckv, b_kr, positions, mla_q_lora_gain, mla_w_uq,
                    mla_kv_lora_gain, mla_w_ukv, mla_q_gain, mla_k_gain)
    mixed = jnp.concatenate([rmsnorm(o_a, out_gain_a), rmsnorm(o_b, out_gain_b)], axis=-1) @ w_out
    h = x + mixed
    return h + peer_ffn(rmsnorm(h, norm2_gain), peer_w_q, peer_sub_keys, peer_u, peer_v)


def setup_inputs(seed: int = 0) -> dict:
    key = jax.random.key(seed)
    ks = jax.random.split(key, 32)
    L = DEPTH
    nrm = lambda k, shape, s: jax.random.normal(k, shape, jnp.float32) * s
    gain = lambda k, n: 1.0 + 0.02 * jax.random.normal(k, (L, n), jnp.float32)
    start = jax.random.randint(ks[1], (BATCH, 1), 0, 4096, dtype=jnp.int32)
    return {
        'x': nrm(ks[0], (BATCH, SEQ, D_MODEL), 1.0),
        'positions': (start + jnp.arange(SEQ, dtype=jnp.int32)[None, :]).astype(jnp.int32),
        'norm1_gain': gain(ks[2], D_MODEL),
        'w_in': nrm(ks[3], (L, D_MODEL, IN_WIDTH), D_MODEL ** -0.5),
        'nsa_q_gain': gain(ks[4], A_HEAD_DIM),
        'nsa_kc_gain': gain(ks[5], A_HEAD_DIM),
        'nsa_ks_gain': gain(ks[6], A_HEAD_DIM),
        'nsa_kw_gain': gain(ks[7], A_HEAD_DIM),
        'cmp_pos': nrm(ks[8], (L, CMP_LEN, A_HEAD_DIM), 0.1),
        'cmp_k_w1': nrm(ks[9], (L, CMP_LEN * A_HEAD_DIM, CMP_HIDDEN), (CMP_LEN * A_HEAD_DIM) ** -0.5),
        'cmp_k_w2': nrm(ks[10], (L, CMP_HIDDEN, A_HEAD_DIM), CMP_HIDDEN ** -0.5),
        'cmp_v_w1': nrm(ks[11], (L, CMP_LEN * A_HEAD_DIM, CMP_HIDDEN), (CMP_LEN * A_HEAD_DIM) ** -0.5),
        'cmp_v_w2': nrm(ks[12], (L, CMP_HIDDEN, A_HEAD_DIM), CMP_HIDDEN ** -0.5),
        'mla_q_lora_gain': gain(ks[13], Q_LORA),
        'mla_w_uq': nrm(ks[14], (L, Q_LORA, B_HEADS * B_QK), Q_LORA ** -0.5),
        'mla_kv_lora_gain': gain(ks[15], KV_LORA),
        'mla_w_ukv': nrm(ks[16], (L, KV_LORA, B_HEADS * (B_NOPE + B_V)), KV_LORA ** -0.5),
        'mla_q_gain': gain(ks[17], B_QK),
        'mla_k_gain': gain(ks[18], B_QK),
        'out_gain_a': gain(ks[19], A_WIDTH),
        'out_gain_b': gain(ks[20], B_WIDTH),
        'w_out': nrm(ks[21], (L, MIX_WIDTH, D_MODEL), MIX_WIDTH ** -0.5),
        'norm2_gain': gain(ks[22], D_MODEL),
        'peer_w_q': nrm(ks[23], (L, D_MODEL, P_HEADS * P_KEY_DIM), D_MODEL ** -0.5),
        'peer_sub_keys': nrm(ks[24], (L, P_HEADS, 2, N_KEYS, P_KEY_DIM // 2), (P_KEY_DIM // 2) ** -0.5),
        'peer_u': nrm(ks[25], (L, N_EXPERTS, D_MODEL), D_MODEL ** -0.5),
        'peer_v': nrm(ks[26], (L, N_EXPERTS, D_MODEL), D_MODEL ** -0.5),
    }


def reference(x, positions, norm1_gain, w_in,
              nsa_q_gain, nsa_kc_gain, nsa_ks_gain, nsa_kw_gain,
              cmp_pos, cmp_k_w1, cmp_k_w2, cmp_v_w1, cmp_v_w2,
              mla_q_lora_gain, mla_w_uq, mla_kv_lora_gain, mla_w_ukv, mla_q_gain, mla_k_gain,
              out_gain_a, out_gain_b, w_out,
              norm2_gain, peer_w_q, peer_sub_keys, peer_u, peer_v):
    h = x
    for l in range(DEPTH):
        h = hybrid_layer(h, positions, norm1_gain[l], w_in[l],
                         nsa_q_gain[l], nsa_kc_gain[l], nsa_ks_gain[l], nsa_kw_gain[l],
                         cmp_pos[l], cmp_k_w1[l], cmp_k_w2[l], cmp_v_w1[l], cmp_v_w2[l],
                         mla_q_lora_gain[l], mla_w_uq[l], mla_kv_lora_gain[l], mla_w_ukv[l],
                         mla_q_gain[l], mla_k_gain[l],
                         out_gain_a[l], out_gain_b[l], w_out[l],
                         norm2_gain[l], peer_w_q[l], peer_sub_keys[l], peer_u[l], peer_v[l])
    return h
```

```python
import math
import types
from contextlib import ExitStack
import numpy as np
import ml_dtypes
import concourse.bass as bass
import concourse.mybir as mybir
from concourse.bass_utils import run_bass_kernel_spmd

F32 = mybir.dt.float32
BF16 = mybir.dt.bfloat16
I32 = mybir.dt.int32
U32 = mybir.dt.uint32
AF = mybir.ActivationFunctionType
ALU = mybir.AluOpType
AX = mybir.AxisListType

EPS = 1e-6
NEGM = -30000.0
TWO_PI = 2.0 * math.pi
CW1 = 6.28125
CW2 = TWO_PI - CW1


class _Op:
    __slots__ = ("eng", "fn", "deps", "signal", "signo", "is_dma", "semkey", "dmaval")

    def __init__(self, eng, fn, is_dma=False, semkey=None):
        self.eng = eng
        self.fn = fn
        self.deps = []
        self.signal = False
        self.signo = None
        self.is_dma = is_dma
        self.semkey = semkey
        self.dmaval = None


def _freeze(fn):
    if fn.__closure__ is None:
        return fn
    cells = []
    for c in fn.__closure__:
        try:
            cells.append(types.CellType(c.cell_contents))
        except ValueError:
            cells.append(c)
    return types.FunctionType(fn.__code__, fn.__globals__, fn.__name__, fn.__defaults__, tuple(cells))


class Sched:
    ENGS = ("pe", "act", "dve", "pool", "sp")

    def __init__(self, nc):
        self.nc = nc
        self.streams = {e: [] for e in self.ENGS}
        self.last_w = {}
        self.readers = {}
        self.dma_cnt = {}
        self.last_dma = {}
        self.bar_deps = []
        self.need_bar = {e: False for e in self.ENGS}

    def barrier(self):
        lasts = []
        for e in self.ENGS:
            for o in reversed(self.streams[e]):
                if not o.is_dma:
                    lasts.append(o)
                    break
        lasts += list(self.last_dma.values())
        self.bar_deps = lasts
        self.need_bar = {e: True for e in self.ENGS}

    def op(self, eng, fn, reads=(), writes=(), dma=False, semkey=None):
        o = _Op(eng, _freeze(fn), is_dma=dma, semkey=semkey)
        if dma:
            c = self.dma_cnt.get(semkey, 0) + 1
            self.dma_cnt[semkey] = c
            o.dmaval = 16 * c
            self.last_dma[semkey] = o
        deps = o.deps
        if self.need_bar[eng]:
            self.need_bar[eng] = False
            for d in self.bar_deps:
                if d.is_dma or d.eng != eng:
                    deps.append(d)
        for r in reads:
            w = self.last_w.get(r)
            if w is not None and w not in deps:
                if not (w.eng == eng and eng == "pe" and not w.is_dma and not dma):
                    deps.append(w)
        for r in writes:
            w = self.last_w.get(r)
            if w is not None and w not in deps:
                if w.is_dma or dma or w.eng != eng or eng != "pe":
                    deps.append(w)
            for rd in self.readers.get(r, ()):
                if (rd.is_dma or dma or rd.eng != eng or eng != "pe") and rd not in deps and rd is not o:
                    deps.append(rd)
        for r in reads:
            self.readers.setdefault(r, []).append(o)
        for r in writes:
            self.last_w[r] = o
            self.readers[r] = []
        self.streams[eng].append(o)
        return o

    def dma(self, eng, fn, reads=(), writes=(), semkey=None):
        if semkey is None:
            semkey = writes[0]
        return self.op(eng, fn, reads, writes, dma=True, semkey=semkey)

    def emit(self, final_wait_keys=()):
        nc = self.nc
        for e in self.ENGS:
            for o in self.streams[e]:
                for d in o.deps:
                    if not d.is_dma:
                        d.signal = True
        for e in self.ENGS:
            n = 0
            for o in self.streams[e]:
                if o.signal and not o.is_dma:
                    n += 1
                    o.signo = n
        sems = {}
        stack = []

        def getsem(key):
            if key not in sems:
                cm = nc.semaphore("s%d" % len(sems))
                sems[key] = cm.__enter__()
                stack.append(cm)
            return sems[key]

        for e in self.ENGS:
            getsem(("eng", e))
        for k in self.dma_cnt:
            getsem(("dma", k))
        streams = self.streams
        final_keys = list(final_wait_keys)
        dma_cnt = self.dma_cnt

        with nc.Block() as block:
            def make(ename):
                def body(eng):
                    waited = {}
                    for o in streams[ename]:
                        for d in o.deps:
                            if d.is_dma:
                                key = ("dma", d.semkey)
                                val = d.dmaval
                            else:
                                key = ("eng", d.eng)
                                val = d.signo
                            if waited.get(key, 0) >= val:
                                continue
                            waited[key] = val
                            eng.wait_ge(sems[key], val)
                        ins = o.fn(eng)
                        if o.is_dma:
                            ins.then_inc(sems[("dma", o.semkey)], 16)
                        elif o.signal:
                            ins.then_inc(sems[("eng", ename)], 1)
                    if ename == "sp":
                        for k in final_keys:
                            if k not in dma_cnt:
                                continue
                            eng.wait_ge(sems[("dma", k)], 16 * dma_cnt[k])
                return body

            block.sync(make("sp"))
            block.tensor(make("pe"))
            block.scalar(make("act"))
            block.vector(make("dve"))
            block.gpsimd(make("pool"))
        for cm in reversed(stack):
            cm.__exit__(None, None, None)
        print("sems used:", len(sems), "ops:", {e: len(streams[e]) for e in self.ENGS})


KV_COLS = 928
Q_COLS = 792


def build(NT, L, stage=3, dbg=False, cut=99, peer_rows=16384):
    SEQ = NT * 128
    NCMP = (SEQ - 32) // 16 + 1
    NCK = (NCMP + 127) // 128
    NCP = NCK * 128
    NSLC = SEQ // 64
    assert NSLC <= 128 and NSLC >= 16
    nc = bass.Bass("TRN2", target_bir_lowering=False)
    D = {}

    def din(name, shape, dt=F32):
        D[name] = nc.dram_tensor(name, list(shape), dt, kind="ExternalInput").ap()
        return D[name]

    x_d = din("x", [NT, 128, 1024])
    xq_d = din("xq", [L, 128, 1024])
    pos_d = din("pos", [128, NT], I32)
    posq_d = din("posq", [128, L], I32)
    posc_d = din("posc", [128, NCK], I32)
    wkv_d = din("wkv", [128, 8, KV_COLS])
    wq_d = din("wq", [128, 8, Q_COLS])
    g1_d = din("g1", [128, 8])
    vec_d = din("vecs", [128, 576])
    invf_d = din("invf", [128, 24])
    cposT_d = din("cposT", [128, 32])
    w1k_d = din("w1k", [128, 32, 256])
    w1v_d = din("w1v", [128, 32, 256])
    w2k_d = din("w2k", [128, 2, 64])
    w2v_d = din("w2v", [128, 2, 64])
    wuq_d = din("wuq", [128, 2, 768])
    gql_d = din("gql", [128, 2])
    wukall_d = din("wukall", [128, 512])
    wukT_d = din("wukT", [128, 8, 128])
    wuv_d = din("wuv", [128, 512])
    wout_d = din("wout", [128, 8, 1024])
    gout_d = din("gout", [128, 8])
    ident_d = din("ident", [128, 128])
    e64_d = din("e64", [128, 2, 2048])
    ovl_d = din("ovl", [128, NCK, 128])
    cmask_d = din("cmask", [L, 128, NCP])
    pbias_d = din("pbias", [L, 128, 128])
    dmask_d = din("dmask", [128, 4, 128])
    wmask_d = din("wmask", [128, 8, 128])
    wpq_d = din("wpq", [128, 8, 2048])
    skT_d = din("skT", [128, 16, 128])
    iota_d = din("iota16", [128, 16])
    pu_d = din("peer_u", [peer_rows, 1024])
    pv_d = din("peer_v", [peer_rows, 1024])
    out_d = nc.dram_tensor("out", [L, 128, 1024], F32, kind="ExternalOutput").ap()
    hs_d = nc.dram_tensor("hs", [L, 128, 1024], F32, kind="Internal").ap()
    dbg_outs = {}

    def dout(name, shape, dt=F32):
        dbg_outs[name] = nc.dram_tensor(name, list(shape), dt, kind="ExternalOutput").ap()
        return dbg_outs[name]

    VOFF = {}
    off = 0
    for nm, n in (("qa", 64), ("kc", 64), ("ks", 64), ("kw", 64), ("kvl", 128), ("mq", 96), ("mk", 96)):
        VOFF[nm] = (off, n)
        off += n
    n2_d = din("n2g", [128, 1024])

    S = Sched(nc)
    SCALE_A = 64 ** -0.5
    SCALE_B = 96 ** -0.5

    with ExitStack() as top:
        uniq = [0]

        def sb(name, shape, dt=F32, st=top):
            uniq[0] += 1
            return st.enter_context(nc.sbuf_tensor("sb%d_%s" % (uniq[0], name), list(shape), dt))

        def ps(name, shape, dt=F32, st=top):
            uniq[0] += 1
            return st.enter_context(nc.psum_tensor("ps%d_%s" % (uniq[0], name), list(shape), dt))

        ident = sb("ident", [128, 128])
        identb = sb("identb", [128, 128], BF16)
        vecs = sb("vecs", [128, 576])
        invf = sb("invf", [128, 24])
        S.dma("sp", lambda e: e.dma_start(out=ident[:], in_=ident_d), writes=["ident"])
        S.dma("sp", lambda e: e.dma_start(out=vecs[:], in_=vec_d), writes=["vecs"])
        S.dma("sp", lambda e: e.dma_start(out=invf[:], in_=invf_d), writes=["invf"])
        S.op("dve", lambda e: e.tensor_copy(out=identb[:], in_=ident[:]), reads=["ident"], writes=["identb"])

        def gv(nm):
            o, n = VOFF[nm]
            return vecs[:, o:o + n]

        def sincos(st, pos_dram, ncol, F, foff, tag, res_pair):
            pi_ = sb("pi_" + tag, [128, ncol], I32, st)
            pf = sb("pf_" + tag, [128, ncol], F32, st)
            ang = sb("ang_" + tag, [128, ncol, F], F32, st)
            ki = sb("ki_" + tag, [128, ncol, F], I32, st)
            kf = sb("kf_" + tag, [128, ncol, F], F32, st)
            r = sb("r_" + tag, [128, ncol, F], F32, st)
            S.dma("sp", lambda e: e.dma_start(out=pi_[:], in_=pos_dram), writes=["pi_" + tag])
            S.op("dve", lambda e: e.tensor_copy(out=pf[:], in_=pi_[:]), reads=["pi_" + tag], writes=["pf_" + tag])
            S.op("dve", lambda e: e.tensor_tensor(out=ang[:], in0=pf[:].unsqueeze(2).to_broadcast([128, ncol, F]),
                                                  in1=invf[:, foff:foff + F].unsqueeze(1).to_broadcast([128, ncol, F]), op=ALU.mult),
                 reads=["pf_" + tag, "invf"], writes=["ang_" + tag])
            outs = []
            for which, shift in (("c", math.pi / 2), ("s", 0.0)):
                res = res_pair[0 if which == "c" else 1]
                rn = "r_" + tag
                if shift != 0.0:
                    S.op("dve", lambda e: e.tensor_scalar(out=r[:], in0=ang[:], scalar1=shift, scalar2=None, op0=ALU.add),
                         reads=["ang_" + tag], writes=[rn])
                    src, srcn = r, rn
                else:
                    src, srcn = ang, "ang_" + tag
                S.op("dve", lambda e, src=src: e.tensor_scalar(out=ki[:], in0=src[:], scalar1=1.0 / TWO_PI, scalar2=None, op0=ALU.mult),
                     reads=[srcn], writes=["ki_" + tag])
                S.op("dve", lambda e: e.tensor_copy(out=kf[:], in_=ki[:]), reads=["ki_" + tag], writes=["kf_" + tag])
                S.op("dve", lambda e, src=src: e.scalar_tensor_tensor(out=r[:], in0=kf[:], scalar=-CW1, in1=src[:], op0=ALU.mult, op1=ALU.add),
                     reads=["kf_" + tag, srcn], writes=[rn])
                S.op("dve", lambda e: e.scalar_tensor_tensor(out=r[:], in0=kf[:], scalar=-CW2, in1=r[:], op0=ALU.mult, op1=ALU.add),
                     reads=["kf_" + tag, rn], writes=[rn])
                S.op("dve", lambda e: e.tensor_scalar(out=r[:], in0=r[:], scalar1=math.pi, scalar2=-math.pi, op0=ALU.min, op1=ALU.max),
                     reads=[rn], writes=[rn])
                S.op("act", lambda e, res=res: e.activation(out=res[:], in_=r[:], func=AF.Sin), reads=[rn], writes=["%s_%s" % (which, tag)])
                outs.append((res, "%s_%s" % (which, tag)))
            return outs

        rp = {}
        for tag, ncol, F in (("QA", L, 8), ("QB", L, 16)):
            rp[tag] = (sb("c_" + tag, [128, ncol, F]), sb("s_" + tag, [128, ncol, F]))
        kvst = ExitStack()

        def rope(eng_name, xv, cosv, sinv, outv, tmpv, rd, wr, half):
            G = xv.shape[1]
            tn = wr[0] + "_ropetmp"
            if G == 1:
                x1 = xv[:, 0, 0:half]
                x2 = xv[:, 0, half:2 * half]
                t0, t1, t2, t3 = (tmpv[:, i, 0, :] for i in range(4))
                S.op("dve", lambda e: e.tensor_tensor(out=t0, in0=x1, in1=cosv, op=ALU.mult), reads=rd, writes=[tn + "0"])
                S.op("dve", lambda e: e.tensor_tensor(out=t1, in0=x2, in1=sinv, op=ALU.mult), reads=rd, writes=[tn + "1"])
                S.op("dve", lambda e: e.tensor_tensor(out=t2, in0=x2, in1=cosv, op=ALU.mult), reads=rd, writes=[tn + "2"])
                S.op("dve", lambda e: e.tensor_tensor(out=t3, in0=x1, in1=sinv, op=ALU.mult), reads=rd, writes=[tn + "3"])
                S.op("dve", lambda e: e.tensor_tensor(out=outv[:, 0, 0:half], in0=t0, in1=t1, op=ALU.subtract), reads=[tn + "0", tn + "1"], writes=wr)
                S.op("dve", lambda e: e.tensor_tensor(out=outv[:, 0, half:2 * half], in0=t2, in1=t3, op=ALU.add), reads=[tn + "2", tn + "3"], writes=wr)
                return
            cb = cosv.unsqueeze(1).to_broadcast([128, G, half])
            sbb = sinv.unsqueeze(1).to_broadcast([128, G, half])
            x1 = xv[:, :, 0:half]
            x2 = xv[:, :, half:2 * half]
            S.op("dve", lambda e: e.tensor_tensor(out=tmpv[:, 0], in0=x1, in1=cb, op=ALU.mult), reads=rd, writes=[tn + "0"])
            S.op("dve", lambda e: e.tensor_tensor(out=tmpv[:, 1], in0=x2, in1=sbb, op=ALU.mult), reads=rd, writes=[tn + "1"])
            S.op("dve", lambda e: e.tensor_tensor(out=tmpv[:, 2], in0=x2, in1=cb, op=ALU.mult), reads=rd, writes=[tn + "2"])
            S.op("dve", lambda e: e.tensor_tensor(out=tmpv[:, 3], in0=x1, in1=sbb, op=ALU.mult), reads=rd, writes=[tn + "3"])
            S.op("dve", lambda e: e.tensor_tensor(out=outv[:, :, 0:half], in0=tmpv[:, 0], in1=tmpv[:, 1], op=ALU.subtract),
                 reads=[tn + "0", tn + "1"], writes=wr)
            S.op("dve", lambda e: e.tensor_tensor(out=outv[:, :, half:2 * half], in0=tmpv[:, 2], in1=tmpv[:, 3], op=ALU.add),
                 reads=[tn + "2", tn + "3"], writes=wr)

        def load_w(st, dst, dstn, src_d, K, F, gain=None, gainn=None, part=None):
            stg = [sb("stg%s%d" % (dstn, i), [128, F], F32, st) for i in range(2)]
            for k in range(K):
                sg = stg[k % 2]
                sgn = "stg%s%d" % (dstn, k % 2)
                if part is None:
                    S.dma("sp", lambda e, sg=sg, k=k: e.dma_start(out=sg[:], in_=src_d[:, k, :]), writes=[sgn])
                else:
                    S.dma("sp", lambda e, sg=sg, k=k: e.dma_start(out=sg[0:part], in_=src_d[0:part, k, :]), writes=[sgn])
                if gain is not None:
                    S.op("dve", lambda e, sg=sg, k=k: e.tensor_scalar(out=dst[:, k, :], in0=sg[:], scalar1=gain[:, k:k + 1], scalar2=None, op0=ALU.mult),
                         reads=[sgn, gainn], writes=[dstn])
                else:
                    pp = 128 if part is None else part
                    S.op("dve", lambda e, sg=sg, k=k, pp=pp: e.tensor_copy(out=dst[0:pp, k, :], in_=sg[0:pp]), reads=[sgn], writes=[dstn])

        KsT = sb("KsT", [128, SEQ], BF16, kvst)
        Vs = sb("Vs", [128, NT, 2, 65], BF16, kvst)
        ckvT = sb("ckvT", [128, SEQ], BF16, kvst)
        krT = sb("krT", [128, SEQ], BF16, kvst)
        ckva = sb("ckva", [128, NT, 129], BF16, kvst)
        srh = sb("srh", [128, NT, 8], F32, kvst)
        KcT = sb("KcT", [128, NCP], BF16, kvst)
        Vc = sb("Vc", [128, NCK, 2, 64], BF16, kvst)
        kw_d = nc.dram_tensor("kw_scr", [128, SEQ], BF16, kind="Internal").ap()
        vw_d = nc.dram_tensor("vw_scr", [128, NT, 130], BF16, kind="Internal").ap()
        S.op("pool", lambda e: e.memset(Vs[:], 1.0), writes=["Vs"])
        S.op("pool", lambda e: e.memset(ckva[:], 1.0), writes=["ckva"])
        S.op("pool", lambda e: e.memset(KcT[:], 0.0), writes=["KcT"])
        S.op("pool", lambda e: e.memset(krT[:], 0.0), writes=["krT"])
        S.op("pool", lambda e: e.memset(Vc[:], 0.0), writes=["Vc"])

        def rstd_from_ss(ss_ap, ssn, out_ap, outn, n_elems, tmp_ap, tmpn):
            S.op("dve", lambda e: e.tensor_scalar(out=tmp_ap, in0=ss_ap, scalar1=1.0 / n_elems, scalar2=EPS, op0=ALU.mult, op1=ALU.add),
                 reads=[ssn], writes=[tmpn])
            S.op("act", lambda e: e.activation(out=tmp_ap, in_=tmp_ap, func=AF.Sqrt), reads=[tmpn], writes=[tmpn])
            S.op("dve", lambda e: e.reciprocal(out=out_ap, in_=tmp_ap), reads=[tmpn], writes=[outn])

        def xnorm_T(xt, xtn, junk, junkn, st_small, sfx, xnb, xnbn, pT, pTn, xnT, xnTn):
            ss, rs, tm = st_small
            S.op("act", lambda e: e.activation(out=junk[:], in_=xt[:], func=AF.Square, accum_out=ss[:, 0:1]), reads=[xtn], writes=[junkn, "ss" + sfx])
            rstd_from_ss(ss[:, 0:1], "ss" + sfx, rs[:, 0:1], "rs" + sfx, 1024.0, tm[:, 0:1], "tm" + sfx)
            S.op("dve", lambda e: e.tensor_scalar(out=xnb[:], in0=xt[:], scalar1=rs[:, 0:1], scalar2=None, op0=ALU.mult),
                 reads=[xtn, "rs" + sfx], writes=[xnbn])
            for k in range(8):
                S.op("pe", lambda e, k=k: e.transpose(out=pT[:, k * 128:(k + 1) * 128], in_=xnb[:, k * 128:(k + 1) * 128], identity=identb[:]),
                     reads=[xnbn, "identb"], writes=[pTn])
            S.op("act", lambda e: e.copy(out=xnT[:].rearrange("p k t -> p (k t)"), in_=pT[:, 0:1024]), reads=[pTn], writes=[xnTn])

        with ExitStack() as p1:
            for tag, ncol, F in (("A", NT, 8), ("B", NT, 16), ("C", NCK, 8)):
                rp[tag] = (sb("c_" + tag, [128, ncol, F], F32, p1), sb("s_" + tag, [128, ncol, F], F32, p1))
            with ExitStack() as tmp0:
                (cosA, cosA_n), (sinA, sinA_n) = sincos(tmp0, pos_d, NT, 8, 0, "A", rp["A"])
                (cosB, cosB_n), (sinB, sinB_n) = sincos(tmp0, pos_d, NT, 16, 8, "B", rp["B"])
                (cosC, cosC_n), (sinC, sinC_n) = sincos(tmp0, posc_d, NCK, 8, 0, "C", rp["C"])
                (cosQA, cosQA_n), (sinQA, sinQA_n) = sincos(tmp0, posq_d, L, 8, 0, "QA", rp["QA"])
                (cosQB, cosQB_n), (sinQB, sinQB_n) = sincos(tmp0, posq_d, L, 16, 8, "QB", rp["QB"])
                S.barrier()
            KcrT = sb("KcrT", [128, SEQ], BF16, p1)
            VcrT = sb("VcrT", [128, SEQ], BF16, p1)
            with ExitStack() as p1a:
                wkv = sb("wkv", [128, 8, KV_COLS], BF16, p1a)
                g1 = sb("g1", [128, 8], F32, p1a)
                wukall = sb("wukall", [128, 512], BF16, p1a)
                S.dma("sp", lambda e: e.dma_start(out=g1[:], in_=g1_d), writes=["g1"])
                with ExitStack() as ld:
                    load_w(ld, wkv, "wkv", wkv_d, 8, KV_COLS, gain=g1, gainn="g1")
                    stg = sb("stg_wuk", [128, 512], F32, ld)
                    S.dma("sp", lambda e: e.dma_start(out=stg[:], in_=wukall_d), writes=["stg_wuk"])
                    S.op("dve", lambda e: e.tensor_copy(out=wukall[:], in_=stg[:]), reads=["stg_wuk"], writes=["wukall"])
                    S.barrier()
                xts = [sb("xt%d" % i, [128, 1024], F32, p1a) for i in range(2)]
                junk = sb("junk", [128, 1024], BF16, p1a)
                xnb = sb("xnb", [128, 1024], BF16, p1a)
                xnTs = [sb("xnT%d" % i, [128, 8, 128], BF16, p1a) for i in range(2)]
                small = [(sb("ss%d" % i, [128, 8], F32, p1a), sb("rs%d" % i, [128, 8], F32, p1a), sb("tm%d" % i, [128, 8], F32, p1a)) for i in range(2)]
                kn4 = sb("kn4", [128, 4, 64], F32, p1a)
                rtmp = sb("rtmp", [128, 4, 4, 16], F32, p1a)
                kwst = [sb("kwst%d" % i, [128, 128], BF16, p1a) for i in range(2)]
                vwst = [sb("vwst%d" % i, [128, 2, 65], BF16, p1a) for i in range(2)]
                for i_ in range(2):
                    S.op("pool", lambda e, i_=i_: e.memset(vwst[i_][:], 1.0), writes=["vwst%d" % i_])
                kb = sb("kb", [128, 7, 128], BF16, p1a)
                ckvn = sb("ckvn", [128, 128], F32, p1a)
                krg = sb("krg", [128, 1, 32], F32, p1a)
                sqj = sb("sqj", [128, 512], F32, p1a)
                ssn8 = sb("ssn8", [128, 8], F32, p1a)
                pTs = [ps("pT%d" % i, [128, 1024], BF16, p1a) for i in range(2)]
                pPs = [ps("pP%d" % i, [128, 1024], F32, p1a) for i in range(2)]
                pK = ps("pK", [128, 512], F32, p1a)
                pT2 = ps("pT2", [128, 1024], BF16, p1a)

                for tt in range(NT if cut > 1 else 0):
                    b = tt % 2
                    xt, xtn = xts[b], "xt%d" % b
                    ss, rs, tm = small[b]
                    sfx = str(b)
                    pP, pPn = pPs[b], "pP%d" % b
                    xnT, xnTn = xnTs[b], "xnT%d" % b
                    S.dma("sp", lambda e, xt=xt, tt=tt: e.dma_start(out=xt[:], in_=x_d[tt]), writes=[xtn])
                    xnorm_T(xt, xtn, junk, "junk", small[b], sfx, xnb, "xnb", pTs[b], "pT%d" % b, xnT, xnTn)
                    for half, (c0, c1) in enumerate(((0, 512), (512, KV_COLS))):
                        for k in range(8):
                            S.op("pe", lambda e, k=k, c0=c0, c1=c1, half=half, pP=pP, xnT=xnT: e.matmul(
                                pP[:, half * 512: half * 512 + (c1 - c0)], lhsT=xnT[:, k, :], rhs=wkv[:, k, c0:c1], start=(k == 0), stop=(k == 7)),
                                reads=[xnTn, "wkv"], writes=[pPn])
                    if cut < 3:
                        continue
                    S.op("act", lambda e, pP=pP: e.copy(out=kb[:, 2:4, :].rearrange("p a b -> p (a b)"), in_=pP[:, 0:256]), reads=[pPn], writes=["kb23"])
                    S.op("act", lambda e, pP=pP, tt=tt: e.copy(out=Vs[:, tt, :, 0:64], in_=pP[:, 384:512].rearrange("p (g d) -> p g d", g=2)),
                         reads=[pPn], writes=["Vs"])
                    S.op("act", lambda e, pP=pP, b=b: e.copy(out=vwst[b][:, :, 0:64], in_=pP[:, 640:768].rearrange("p (g d) -> p g d", g=2)),
                         reads=[pPn], writes=["vwst%d" % b])
                    S.dma("sp", lambda e, tt=tt, b=b: e.dma_start(out=vw_d[:, tt, :], in_=vwst[b][:].rearrange("p g d -> p (g d)")), reads=["vwst%d" % b], writes=["vw_scr"], semkey="vwd%d" % b)
                    if cut < 3.2:
                        continue
                    for i, c0 in enumerate((256, 320, 512, 576)):
                        S.op("act", lambda e, pP=pP, c0=c0, i=i, ss=ss: e.activation(out=sqj[:, 0:64], in_=pP[:, c0:c0 + 64], func=AF.Square, accum_out=ss[:, 1 + i:2 + i]),
                             reads=[pPn], writes=["sqj", "ss4" + sfx])
                    rstd_from_ss(ss[:, 1:5], "ss4" + sfx, rs[:, 1:5], "rs4" + sfx, 64.0, tm[:, 1:5], "tm4" + sfx)
                    for i, c0 in enumerate((256, 320, 512, 576)):
                        gn = gv("ks") if i < 2 else gv("kw")
                        S.op("dve", lambda e, pP=pP, c0=c0, i=i, rs=rs, gn=gn: e.scalar_tensor_tensor(
                            out=kn4[:, i, :], in0=pP[:, c0:c0 + 64], scalar=rs[:, 1 + i:2 + i], in1=gn, op0=ALU.mult, op1=ALU.mult),
                            reads=[pPn, "rs4" + sfx, "vecs"], writes=["kn4"])
                    if cut < 3.4:
                        continue
                    kb01 = kb[:, 0:2, :].rearrange("p a (g d) -> p (a g) d", g=2)
                    S.op("dve", lambda e: e.tensor_copy(out=kb01[:, :, 16:64], in_=kn4[:, :, 16:64]), reads=["kn4"], writes=["kb01"])
                    rope("dve", kn4[:], cosA[:, tt, :], sinA[:, tt, :], kb01, rtmp[:, :, :, 0:8], ["kn4", cosA_n, sinA_n], ["kb01"], 8)
                    if cut < 3.6:
                        continue
                    S.op("act", lambda e, pP=pP, ss=ss: e.activation(out=sqj[:, 0:128], in_=pP[:, 768:896], func=AF.Square, accum_out=ss[:, 5:6]),
                         reads=[pPn], writes=["sqj", "ss5" + sfx])
                    rstd_from_ss(ss[:, 5:6], "ss5" + sfx, rs[:, 5:6], "rs5" + sfx, 128.0, tm[:, 5:6], "tm5" + sfx)
                    S.op("dve", lambda e, pP=pP, rs=rs: e.scalar_tensor_tensor(out=ckvn[:], in0=pP[:, 768:896], scalar=rs[:, 5:6], in1=gv("kvl"), op0=ALU.mult, op1=ALU.mult),
                         reads=[pPn, "rs5" + sfx, "vecs"], writes=["ckvn"])
                    S.op("dve", lambda e: e.tensor_copy(out=kb[:, 4, :], in_=ckvn[:]), reads=["ckvn"], writes=["kb4"])
                    S.op("pool", lambda e, tt=tt: e.tensor_copy(out=ckva[:, tt, 0:128], in_=ckvn[:]), reads=["ckvn"], writes=["ckva"])
                    if cut < 3.8:
                        continue
                    S.op("act", lambda e, pP=pP, ss=ss: e.activation(out=sqj[:, 0:32], in_=pP[:, 896:928], func=AF.Square, accum_out=ss[:, 6:7]),
                         reads=[pPn], writes=["sqj", "ss6" + sfx])
                    mk_o = VOFF["mk"][0]
                    S.op("dve", lambda e, pP=pP: e.tensor_tensor(out=krg[:, 0, :], in0=pP[:, 896:928], in1=vecs[:, mk_o + 64:mk_o + 96], op=ALU.mult),
                         reads=[pPn, "vecs"], writes=["krg"])
                    if cut < 3.9:
                        continue
                    kb5 = kb[:, 5, 0:32].unsqueeze(1)
                    rope("dve", krg[:], cosB[:, tt, :], sinB[:, tt, :], kb5, rtmp[:, :, 0:1, 0:16], ["krg", cosB_n, sinB_n], ["kb5"], 16)
                    if cut < 4.1:
                        continue
                    for i in range(5):
                        S.op("pe", lambda e, i=i: e.transpose(out=pT2[:, i * 128:(i + 1) * 128], in_=kb[:, i, :], identity=identb[:]),
                             reads=["kb01", "kb23", "kb4", "identb"], writes=["pT2"])
                    S.op("pe", lambda e: e.transpose(out=pT2[0:32, 640:768], in_=kb[:, 5, 0:32], identity=identb[:]),
                         reads=["kb5", "identb"], writes=["pT2"])
                    if cut < 4.2:
                        continue
                    tsl = slice(tt * 128, (tt + 1) * 128)
                    S.op("act", lambda e, tsl=tsl: e.copy(out=KsT[:, tsl], in_=pT2[:, 0:128]), reads=["pT2"], writes=["KsT"])
                    S.op("act", lambda e, b=b: e.copy(out=kwst[b][:], in_=pT2[:, 128:256]), reads=["pT2"], writes=["kwst%d" % b])
                    S.dma("sp", lambda e, tsl=tsl, b=b: e.dma_start(out=kw_d[:, tsl], in_=kwst[b][:]), reads=["kwst%d" % b], writes=["kw_scr"], semkey="kwd%d" % b)
                    S.op("act", lambda e, tsl=tsl: e.copy(out=KcrT[:, tsl], in_=pT2[:, 256:384]), reads=["pT2"], writes=["KcrT"])
                    S.op("act", lambda e, tsl=tsl: e.copy(out=VcrT[:, tsl], in_=pT2[:, 384:512]), reads=["pT2"], writes=["VcrT"])
                    S.op("act", lambda e, tsl=tsl: e.copy(out=ckvT[:, tsl], in_=pT2[:, 512:640]), reads=["pT2"], writes=["ckvT"])
                    if cut < 4.3:
                        continue
                    S.op("act", lambda e, tsl=tsl: e.copy(out=krT[0:32, tsl], in_=pT2[0:32, 640:768]), reads=["pT2"], writes=["krT"])
                    if cut < 4.4:
                        continue
                    S.op("pe", lambda e, tsl=tsl: e.matmul(pK[:], lhsT=ckvT[:, tsl], rhs=wukall[:], start=True, stop=True),
                         reads=["ckvT", "wukall"], writes=["pK"])
                    S.op("act", lambda e: e.activation(out=sqj[:], in_=pK[:], func=AF.Square), reads=["pK"], writes=["sqj"])
                    S.op("dve", lambda e: e.tensor_reduce(out=ssn8[:], in_=sqj[:].rearrange("p (h d) -> p h d", h=8), axis=AX.X, op=ALU.add),
                         reads=["sqj"], writes=["ssn8"])
                    S.op("dve", lambda e, ss=ss: e.tensor_scalar(out=ssn8[:], in0=ssn8[:], scalar1=ss[:, 6:7], scalar2=None, op0=ALU.add),
                         reads=["ssn8", "ss6" + sfx], writes=["ssn8"])
                    S.op("dve", lambda e: e.tensor_scalar(out=ssn8[:], in0=ssn8[:], scalar1=1.0 / 96.0, scalar2=EPS, op0=ALU.mult, op1=ALU.add),
                         reads=["ssn8"], writes=["ssn8"])
                    S.op("act", lambda e: e.activation(out=ssn8[:], in_=ssn8[:], func=AF.Sqrt), reads=["ssn8"], writes=["ssn8"])
                    S.op("dve", lambda e: e.reciprocal(out=ssn8[:], in_=ssn8[:]), reads=["ssn8"], writes=["ssn8"])
                    S.op("dve", lambda e, tt=tt: e.tensor_scalar(out=srh[:, tt, :], in0=ssn8[:], scalar1=SCALE_B, scalar2=None, op0=ALU.mult),
                         reads=["ssn8"], writes=["srh"])
                S.barrier()

            with ExitStack() as p1b:
                W1 = sb("W1", [128, 32, 256], BF16, p1b)
                W2 = sb("W2", [128, 2, 64], BF16, p1b)
                cposT = sb("cposT", [128, 32], F32, p1b)
                cposTb = sb("cposTb", [128, 32], BF16, p1b)
                c1 = sb("c1", [128, 2], F32, p1b)
                GH = sb("GH", [128, 2, NCP], BF16, p1b)
                kc4 = sb("kc4", [128, 2, 64], F32, p1b)
                kcb = sb("kcb", [128, 2, 64], BF16, p1b)
                rtmp2 = sb("rtmp2", [128, 4, 2, 8], F32, p1b)
                ssc = sb("ssc", [128, 2], F32, p1b)
                rsc = sb("rsc", [128, 2], F32, p1b)
                tmc = sb("tmc", [128, 2], F32, p1b)
                sqj2 = sb("sqj2", [128, 64], F32, p1b)
                pH = [ps("pH%d" % i, [128, 512], F32, p1b) for i in range(2)]
                pC = ps("pC", [128, 512], F32, p1b)
                pO = ps("pO1", [128, 512], F32, p1b)
                pTc = ps("pTc", [128, 1024], BF16, p1b)
                S.dma("sp", lambda e: e.dma_start(out=cposT[:], in_=cposT_d), writes=["cposT"])
                S.op("dve", lambda e: e.tensor_copy(out=cposTb[:], in_=cposT[:]), reads=["cposT"], writes=["cposTb"])
                S.op("pool", lambda e: e.memset(GH[:], 0.0), writes=["GH"])
                for which, (w1_d, w2_d, srcT, srcn) in enumerate(((w1k_d, w2k_d, KcrT, "KcrT"), (w1v_d, w2v_d, VcrT, "VcrT")) if cut > 5 else ()):
                    with ExitStack() as ld:
                        load_w(ld, W1, "W1", w1_d, 32, 256)
                        load_w(ld, W2, "W2", w2_d, 2, 64)
                        S.barrier()
                    for hh in range(2):
                        for l in range(32):
                            S.op("pe", lambda e, hh=hh, l=l: e.matmul(pC[:, hh:hh + 1], lhsT=W1[0:64, l, hh * 128:(hh + 1) * 128], rhs=cposTb[0:64, l:l + 1],
                                                                       start=(l == 0), stop=(l == 31)), reads=["W1", "cposTb"], writes=["pC"])
                    S.op("dve", lambda e: e.tensor_copy(out=c1[:], in_=pC[:, 0:2]), reads=["pC"], writes=["c1"])
                    for g in range(2):
                        for hh in range(2):
                            ph, phn = pH[hh], "pH%d" % hh
                            for l in range(32):
                                sv = srcT[64 * g:64 * g + 64, :].rearrange("p (n l) -> p n l", l=16)
                                rhs = sv[:, 0:NCMP, l] if l < 16 else sv[:, 1:NCMP + 1, l - 16]
                                S.op("pe", lambda e, l=l, hh=hh, g=g, rhs=rhs, ph=ph: e.matmul(ph[:, 0:NCMP], lhsT=W1[64 * g:64 * g + 64, l, hh * 128:(hh + 1) * 128], rhs=rhs,
                                                                                         start=(l == 0), stop=(l == 31)), reads=[srcn, "W1"], writes=[phn])
                            S.op("act", lambda e, hh=hh, ph=ph: e.activation(out=GH[:, hh, 0:NCMP], in_=ph[:, 0:NCMP], func=AF.Gelu_apprx_tanh, bias=c1[:, hh:hh + 1]),
                                 reads=[phn, "c1"], writes=["GH"])
                        for k in range(NCK):
                            for hh in range(2):
                                S.op("pe", lambda e, k=k, hh=hh, g=g: e.matmul(pO[:, (k * 2 + g) * 64:(k * 2 + g) * 64 + 64], lhsT=GH[:, hh, k * 128:(k + 1) * 128], rhs=W2[:, hh, :],
                                                                           start=(hh == 0), stop=(hh == 1)), reads=["GH", "W2"], writes=["pO1"])
                    for k in range(NCK):
                        pv = pO[:, k * 128:(k + 1) * 128].rearrange("p (g d) -> p g d", g=2)
                        if which == 1:
                            S.op("act", lambda e, k=k, pv=pv: e.copy(out=Vc[:, k, :, :], in_=pv), reads=["pO1"], writes=["Vc"])
                            continue
                        for g in range(2):
                            S.op("act", lambda e, g=g, pv=pv: e.activation(out=sqj2[:], in_=pv[:, g, :], func=AF.Square, accum_out=ssc[:, g:g + 1]),
                                 reads=["pO1"], writes=["sqj2", "ssc"])
                        rstd_from_ss(ssc[:], "ssc", rsc[:], "rsc", 64.0, tmc[:], "tmc")
                        for g in range(2):
                            S.op("dve", lambda e, g=g, pv=pv: e.scalar_tensor_tensor(out=kc4[:, g, :], in0=pv[:, g, :], scalar=rsc[:, g:g + 1], in1=gv("kc"), op0=ALU.mult, op1=ALU.mult),
                                 reads=["pO1", "rsc", "vecs"], writes=["kc4"])
                        S.op("dve", lambda e: e.tensor_copy(out=kcb[:, :, 16:64], in_=kc4[:, :, 16:64]), reads=["kc4"], writes=["kcb"])
                        rope("dve", kc4[:], cosC[:, k, :], sinC[:, k, :], kcb[:], rtmp2[:], ["kc4", cosC_n, sinC_n], ["kcb"], 8)
                        S.op("pe", lambda e: e.transpose(out=pTc[:, 0:128], in_=kcb[:].rearrange("p g d -> p (g d)"), identity=identb[:]), reads=["kcb", "identb"], writes=["pTc"])
                        nval = min(128, NCMP - k * 128)
                        S.op("act", lambda e, k=k, nval=nval: e.copy(out=KcT[:, k * 128:k * 128 + nval], in_=pTc[:, 0:nval]), reads=["pTc"], writes=["KcT"])
                S.barrier()

        if dbg and cut > 6:
            for nm, t, shp, dt in (("d_KsT", KsT, [128, SEQ], BF16), ("d_ckvT", ckvT, [128, SEQ], BF16),
                                   ("d_KcT", KcT, [128, NCP], BF16), ("d_srh", srh, [128, NT, 8], F32), ("d_Vc", Vc, [128, NCK, 2, 64], BF16),
                                   ("d_Vs", Vs, [128, NT, 2, 65], BF16), ("d_ckva", ckva, [128, NT, 129], BF16)):
                o = dout(nm, shp, dt)
                S.dma("sp", lambda e, o=o, t=t: e.dma_start(out=o, in_=t[:]), reads=[nm[2:]], writes=[nm])
            o = dout("d_krT", [32, SEQ], BF16)
            S.dma("sp", lambda e, o=o: e.dma_start(out=o, in_=krT[0:32, :]), reads=["krT"], writes=["d_krT"])

        PHASE2 = stage in (2, 3)
        PHASE3 = stage in (3, 4)
        if PHASE2:
            with ExitStack() as p2:
                wq = sb("wq", [128, 8, Q_COLS], BF16, p2)
                wuq = sb("wuq", [128, 2, 768], BF16, p2)
                wukT = sb("wukT", [128, 8, 128], BF16, p2)
                wuv = sb("wuv", [128, 512], BF16, p2)
                wout = sb("wout", [128, 8, 1024], BF16, p2)
                e64 = sb("e64", [128, 32 * 128], BF16, p2)
                ovl = sb("ovl", [128, NCK, 128], F32, p2)
                dmask = sb("dmask", [128, 4, 128], BF16, p2)
                wmask = sb("wmask", [128, 8, 128], BF16, p2)
                g1b = sb("g1b", [128, 8], F32, p2)
                gql = sb("gql", [128, 2], F32, p2)
                gout = sb("gout", [128, 8], F32, p2)
                for t_, d_, n_ in ((g1b, g1_d, "g1b"), (gql, gql_d, "gql"), (gout, gout_d, "gout"), (ovl, ovl_d, "ovl")):
                    S.dma("sp", lambda e, t_=t_, d_=d_: e.dma_start(out=t_[:], in_=d_), writes=[n_])
                with ExitStack() as ld:
                    load_w(ld, wq, "wq", wq_d, 8, Q_COLS, gain=g1b, gainn="g1b")
                    load_w(ld, e64[:].rearrange("p (k f) -> p k f", k=2), "e64", e64_d, 2, 2048)
                    load_w(ld, dmask, "dmask", dmask_d, 4, 128)
                    load_w(ld, wmask, "wmask", wmask_d, 8, 128)
                    load_w(ld, wuq, "wuq", wuq_d, 2, 768, gain=gql, gainn="gql")
                    load_w(ld, wukT, "wukT", wukT_d, 8, 128)
                    load_w(ld, wout, "wout", wout_d, 8, 1024, gain=gout, gainn="gout")
                    stg = sb("stg_wuv", [128, 512], F32, ld)
                    S.dma("sp", lambda e: e.dma_start(out=stg[:], in_=wuv_d), writes=["stg_wuv"])
                    S.op("dve", lambda e: e.tensor_copy(out=wuv[:], in_=stg[:]), reads=["stg_wuv"], writes=["wuv"])
                    S.barrier()
                xt = sb("xt", [128, 1024], F32, p2)
                xnb = sb("xnb", [128, 1024], BF16, p2)
                junk = xnb
                xnT = sb("xnT", [128, 8, 128], BF16, p2)
                small = (sb("ss", [128, 8], F32, p2), sb("rs", [128, 8], F32, p2), sb("tm", [128, 8], F32, p2))
                sqj = sb("sqj", [128, 768], F32, p2)
                s8 = sb("s8", [128, 8], F32, p2)
                r8 = sb("r8", [128, 8], F32, p2)
                t8 = sb("t8", [128, 8], F32, p2)
                qn = sb("qn", [128, 8, 64], F32, p2)
                qab = sb("qab", [128, 4, 2, 64], BF16, p2)
                rtmp = sb("rtmp", [128, 4, 8, 16], F32, p2)
                QaTz = [sb("QaTz%d" % i, [128, 4, 128], BF16, p2) for i in range(2)]
                gat = sb("gat", [128, 24], F32, p2)
                cqn = sb("cqn", [128, 256], BF16, p2)
                cqT = sb("cqT", [128, 2, 128], BF16, p2)
                qm = sb("qm", [128, 8, 96], F32, p2)
                qmn = sb("qmn", [128, 8, 64], BF16, p2)
                qmr = sb("qmr", [128, 8, 32], BF16, p2)
                qnT = sb("qnT", [128, 4, 128], BF16, p2)
                QlatT = sb("QlatT", [128, 8, 128], BF16, p2)
                QropeT = sb("QropeT", [128, 8, 128], BF16, p2)
                cmask = sb("cmask", [128, NCP], F32, p2)
                pbias = sb("pbias", [128, 128], F32, p2)
                Pc = sb("Pc", [128, 4, NCP], F32, p2)
                Pcb = sb("Pcb", [128, NCP], BF16, p2)
                Psm = sb("Psm", [128, NCP], F32, p2)
                PsT = sb("PsT", [128, NCK, 128], F32, p2)
                PcT = sb("PcT", [128, NCK, 128], BF16, p2)
                z4 = sb("z4", [128, 4], F32, p2)
                f4 = sb("f4", [128, 4], F32, p2)
                impb = sb("impb", [128, 128], F32, p2)
                wk = sb("wk", [128, 128], F32, p2)
                m8 = sb("m8", [128, 16], F32, p2)
                nsl = sb("nsl", [128, 128], F32, p2)
                nslb = sb("nslb", [128, 128], BF16, p2)
                nselZ = [sb("nselZ%d" % i, [128, 4, 128], BF16, p2) for i in range(2)]
                PTs = [sb("PT%d" % i, [128, 512], BF16, p2) for i in range(3)]
                KwTw = sb("KwTw", [128, 8 * 128], BF16, p2)
                Vww = sb("Vww", [128, 8, 2, 65], BF16, p2)
                oa = sb("oa", [128, 8, 64], F32, p2)
                olb = sb("olb", [128, 128], BF16, p2)
                OlatT = sb("OlatT", [128, 128], BF16, p2)
                onb = xnb
                onT = xnT
                ht = xt
                pT0 = ps("pT0", [128, 1024], BF16, p2)
                pT1 = ps("pT1", [128, 1024], BF16, p2)
                pP0 = ps("pP0", [128, 512], F32, p2)
                pP1 = ps("pP1", [128, 512], F32, p2)
                pSs = [ps("pS%d" % i, [128, 512], F32, p2) for i in range(2)]
                pO = ps("pO", [128, 512], F32, p2)
                pX = ps("pX", [128, 512], F32, p2)
                S.op("pool", lambda e: e.memset(nsl[:], NEGM), writes=["nsl"])
                S.op("pool", lambda e: e.memset(QropeT[:], 0.0), writes=["QropeT"])
                for i_ in range(2):
                    S.op("pool", lambda e, i_=i_: e.memset(QaTz[i_][:], 0.0), writes=["QaT"])
                    S.op("pool", lambda e, i_=i_: e.memset(nselZ[i_][:], 0.0), writes=["nselT4"])
                S.op("pool", lambda e: e.memset(impb[:], 0.0), writes=["impb"])
                mq_o = VOFF["mq"][0]
                mk_o = VOFF["mk"][0]
                scnt = [0]

                def attn_chunks(lhsK, lhsKn, rhsQ, rhsQn, N, extra, scale, scalen, pv_list, chunks, first_last):
                    nch = len(chunks)
                    st = []
                    for idx, c in enumerate(chunks):
                        sl = scnt[0] % 2
                        pl = scnt[0] % 3
                        scnt[0] += 1
                        st.append((c, pSs[sl], "pS%d" % sl, PTs[pl], "PT%d" % pl))

                    def qk(idx):
                        c, pSx, pSn, PT, PTn = st[idx]
                        ex = extra(c)
                        for kk, (lk, lkn, rq, rqn) in enumerate(zip(lhsK, lhsKn, rhsQ, rhsQn)):
                            last = (kk == len(lhsK) - 1) and not ex
                            S.op("pe", lambda e, lk=lk, rq=rq, c=c, kk=kk, last=last, pSx=pSx: e.matmul(pSx[:, 0:N], lhsT=lk[:, c * 128:(c + 1) * 128], rhs=rq, start=(kk == 0), stop=last),
                                 reads=[lkn, rqn], writes=[pSn])
                        for i, (cols, lT, lTn, rh, rhn) in enumerate(ex):
                            S.op("pe", lambda e, cols=cols, lT=lT, rh=rh, i=i, pSx=pSx, nex=len(ex): e.matmul(pSx[:, cols[0]:cols[1]], lhsT=lT, rhs=rh, start=False, stop=(i == nex - 1)),
                                 reads=[lTn, rhn], writes=[pSn])

                    def ex_pv(idx):
                        c, pSx, pSn, PT, PTn = st[idx]
                        for (cols, sc_ap) in scale(c):
                            S.op("act", lambda e, cols=cols, sc_ap=sc_ap, PT=PT, pSx=pSx: e.activation(out=PT[:, cols[0]:cols[1]], in_=pSx[:, cols[0]:cols[1]], func=AF.Exp, scale=sc_ap),
                                 reads=[pSn, scalen], writes=[PTn])
                        seen = set()
                        for (ocols, otile, otn, qcols, vfn, vn) in pv_list:
                            first = (idx == 0) and (otn not in seen)
                            seen.add(otn)
                            lastr = [p[2] for p in pv_list if p[2] == otn][-1] is otn and all(not (p[2] == otn) for p in pv_list[pv_list.index((ocols, otile, otn, qcols, vfn, vn)) + 1:])
                            S.op("pe", lambda e, ocols=ocols, otile=otile, qcols=qcols, vfn=vfn, c=c, PT=PT, first=first, lastm=(idx == nch - 1 and lastr): e.matmul(
                                otile[:, ocols[0]:ocols[1]], lhsT=PT[:, qcols[0]:qcols[1]], rhs=vfn(c), start=first, stop=lastm),
                                reads=[PTn, vn], writes=[otn])

                    qk(0)
                    for idx in range(nch):
                        if idx + 1 < nch:
                            qk(idx + 1)
                        ex_pv(idx)

                for l in range(L):
                    cmax = min(4 * l + 3, NT - 1)
                    S.dma("sp", lambda e, l=l: e.dma_start(out=xt[:], in_=xq_d[l]), writes=["xt"])
                    S.dma("sp", lambda e, l=l: e.dma_start(out=cmask[:], in_=cmask_d[l]), writes=["cmask"])
                    S.dma("sp", lambda e, l=l: e.dma_start(out=pbias[:], in_=pbias_d[l]), writes=["pbias"])
                    xnorm_T(xt, "xt", junk, "xnb", small, "", xnb, "xnb", pT0, "pT0", xnT, "xnT")
                    for (pp, ppn, c0, c1) in ((pP0, "pP0", 0, 512), (pP1, "pP1", 512, Q_COLS)):
                        for k in range(8):
                            S.op("pe", lambda e, k=k, pp=pp, c0=c0, c1=c1: e.matmul(pp[:, 0:c1 - c0], lhsT=xnT[:, k, :], rhs=wq[:, k, c0:c1], start=(k == 0), stop=(k == 7)),
                                 reads=["xnT", "wq"], writes=[ppn])
                    if cut < 10:
                        continue
                    S.op("act", lambda e: e.activation(out=sqj[:, 0:512], in_=pP0[:], func=AF.Square), reads=["pP0"], writes=["sqj"])
                    S.op("dve", lambda e: e.tensor_reduce(out=s8[:], in_=sqj[:, 0:512].rearrange("p (h d) -> p h d", h=8), axis=AX.X, op=ALU.add), reads=["sqj"], writes=["s8"])
                    rstd_from_ss(s8[:], "s8", r8[:], "r8", 64.0, t8[:], "t8")
                    S.op("act", lambda e: e.copy(out=qn[:].rearrange("p h d -> p (h d)"), in_=pP0[:]), reads=["pP0"], writes=["qn"])
                    S.op("dve", lambda e: e.tensor_tensor(out=qn[:], in0=qn[:], in1=r8[:].unsqueeze(2).to_broadcast([128, 8, 64]), op=ALU.mult),
                         reads=["qn", "r8"], writes=["qn"])
                    S.op("dve", lambda e: e.tensor_tensor(out=qn[:], in0=qn[:], in1=gv("qa").unsqueeze(1).to_broadcast([128, 8, 64]), op=ALU.mult),
                         reads=["qn", "vecs"], writes=["qn"])
                    if cut < 10.2:
                        continue
                    for g in range(2):
                        S.op("dve", lambda e, g=g: e.tensor_copy(out=qab[:, :, g, 16:64], in_=qn[:, 4 * g:4 * g + 4, 16:64]), reads=["qn"], writes=["qab"])
                        rope("dve", qn[:, 4 * g:4 * g + 4, :], cosQA[:, l, :], sinQA[:, l, :], qab[:, :, g, :], rtmp[:, :, 0:4, 0:8], ["qn", cosQA_n, sinQA_n], ["qab"], 8)
                    if cut < 10.4:
                        continue
                    for r in range(4):
                        S.op("pe", lambda e, r=r: e.transpose(out=pT1[:, r * 128:(r + 1) * 128], in_=qab[:, r, :, :].rearrange("p g d -> p (g d)"), identity=identb[:]),
                             reads=["qab", "identb"], writes=["pT1"])
                    for g_ in range(2):
                        S.op("act", lambda e, g_=g_: e.copy(out=QaTz[g_][64 * g_:64 * g_ + 64, :, :].rearrange("p r t -> p (r t)"), in_=pT1[64 * g_:64 * g_ + 64, 0:512]), reads=["pT1"], writes=["QaT"])
                    if cut < 10.6:
                        continue
                    S.op("act", lambda e: e.activation(out=gat[:], in_=pP1[:, 0:24], func=AF.Sigmoid), reads=["pP1"], writes=["gat"])
                    ss, rs, tm = small
                    S.op("act", lambda e: e.activation(out=sqj[:, 0:256], in_=pP1[:, 24:280], func=AF.Square, accum_out=ss[:, 1:2]), reads=["pP1"], writes=["sqj", "ss1"])
                    rstd_from_ss(ss[:, 1:2], "ss1", rs[:, 1:2], "rs1", 256.0, tm[:, 1:2], "tm1")
                    S.op("act", lambda e: e.activation(out=cqn[:], in_=pP1[:, 24:280], func=AF.Copy, scale=rs[:, 1:2]), reads=["pP1", "rs1"], writes=["cqn"])
                    if cut < 10.8:
                        continue
                    for k in range(2):
                        S.op("pe", lambda e, k=k: e.transpose(out=pT1[:, 512 + k * 128:512 + (k + 1) * 128], in_=cqn[:, k * 128:(k + 1) * 128], identity=identb[:]),
                             reads=["cqn", "identb"], writes=["pT1b"])
                    S.op("act", lambda e: e.copy(out=cqT[:].rearrange("p k t -> p (k t)"), in_=pT1[:, 512:768]), reads=["pT1b"], writes=["cqT"])
                    for hb in range(2):
                        for k in range(2):
                            S.op("pe", lambda e, hb=hb, k=k: e.matmul(pSs[hb][:, 0:384], lhsT=cqT[:, k, :], rhs=wuq[:, k, hb * 384:(hb + 1) * 384], start=(k == 0), stop=(k == 1)),
                                 reads=["cqT", "wuq"], writes=["pS%d" % hb])
                        S.op("act", lambda e, hb=hb: e.activation(out=sqj[:, hb * 384:(hb + 1) * 384], in_=pSs[hb][:, 0:384], func=AF.Square), reads=["pS%d" % hb], writes=["sqj"])
                    S.op("dve", lambda e: e.tensor_reduce(out=s8[:], in_=sqj[:, 0:768].rearrange("p (h d) -> p h d", h=8), axis=AX.X, op=ALU.add), reads=["sqj"], writes=["s8"])
                    rstd_from_ss(s8[:], "s8", r8[:], "r8", 96.0, t8[:], "t8")
                    for hb in range(2):
                        S.op("act", lambda e, hb=hb: e.copy(out=qm[:, 4 * hb:4 * hb + 4, :].rearrange("p h d -> p (h d)"), in_=pSs[hb][:, 0:384]), reads=["pS%d" % hb], writes=["qm"])
                        S.op("dve", lambda e, hb=hb: e.tensor_tensor(out=qm[:, 4 * hb:4 * hb + 4, :], in0=qm[:, 4 * hb:4 * hb + 4, :],
                                                                   in1=r8[:, 4 * hb:4 * hb + 4].unsqueeze(2).to_broadcast([128, 4, 96]), op=ALU.mult),
                             reads=["qm", "r8"], writes=["qm"])
                    S.op("dve", lambda e: e.tensor_tensor(out=qm[:], in0=qm[:], in1=vecs[:, mq_o:mq_o + 96].unsqueeze(1).to_broadcast([128, 8, 96]), op=ALU.mult),
                         reads=["qm", "vecs"], writes=["qm"])
                    if cut < 10.9:
                        continue
                    S.op("dve", lambda e: e.tensor_tensor(out=qmn[:], in0=qm[:, :, 0:64], in1=vecs[:, mk_o:mk_o + 64].unsqueeze(1).to_broadcast([128, 8, 64]), op=ALU.mult),
                         reads=["qm", "vecs"], writes=["qmn"])
                    if cut < 11:
                        continue
                    rope("dve", qm[:, :, 64:96], cosQB[:, l, :], sinQB[:, l, :], qmr[:], rtmp[:, :, :, :], ["qm", cosQB_n, sinQB_n], ["qmr"], 16)
                    if cut < 11.2:
                        continue
                    for i in range(4):
                        S.op("pe", lambda e, i=i: e.transpose(out=pT0[:, i * 128:(i + 1) * 128], in_=qmn[:, 2 * i:2 * i + 2, :].rearrange("p a d -> p (a d)"), identity=identb[:]),
                             reads=["qmn", "identb"], writes=["pT0"])
                    S.op("act", lambda e: e.copy(out=qnT[:].rearrange("p i t -> p (i t)"), in_=pT0[:, 0:512]), reads=["pT0"], writes=["qnT"])
                    for h in range(8):
                        S.op("pe", lambda e, h=h: e.matmul(pSs[h // 4][:, (h % 4) * 128:(h % 4 + 1) * 128], lhsT=wukT[:, h, :],
                                                          rhs=qnT[:, h // 2, :], start=True, stop=True),
                             reads=["wukT", "qnT"], writes=["pS%d" % (h // 4)])
                    for hb in range(2):
                        S.op("act", lambda e, hb=hb: e.copy(out=QlatT[:, 4 * hb:4 * hb + 4, :].rearrange("p h t -> p (h t)"), in_=pSs[hb][:, 0:512]), reads=["pS%d" % hb], writes=["QlatT"])
                    if cut < 11.5:
                        continue
                    for h in range(8):
                        S.op("pe", lambda e, h=h: e.transpose(out=pT1[0:32, h * 128:(h + 1) * 128], in_=qmr[:, h, :], identity=identb[:]), reads=["qmr", "identb"], writes=["pT1", "pT1b"])
                    S.op("act", lambda e: e.copy(out=QropeT[0:32, :, :].rearrange("p h t -> p (h t)"), in_=pT1[0:32, 0:1024]), reads=["pT1", "pT1b"], writes=["QropeT"])

                    if cut < 12:
                        continue
                    nvmax = min(8 * (4 * l + 3) + 7, NCMP)
                    nvk = (nvmax + 127) // 128
                    nvc = nvk * 128
                    wc0 = max(0, 4 * l - 4)
                    nwc = cmax - wc0 + 1
                    S.dma("sp", lambda e, wc0=wc0, nwc=nwc: e.dma_start(out=KwTw[:, 0:nwc * 128], in_=kw_d[:, wc0 * 128:(wc0 + nwc) * 128]), reads=["kw_scr"], writes=["KwTw"])
                    S.dma("sp", lambda e, wc0=wc0, nwc=nwc: e.dma_start(out=Vww[:, 0:nwc, :, :].rearrange("p c g d -> p c (g d)"), in_=vw_d[:, wc0:wc0 + nwc, :]), reads=["vw_scr"], writes=["Vww"])
                    for g in range(2):
                        for r in range(4):
                            pSx, pSn = pSs[r % 2], "pS%d" % (r % 2)
                            S.op("pe", lambda e, r=r, g=g, pSx=pSx: e.matmul(pSx[:, 0:nvc], lhsT=QaTz[g][:, r, :], rhs=KcT[:, 0:nvc], start=True, stop=True),
                                 reads=["QaT", "KcT"], writes=[pSn])
                            S.op("act", lambda e, r=r, pSx=pSx: e.activation(out=Pc[:, r, 0:nvc], in_=pSx[:, 0:nvc], func=AF.Exp, scale=SCALE_A), reads=[pSn], writes=["Pc"])
                        if cut < 13:
                            continue
                        S.op("dve", lambda e: e.tensor_tensor(out=Pc[:, :, 0:nvc], in0=Pc[:, :, 0:nvc], in1=cmask[:, 0:nvc].unsqueeze(1).to_broadcast([128, 4, nvc]), op=ALU.mult),
                             reads=["Pc", "cmask"], writes=["Pc"])
                        S.op("dve", lambda e: e.tensor_reduce(out=z4[:], in_=Pc[:, :, 0:nvc], axis=AX.X, op=ALU.add), reads=["Pc"], writes=["z4"])
                        S.op("dve", lambda e: e.tensor_scalar(out=z4[:], in0=z4[:], scalar1=1e-30, scalar2=None, op0=ALU.max), reads=["z4"], writes=["z4"])
                        S.op("dve", lambda e: e.reciprocal(out=z4[:], in_=z4[:]), reads=["z4"], writes=["z4"])
                        S.op("dve", lambda e: e.tensor_tensor(out=Pc[:, :, 0:nvc], in0=Pc[:, :, 0:nvc], in1=z4[:].unsqueeze(2).to_broadcast([128, 4, nvc]), op=ALU.mult),
                             reads=["Pc", "z4"], writes=["Pc"])
                        S.op("dve", lambda e: e.tensor_tensor(out=Psm[:, 0:nvc], in0=Pc[:, 0, 0:nvc], in1=Pc[:, 1, 0:nvc], op=ALU.add), reads=["Pc"], writes=["Psm"])
                        S.op("dve", lambda e: e.tensor_tensor(out=Psm[:, 0:nvc], in0=Psm[:, 0:nvc], in1=Pc[:, 2, 0:nvc], op=ALU.add), reads=["Pc", "Psm"], writes=["Psm"])
                        S.op("dve", lambda e: e.tensor_tensor(out=Psm[:, 0:nvc], in0=Psm[:, 0:nvc], in1=Pc[:, 3, 0:nvc], op=ALU.add), reads=["Pc", "Psm"], writes=["Psm"])
                        for k in range(nvk):
                            S.op("pe", lambda e, k=k: e.transpose(out=pX[:, k * 128:(k + 1) * 128], in_=Psm[:, k * 128:(k + 1) * 128], identity=ident[:]), reads=["Psm", "ident"], writes=["pX"])
                        S.op("act", lambda e: e.copy(out=PsT[:, 0:nvk, :].rearrange("p k t -> p (k t)"), in_=pX[:, 0:nvc]), reads=["pX"], writes=["PsT"])
                        if cut < 14:
                            continue
                        for k in range(nvk):
                            S.op("pe", lambda e, k=k: e.matmul(pP0[:, 0:NSLC], lhsT=PsT[:, k, :], rhs=ovl[:, k, 0:NSLC], start=(k == 0), stop=(k == nvk - 1)), reads=["PsT", "ovl"], writes=["pP0"])
                        S.op("dve", lambda e: e.tensor_tensor(out=impb[:, 0:NSLC], in0=pP0[:, 0:NSLC], in1=pbias[:, 0:NSLC], op=ALU.add), reads=["pP0", "pbias"], writes=["impb"])
                        if cut < 14.3:
                            continue
                        S.op("dve", lambda e: e.max(out=m8[:, 0:8], in_=impb[:, 0:NSLC]), reads=["impb"], writes=["m8a"])
                        S.op("dve", lambda e: e.match_replace(out=wk[:, 0:NSLC], in_to_replace=m8[:, 0:8], in_values=impb[:, 0:NSLC], imm_value=-3e38), reads=["impb", "m8a"], writes=["wk"])
                        S.op("dve", lambda e: e.max(out=m8[:, 8:16], in_=wk[:, 0:NSLC]), reads=["wk"], writes=["m8b"])
                        S.op("dve", lambda e: e.tensor_scalar(out=nsl[:, 0:NSLC], in0=impb[:, 0:NSLC], scalar1=m8[:, 15:16], scalar2=1.0, op0=ALU.is_ge, op1=ALU.subtract),
                             reads=["impb", "m8b"], writes=["nsl"])
                        S.op("dve", lambda e: e.tensor_scalar(out=nslb[:], in0=nsl[:], scalar1=-NEGM, scalar2=None, op0=ALU.mult), reads=["nsl"], writes=["nslb"])
                        if cut < 14.6:
                            continue
                        S.op("pe", lambda e: e.transpose(out=pT0[:, 0:128], in_=nslb[:], identity=identb[:]), reads=["nslb", "identb"], writes=["pT0"])
                        for hv in range(2):
                            S.op("act", lambda e, hv=hv: e.copy(out=nselZ[hv][64 * hv:64 * hv + 64, :, :], in_=pT0[64 * hv:64 * hv + 64, 0:128].unsqueeze(1).to_broadcast([64, 4, 128])), reads=["pT0"], writes=["nselT4"])
                        if cut < 15:
                            continue
                        if dbg and l == L - 1 and g == 0:
                            o = dout("d_impb", [128, 128]); S.dma("sp", lambda e, o=o: e.dma_start(out=o, in_=impb[:]), reads=["impb"], writes=["d_impb"])
                            o = dout("d_nsl", [128, 128]); S.dma("sp", lambda e, o=o: e.dma_start(out=o, in_=nsl[:]), reads=["nsl"], writes=["d_nsl"])
                        for r in range(4):
                            S.op("pool", lambda e, r=r: e.tensor_copy(out=Pcb[:, 0:nvc], in_=Pc[:, r, 0:nvc]), reads=["Pc"], writes=["Pcb"])
                            for k in range(nvk):
                                S.op("pe", lambda e, r=r, k=k: e.transpose(out=pT1[:, k * 128:(k + 1) * 128], in_=Pcb[:, k * 128:(k + 1) * 128], identity=identb[:]),
                                     reads=["Pcb", "identb"], writes=["pT1"])
                            S.op("act", lambda e: e.copy(out=PcT[:, 0:nvk, :].rearrange("p k t -> p (k t)"), in_=pT1[:, 0:nvc]), reads=["pT1"], writes=["PcT"])
                            for k in range(nvk):
                                S.op("pe", lambda e, r=r, k=k, g=g: e.matmul(pO[:, r * 64:(r + 1) * 64], lhsT=PcT[:, k, :], rhs=Vc[:, k, g, :], start=(k == 0 and r == 0), stop=(k == nvk - 1 and r == 3)),
                                     reads=["PcT", "Vc"], writes=["pO"])
                        for r in range(4):
                            h = 4 * g + r
                            S.op("dve", lambda e, r=r, h=h: e.tensor_scalar(out=oa[:, h, :], in0=pO[:, r * 64:(r + 1) * 64], scalar1=gat[:, 3 * h:3 * h + 1], scalar2=None, op0=ALU.mult),
                                 reads=["pO", "gat"], writes=["oa"])
                        if cut < 16:
                            continue
                        QaTg = QaTz[g][:, :, :].rearrange("p r t -> p (r t)")
                        for br in range(2):
                            if br == 1 and cut < 17:
                                continue
                            if br == 0:
                                chunks = list(range(0, cmax + 1))
                                Kt, Ktn, Vt, Vtn = KsT, "KsT", Vs, "Vs"

                                def extra(c, l=l, g=g):
                                    ex = [((0, 512), e64[:, (c % 32) * 128:(c % 32 + 1) * 128], "e64",
                                           nselZ[c // 32][:, :, :].rearrange("p r t -> p (r t)"), "nselT4")]
                                    if c >= 4 * l:
                                        for r in range(4):
                                            ex.append(((r * 128, (r + 1) * 128), identb[:], "identb", dmask[:, c - 4 * l, :], "dmask"))
                                    return ex
                            else:
                                chunks = list(range(0, cmax - wc0 + 1))
                                Kt, Ktn, Vt, Vtn = KwTw, "KwTw", Vww, "Vww"

                                def extra(c, l=l, g=g, wc0=wc0):
                                    return [((r * 128, (r + 1) * 128), identb[:], "identb", wmask[:, c + wc0 - (4 * l - 4), :], "wmask") for r in range(4)]
                            pv_list = [((r * 65, (r + 1) * 65), pO, "pO", (r * 128, (r + 1) * 128), (lambda c, Vt=Vt, g=g: Vt[:, c, g, :]), Vtn) for r in range(4)]
                            attn_chunks([Kt[:, :]], [Ktn], [QaTg], ["QaT"], 512, extra, (lambda c: [((0, 512), SCALE_A)]), "vecs", pv_list, chunks, None)
                            S.op("dve", lambda e: e.tensor_scalar(out=z4[:], in0=pO[:, 0:260].rearrange("p (r d) -> p r d", r=4)[:, :, 64], scalar1=1e-30, scalar2=None, op0=ALU.max), reads=["pO"], writes=["z4"])
                            S.op("dve", lambda e: e.reciprocal(out=z4[:], in_=z4[:]), reads=["z4"], writes=["z4"])
                            gsl = gat[:, 12 * g:12 * g + 12].rearrange("p (r t) -> p r t", t=3)[:, :, 1 + br]
                            S.op("dve", lambda e, gsl=gsl: e.tensor_tensor(out=f4[:], in0=z4[:], in1=gsl, op=ALU.mult), reads=["z4", "gat"], writes=["f4"])
                            for r in range(4):
                                h = 4 * g + r
                                S.op("dve", lambda e, r=r, h=h: e.scalar_tensor_tensor(out=oa[:, h, :], in0=pO[:, r * 65:r * 65 + 64], scalar=f4[:, r:r + 1], in1=oa[:, h, :], op0=ALU.mult, op1=ALU.add),
                                     reads=["pO", "f4", "oa"], writes=["oa"])
                    if cut < 18:
                        continue
                    chunks = list(range(0, cmax + 1))
                    for hb in range(2):
                        def extra(c, l=l):
                            if c >= 4 * l:
                                return [((r * 128, (r + 1) * 128), identb[:], "identb", dmask[:, c - 4 * l, :], "dmask") for r in range(4)]
                            return []
                        pv_list = []
                        for r in range(4):
                            ot, otn = (pO, "pO") if r < 2 else (pX, "pX")
                            pv_list.append((((r % 2) * 129, (r % 2 + 1) * 129), ot, otn, (r * 128, (r + 1) * 128), (lambda c: ckva[:, c, :]), "ckva"))
                        attn_chunks([ckvT[:, :], krT[:, :]], ["ckvT", "krT"],
                                    [QlatT[:, 4 * hb:4 * hb + 4, :].rearrange("p h t -> p (h t)"), QropeT[:, 4 * hb:4 * hb + 4, :].rearrange("p h t -> p (h t)")], ["QlatT", "QropeT"],
                                    512, extra, (lambda c, hb=hb: [((r * 128, (r + 1) * 128), srh[:, c, 4 * hb + r:4 * hb + r + 1]) for r in range(4)]), "srh", pv_list, chunks, None)
                        for r in range(4):
                            h = 4 * hb + r
                            ot, otn = (pO, "pO") if r < 2 else (pX, "pX")
                            b0 = (r % 2) * 129
                            S.op("dve", lambda e, ot=ot, b0=b0: e.reciprocal(out=z4[:, 0:1], in_=ot[:, b0 + 128:b0 + 129]), reads=[otn], writes=["z4"])
                            S.op("act", lambda e, ot=ot, b0=b0: e.activation(out=olb[:], in_=ot[:, b0:b0 + 128], func=AF.Copy, scale=z4[:, 0:1]), reads=[otn, "z4"], writes=["olb"])
                            S.op("pe", lambda e: e.transpose(out=pT0[:, 0:128], in_=olb[:], identity=identb[:]), reads=["olb", "identb"], writes=["pT0"])
                            S.op("act", lambda e: e.copy(out=OlatT[:], in_=pT0[:, 0:128]), reads=["pT0"], writes=["OlatT"])
                            S.op("pe", lambda e, h=h: e.matmul(pP0[:, h * 64:(h + 1) * 64], lhsT=OlatT[:], rhs=wuv[:, h * 64:(h + 1) * 64], start=True, stop=True), reads=["OlatT", "wuv"], writes=["pP0"])
                    if cut < 19:
                        continue
                    S.op("act", lambda e: e.activation(out=sqj[:, 0:512], in_=oa[:].rearrange("p h d -> p (h d)"), func=AF.Square, accum_out=ss[:, 2:3]), reads=["oa"], writes=["sqj", "ss2"])
                    S.op("act", lambda e: e.activation(out=sqj[:, 0:512], in_=pP0[:], func=AF.Square, accum_out=ss[:, 3:4]), reads=["pP0"], writes=["sqj", "ss2"])
                    rstd_from_ss(ss[:, 2:4], "ss2", rs[:, 2:4], "rs2", 512.0, tm[:, 2:4], "tm2")
                    S.op("dve", lambda e: e.tensor_scalar(out=onb[:, 0:512], in0=oa[:].rearrange("p h d -> p (h d)"), scalar1=rs[:, 2:3], scalar2=None, op0=ALU.mult), reads=["oa", "rs2"], writes=["xnb"])
                    S.op("act", lambda e: e.activation(out=onb[:, 512:1024], in_=pP0[:], func=AF.Copy, scale=rs[:, 3:4]), reads=["pP0", "rs2"], writes=["xnb"])
                    if dbg:
                        o = dout("d_oa%d" % l, [128, 512]); S.dma("sp", lambda e, o=o: e.dma_start(out=o, in_=oa[:].rearrange("p h d -> p (h d)")), reads=["oa"], writes=["d_oa%d" % l])
                        o = dout("d_on%d" % l, [128, 1024], BF16); S.dma("sp", lambda e, o=o: e.dma_start(out=o, in_=onb[:]), reads=["xnb"], writes=["d_on%d" % l])
                    for k in range(8):
                        S.op("pe", lambda e, k=k: e.transpose(out=pT1[:, k * 128:(k + 1) * 128], in_=onb[:, k * 128:(k + 1) * 128], identity=identb[:]), reads=["xnb", "identb"], writes=["pT1", "pT1b"])
                    S.op("act", lambda e: e.copy(out=onT[:].rearrange("p k t -> p (k t)"), in_=pT1[:, 0:1024]), reads=["pT1", "pT1b"], writes=["xnT"])
                    for hf in range(2):
                        for k in range(8):
                            S.op("pe", lambda e, hf=hf, k=k: e.matmul(pSs[hf][:, 0:512], lhsT=onT[:, k, :], rhs=wout[:, k, hf * 512:(hf + 1) * 512], start=(k == 0), stop=(k == 7)),
                                 reads=["xnT", "wout"], writes=["pS%d" % hf])
                        S.op("dve", lambda e, hf=hf: e.tensor_tensor(out=ht[:, hf * 512:(hf + 1) * 512], in0=pSs[hf][:, 0:512], in1=xt[:, hf * 512:(hf + 1) * 512], op=ALU.add),
                             reads=["pS%d" % hf, "xt"], writes=["xt"])
                    S.dma("sp", lambda e, l=l: e.dma_start(out=hs_d[l], in_=ht[:]), reads=["xt"], writes=["hs"])
                    if dbg:
                        o = dout("d_h%d" % l, [128, 1024]); S.dma("sp", lambda e, o=o: e.dma_start(out=o, in_=ht[:]), reads=["xt"], writes=["d_h%d" % l])
                S.barrier()

        S.barrier()
        kvst.close()
        if PHASE3:
            with ExitStack() as p3:
                wpq = sb("wpq", [128, 8, 2048], BF16, p3)
                skT = sb("skT", [128, 16, 128], BF16, p3)
                n2g = sb("n2g", [128, 1024], F32, p3)
                iota16 = sb("iota16", [128, 16], F32, p3)
                S.dma("sp", lambda e: e.dma_start(out=n2g[:], in_=n2_d), writes=["n2g"])
                S.dma("sp", lambda e: e.dma_start(out=iota16[:], in_=iota_d), writes=["iota16"])
                with ExitStack() as ld:
                    load_w(ld, wpq, "wpq", wpq_d, 8, 2048)
                    load_w(ld, skT, "skT", skT_d, 16, 128)
                    S.barrier()
                ht = sb("ht3", [128, 1024], F32, p3)
                hn = sb("hn", [128, 1024], F32, p3)
                hnb = sb("hnb", [128, 1024], BF16, p3)
                junk = sb("junk3", [128, 1024], BF16, p3)
                junkf = sb("junkf", [128, 1024], F32, p3)
                hnT = sb("hnT", [128, 8, 128], BF16, p3)
                small = (sb("ss_3", [128, 8], F32, p3), sb("rs_3", [128, 8], F32, p3), sb("tm_3", [128, 8], F32, p3))
                sq2 = sb("sq2", [128, 2048], F32, p3)
                s8 = sb("s8_3", [128, 8], F32, p3)
                r8 = sb("r8_3", [128, 8], F32, p3)
                t8 = sb("t8_3", [128, 8], F32, p3)
                qpb = sb("qpb", [128, 2048], BF16, p3)
                qT = sb("qT", [128, 16, 128], BF16, p3)
                sc = sb("sc", [128, 16, 128], F32, p3)
                wk3 = sb("wk3", [128, 256], F32, p3)
                v16 = sb("v16", [128, 16, 16], F32, p3)
                i16 = sb("i16", [128, 16, 16], U32, p3)
                i16f = sb("i16f", [128, 16, 16], F32, p3)
                cand = sb("cand", [128, 8, 16, 16], F32, p3)
                vals = sb("vals", [128, 8, 16], F32, p3)
                ci = sb("ci", [128, 8, 16], U32, p3)
                ca = sb("ca", [128, 8, 16], I32, p3)
                cb = sb("cb", [128, 8, 16], I32, p3)
                caf = sb("caf", [128, 8, 16], F32, p3)
                cbf = sb("cbf", [128, 8, 16], F32, p3)
                oh = sb("oh", [128, 8, 16, 16], F32, p3)
                e1 = sb("e1", [128, 8, 16], F32, p3)
                e2 = sb("e2", [128, 8, 16], F32, p3)
                ei = sb("ei", [128, 128], I32, p3)
                gts = sb("gts", [128, 8, 16], F32, p3)
                gs8 = sb("gs8", [128, 8], F32, p3)
                av = sb("av", [128, 128], F32, p3)
                wv = sb("wv", [128, 128], F32, p3)
                NB = 4
                ugs = [sb("ug%d" % i, [128, 1024], F32, p3) for i in range(NB)]
                vgs = [sb("vg%d" % i, [128, 1024], BF16, p3) for i in range(NB)]
                dgs = [sb("dg%d" % i, [128, 128], BF16, p3) for i in range(NB)]
                ot = sb("ot", [128, 1024], F32, p3)
                pT3 = ps("pT3", [128, 1024], BF16, p3)
                pT4 = ps("pT4", [128, 1024], BF16, p3)
                pQ = [ps("pQ%d" % i, [128, 512], F32, p3) for i in range(4)]
                pY = [ps("pY%d" % i, [128, 512], F32, p3) for i in range(2)]
                for l in range(L):
                    S.dma("sp", lambda e, l=l: e.dma_start(out=ht[:], in_=hs_d[l]), reads=["hs"], writes=["ht3"])
                    ss, rs, tm = small
                    S.op("act", lambda e: e.activation(out=junk[:], in_=ht[:], func=AF.Square, accum_out=ss[:, 0:1]), reads=["ht3"], writes=["junk3", "ss_3"])
                    rstd_from_ss(ss[:, 0:1], "ss_3", rs[:, 0:1], "rs_3", 1024.0, tm[:, 0:1], "tm_3")
                    S.op("dve", lambda e: e.scalar_tensor_tensor(out=hn[:], in0=ht[:], scalar=rs[:, 0:1], in1=n2g[:], op0=ALU.mult, op1=ALU.mult), reads=["ht3", "rs_3", "n2g"], writes=["hn"])
                    S.op("dve", lambda e: e.tensor_copy(out=hnb[:], in_=hn[:]), reads=["hn"], writes=["hnb"])
                    for k in range(8):
                        S.op("pe", lambda e, k=k: e.transpose(out=pT3[:, k * 128:(k + 1) * 128], in_=hnb[:, k * 128:(k + 1) * 128], identity=identb[:]), reads=["hnb", "identb"], writes=["pT3"])
                    S.op("act", lambda e: e.copy(out=hnT[:].rearrange("p k t -> p (k t)"), in_=pT3[:, 0:1024]), reads=["pT3"], writes=["hnT"])
                    for q4 in range(4):
                        for k in range(8):
                            S.op("pe", lambda e, q4=q4, k=k: e.matmul(pQ[q4][:, 0:512], lhsT=hnT[:, k, :], rhs=wpq[:, k, q4 * 512:(q4 + 1) * 512], start=(k == 0), stop=(k == 7)),
                                 reads=["hnT", "wpq"], writes=["pQ%d" % q4])
                        S.op("act", lambda e, q4=q4: e.activation(out=sq2[:, q4 * 512:(q4 + 1) * 512], in_=pQ[q4][:, 0:512], func=AF.Square), reads=["pQ%d" % q4], writes=["sq2"])
                    S.op("dve", lambda e: e.tensor_reduce(out=s8[:], in_=sq2[:].rearrange("p (h d) -> p h d", h=8), axis=AX.X, op=ALU.add), reads=["sq2"], writes=["s8_3"])
                    rstd_from_ss(s8[:], "s8_3", r8[:], "r8_3", 256.0, t8[:], "t8_3")
                    for q4 in range(4):
                        S.op("act", lambda e, q4=q4: e.copy(out=sq2[:, q4 * 512:(q4 + 1) * 512], in_=pQ[q4][:, 0:512]), reads=["pQ%d" % q4, "s8_3"], writes=["sq2"])
                        S.op("dve", lambda e, q4=q4: e.tensor_tensor(out=qpb[:, q4 * 512:(q4 + 1) * 512].rearrange("p (h d) -> p h d", h=2), in0=sq2[:, q4 * 512:(q4 + 1) * 512].rearrange("p (h d) -> p h d", h=2),
                                                                   in1=r8[:, 2 * q4:2 * q4 + 2].unsqueeze(2).to_broadcast([128, 2, 256]), op=ALU.mult), reads=["sq2", "r8_3"], writes=["qpb"])
                    for i in range(16):
                        pt, ptn = (pT3, "pT3") if i < 8 else (pT4, "pT4")
                        S.op("pe", lambda e, i=i, pt=pt: e.transpose(out=pt[:, (i % 8) * 128:(i % 8 + 1) * 128], in_=qpb[:, i * 128:(i + 1) * 128], identity=identb[:]), reads=["qpb", "identb"], writes=[ptn])
                    S.op("act", lambda e: e.copy(out=qT[:, 0:8, :].rearrange("p k t -> p (k t)"), in_=pT3[:, 0:1024]), reads=["pT3"], writes=["qT"])
                    S.op("act", lambda e: e.copy(out=qT[:, 8:16, :].rearrange("p k t -> p (k t)"), in_=pT4[:, 0:1024]), reads=["pT4"], writes=["qT"])
                    for i in range(16):
                        S.op("pe", lambda e, i=i: e.matmul(pQ[i // 4][:, (i % 4) * 128:(i % 4 + 1) * 128], lhsT=qT[:, i, :], rhs=skT[:, i, :], start=True, stop=True), reads=["qT", "skT"], writes=["pQ%d" % (i // 4)])
                    for q4 in range(4):
                        S.op("act", lambda e, q4=q4: e.copy(out=sc[:, 4 * q4:4 * q4 + 4, :].rearrange("p a n -> p (a n)"), in_=pQ[q4][:, 0:512]), reads=["pQ%d" % q4], writes=["sc"])
                    for i in range(16):
                        S.op("dve", lambda e, i=i: e.max(out=v16[:, i, 0:8], in_=sc[:, i, :]), reads=["sc"], writes=["v16"])
                        S.op("dve", lambda e, i=i: e.max_index(out=i16[:, i, 0:8], in_max=v16[:, i, 0:8], in_values=sc[:, i, :]), reads=["sc", "v16"], writes=["i16"])
                        S.op("dve", lambda e, i=i: e.match_replace(out=wk3[:, 0:128], in_to_replace=v16[:, i, 0:8], in_values=sc[:, i, :], imm_value=-3e38), reads=["sc", "v16"], writes=["wk3"])
                        S.op("dve", lambda e, i=i: e.max(out=v16[:, i, 8:16], in_=wk3[:, 0:128]), reads=["wk3"], writes=["v16"])
                        S.op("dve", lambda e, i=i: e.max_index(out=i16[:, i, 8:16], in_max=v16[:, i, 8:16], in_values=wk3[:, 0:128]), reads=["wk3", "v16"], writes=["i16"])
                    S.op("dve", lambda e: e.tensor_copy(out=i16f[:], in_=i16[:]), reads=["i16"], writes=["i16f"])
                    v4 = v16[:].rearrange("p (h two) k -> p h two k", two=2)
                    i4 = i16f[:].rearrange("p (h two) k -> p h two k", two=2)
                    for h in range(8):
                        S.op("dve", lambda e, h=h: e.tensor_tensor(out=cand[:, h, :, :], in0=v4[:, h, 0, :].unsqueeze(2).to_broadcast([128, 16, 16]),
                                                                 in1=v4[:, h, 1, :].unsqueeze(1).to_broadcast([128, 16, 16]), op=ALU.add), reads=["v16"], writes=["cand"])
                    for h in range(8):
                        cf = cand[:, h, :, :].rearrange("p a b -> p (a b)")
                        S.op("dve", lambda e, h=h, cf=cf: e.max(out=vals[:, h, 0:8], in_=cf), reads=["cand"], writes=["vals"])
                        S.op("dve", lambda e, h=h, cf=cf: e.max_index(out=ci[:, h, 0:8], in_max=vals[:, h, 0:8], in_values=cf), reads=["cand", "vals"], writes=["ci"])
                        S.op("dve", lambda e, h=h, cf=cf: e.match_replace(out=wk3[:], in_to_replace=vals[:, h, 0:8], in_values=cf, imm_value=-3e38), reads=["cand", "vals"], writes=["wk3"])
                        S.op("dve", lambda e, h=h: e.max(out=vals[:, h, 8:16], in_=wk3[:]), reads=["wk3"], writes=["vals"])
                        S.op("dve", lambda e, h=h: e.max_index(out=ci[:, h, 8:16], in_max=vals[:, h, 8:16], in_values=wk3[:]), reads=["wk3", "vals"], writes=["ci"])
                    S.op("dve", lambda e: e.tensor_tensor(out=gts[:], in0=vals[:], in1=vals[:, :, 0:1].to_broadcast([128, 8, 16]), op=ALU.subtract), reads=["vals"], writes=["gts"])
                    S.op("act", lambda e: e.activation(out=gts[:], in_=gts[:], func=AF.Exp), reads=["gts"], writes=["gts"])
                    S.op("dve", lambda e: e.tensor_reduce(out=gs8[:], in_=gts[:], axis=AX.X, op=ALU.add), reads=["gts"], writes=["gs8"])
                    S.op("dve", lambda e: e.reciprocal(out=gs8[:], in_=gs8[:]), reads=["gs8"], writes=["gs8"])
                    S.op("dve", lambda e: e.tensor_tensor(out=gts[:], in0=gts[:], in1=gs8[:].unsqueeze(2).to_broadcast([128, 8, 16]), op=ALU.mult), reads=["gts", "gs8"], writes=["gts"])
                    cii = ci[:].bitcast(I32)
                    S.op("dve", lambda e: e.tensor_single_scalar(out=ca[:], in_=cii, scalar=4, op=ALU.arith_shift_right), reads=["ci"], writes=["ca"])
                    S.op("dve", lambda e: e.tensor_single_scalar(out=cb[:], in_=cii, scalar=15, op=ALU.bitwise_and), reads=["ci"], writes=["cb"])
                    S.op("dve", lambda e: e.tensor_copy(out=caf[:], in_=ca[:]), reads=["ca"], writes=["caf"])
                    S.op("dve", lambda e: e.tensor_copy(out=cbf[:], in_=cb[:]), reads=["cb"], writes=["cbf"])
                    for (cf_, cfn, half, eo, eon) in ((caf, "caf", 0, e1, "e1"), (cbf, "cbf", 1, e2, "e2")):
                        for h in range(8):
                            S.op("dve", lambda e, h=h, cf_=cf_: e.tensor_tensor(out=oh[:, h, :, :], in0=cf_[:, h, :].unsqueeze(2).to_broadcast([128, 16, 16]),
                                                                             in1=iota16[:].unsqueeze(1).to_broadcast([128, 16, 16]), op=ALU.is_equal), reads=[cfn, "iota16"], writes=["oh"])
                            S.op("dve", lambda e, h=h, half=half: e.tensor_tensor(out=oh[:, h, :, :], in0=oh[:, h, :, :], in1=i4[:, h, half, :].unsqueeze(1).to_broadcast([128, 16, 16]), op=ALU.mult),
                                 reads=["oh", "i16f"], writes=["oh"])
                        S.op("dve", lambda e, eo=eo: e.tensor_reduce(out=eo[:].rearrange("p h k -> p (h k)"), in_=oh[:].rearrange("p h k a -> p (h k) a"), axis=AX.X, op=ALU.add), reads=["oh"], writes=[eon])
                    S.op("dve", lambda e: e.scalar_tensor_tensor(out=e1[:], in0=e1[:], scalar=128.0, in1=e2[:], op0=ALU.mult, op1=ALU.add), reads=["e1", "e2"], writes=["e1"])
                    S.op("dve", lambda e: e.tensor_copy(out=ei[:], in_=e1[:].rearrange("p h k -> p (h k)")), reads=["e1"], writes=["ei"])
                    if dbg and l == 0:
                        o = dout("d_ei", [128, 128], I32); S.dma("sp", lambda e, o=o: e.dma_start(out=o, in_=ei[:]), reads=["ei"], writes=["d_ei"])
                        o = dout("d_gts", [128, 128]); S.dma("sp", lambda e, o=o: e.dma_start(out=o, in_=gts[:].rearrange("p h k -> p (h k)")), reads=["gts"], writes=["d_gts"])
                    for s_ in range(128):
                        b_ = s_ % NB
                        S.dma("pool", lambda e, s_=s_, b_=b_: e.indirect_dma_start(out=ugs[b_][:], out_offset=None, in_=pu_d, in_offset=bass.IndirectOffsetOnAxis(ap=ei[:, s_:s_ + 1], axis=0)),
                              reads=["ei"], writes=["ug%d" % b_])
                        S.op("dve", lambda e, s_=s_, b_=b_: e.scalar_tensor_tensor(out=junkf[:], in0=ugs[b_][:], scalar=1.0, in1=hn[:], op0=ALU.mult, op1=ALU.mult, accum_out=av[:, s_:s_ + 1]),
                             reads=["ug%d" % b_, "hn"], writes=["junkf", "av"])
                    S.op("act", lambda e: e.activation(out=wv[:], in_=av[:], func=AF.Gelu_apprx_tanh), reads=["av"], writes=["wv"])
                    S.op("dve", lambda e: e.tensor_tensor(out=wv[:], in0=wv[:], in1=gts[:].rearrange("p h k -> p (h k)"), op=ALU.mult), reads=["wv", "gts"], writes=["wv"])
                    for s_ in range(128):
                        b_ = s_ % NB
                        S.dma("pool", lambda e, s_=s_, b_=b_: e.indirect_dma_start(out=vgs[b_][:], out_offset=None, in_=pv_d, in_offset=bass.IndirectOffsetOnAxis(ap=ei[:, s_:s_ + 1], axis=0)),
                              reads=["ei"], writes=["vg%d" % b_])
                        S.op("act", lambda e, s_=s_, b_=b_: e.activation(out=dgs[b_][:], in_=identb[:], func=AF.Copy, scale=wv[:, s_:s_ + 1]), reads=["identb", "wv"], writes=["dg%d" % b_])
                        for hf in range(2):
                            S.op("pe", lambda e, s_=s_, b_=b_, hf=hf: e.matmul(pY[hf][:, 0:512], lhsT=dgs[b_][:], rhs=vgs[b_][:, hf * 512:(hf + 1) * 512], start=(s_ == 0), stop=(s_ == 127)),
                                 reads=["dg%d" % b_, "vg%d" % b_], writes=["pY%d" % hf])
                    for hf in range(2):
                        S.op("dve", lambda e, hf=hf: e.tensor_tensor(out=ot[:, hf * 512:(hf + 1) * 512], in0=pY[hf][:, 0:512], in1=ht[:, hf * 512:(hf + 1) * 512], op=ALU.add), reads=["pY%d" % hf, "ht3"], writes=["ot"])
                    S.dma("sp", lambda e, l=l: e.dma_start(out=out_d[l], in_=ot[:]), reads=["ot"], writes=["out"])
                S.barrier()

        finals = list(dbg_outs.keys())
        if PHASE3:
            finals.append("out")
        elif PHASE2:
            finals.append("hs")
        S.emit(final_wait_keys=finals)
    return nc, list(dbg_outs.keys())


def _kc(w):
    K = w.shape[0] // 128
    return np.ascontiguousarray(w.reshape(K, 128, -1).transpose(1, 0, 2))


def _consts(NT, L, j):
    SEQ = NT * 128
    NCMP = (SEQ - 32) // 16 + 1
    NCK = (NCMP + 127) // 128
    NCP = NCK * 128
    NSLC = SEQ // 64
    bf = ml_dtypes.bfloat16
    c = {}
    c["ident"] = np.eye(128, dtype=np.float32)
    jj = np.arange(64)[:, None, None]
    cc = np.arange(32)[None, :, None]
    s = np.arange(128)[None, None, :]
    e = (jj == 2 * cc + s // 64).astype(np.float32).reshape(64, 32 * 128)
    c["e64"] = np.concatenate([e, e], 0).astype(np.float32).reshape(128, 2, 2048)
    n = np.arange(NCP)
    cs, ce = 16 * n, 16 * n + 31
    sl = 64 * np.arange(128)
    ov = ((cs[:, None] < sl[None, :] + 64) & (ce[:, None] >= sl[None, :]) & (n[:, None] < NCMP) & (np.arange(128)[None, :] < NSLC)).astype(np.float32)
    c["ovl"] = np.ascontiguousarray(ov.reshape(NCK, 128, 128).transpose(1, 0, 2))
    qi = np.arange(128)
    cm = np.zeros((L, 128, NCP), np.float32)
    pb = np.zeros((L, 128, 128), np.float32)
    for l in range(L):
        t = 128 * (4 * l + j) + qi
        cm[l] = ((ce[None, :] <= t[:, None]) & (n[None, :] < NCMP)).astype(np.float32)
        jb = np.arange(128)[None, :]
        causal = (64 * jb <= t[:, None]) & (jb < NSLC)
        b = np.where(causal, 0.0, -1e30)
        b = np.where(jb == (t // 64)[:, None], 1e9, b)
        b = np.where(jb == 0, 2e9, b)
        pb[l] = b
    c["cmask"] = cm
    c["pbias"] = pb
    sq = np.arange(128)
    tri_d = np.where(sq[:, None] <= sq[None, :], 0.0, NEGM)
    tri_w = np.where(sq[:, None] > sq[None, :], 0.0, NEGM)
    full0 = np.zeros((128, 128))
    fulln = np.full((128, 128), NEGM)
    dm = np.stack([full0 if k < j else (tri_d if k == j else fulln) for k in range(4)], 1)
    c["dmask"] = dm.astype(np.float32)
    wm = []
    for k in range(8):
        d = k - 4 - j
        wm.append(fulln if d < -4 else tri_w if d == -4 else full0 if d < 0 else tri_d if d == 0 else fulln)
    c["wmask"] = np.stack(wm, 1).astype(np.float32)
    c["iota16"] = np.tile(np.arange(16, dtype=np.float32)[None, :], (128, 1))
    invA = (500000.0 ** (-np.arange(0, 16, 2, dtype=np.float32) / np.float32(16))).astype(np.float32)
    invB = (500000.0 ** (-np.arange(0, 32, 2, dtype=np.float32) / np.float32(32))).astype(np.float32)
    c["invf"] = np.tile(np.concatenate([invA, invB])[None, :], (128, 1)).astype(np.float32)
    return c


def _prep_core(inp, c, NT, L):
    b, j = c // 4, c % 4
    SEQ = NT * 128
    NCMP = (SEQ - 32) // 16 + 1
    NCK = (NCMP + 127) // 128
    NCP = NCK * 128
    f = np.float32
    m = dict(_consts(NT, L, j))
    x = np.asarray(inp["x"])[b].reshape(NT, 128, 1024)
    own = [4 * l + j for l in range(L)]
    m["x"] = np.ascontiguousarray(x)
    m["xq"] = np.ascontiguousarray(x[own])
    pos = np.asarray(inp["positions"])[b].astype(np.int32)
    pt = pos.reshape(NT, 128).T
    m["pos"] = np.ascontiguousarray(pt)
    m["posq"] = np.ascontiguousarray(pt[:, own])
    pc = np.zeros(NCP, np.int32)
    pc[:NCMP] = pos[16 * np.arange(NCMP) + 31]
    m["posc"] = np.ascontiguousarray(pc.reshape(NCK, 128).T)
    w_in = np.asarray(inp["w_in"])[0]
    m["wkv"] = _kc(np.concatenate([w_in[:, 512:1280], w_in[:, 1560:1720]], 1))
    m["wq"] = _kc(np.concatenate([w_in[:, 0:512], w_in[:, 1280:1304], w_in[:, 1304:1560]], 1))
    m["g1"] = np.ascontiguousarray(np.asarray(inp["norm1_gain"])[0].reshape(8, 128).T)
    vec = np.zeros(576, f)
    off = 0
    for nm in ("nsa_q_gain", "nsa_kc_gain", "nsa_ks_gain", "nsa_kw_gain", "mla_kv_lora_gain", "mla_q_gain", "mla_k_gain"):
        v = np.asarray(inp[nm])[0]
        vec[off:off + v.size] = v
        off += v.size
    m["vecs"] = np.tile(vec[None, :], (128, 1))
    cp = np.asarray(inp["cmp_pos"])[0]
    m["cposT"] = np.ascontiguousarray(np.concatenate([cp.T, cp.T], 0))
    for nm, src in (("w1k", "cmp_k_w1"), ("w1v", "cmp_v_w1")):
        w = np.asarray(inp[src])[0].reshape(32, 64, 256).transpose(1, 0, 2)
        m[nm] = np.ascontiguousarray(np.concatenate([w, w], 0))
    for nm, src in (("w2k", "cmp_k_w2"), ("w2v", "cmp_v_w2")):
        m[nm] = _kc(np.asarray(inp[src])[0])
    m["wuq"] = _kc(np.asarray(inp["mla_w_uq"])[0])
    m["gql"] = np.ascontiguousarray(np.asarray(inp["mla_q_lora_gain"])[0].reshape(2, 128).T)
    wukv = np.asarray(inp["mla_w_ukv"])[0].reshape(128, 8, 128)
    knope, vpart = wukv[:, :, 0:64], wukv[:, :, 64:128]
    m["wukall"] = np.ascontiguousarray(knope.reshape(128, 512))
    wt = np.zeros((128, 8, 128), f)
    for h in range(8):
        wt[(h % 2) * 64:(h % 2) * 64 + 64, h, :] = knope[:, h, :].T
    m["wukT"] = wt
    m["wuv"] = np.ascontiguousarray(vpart.reshape(128, 512))
    m["wout"] = _kc(np.asarray(inp["w_out"])[0])
    m["gout"] = np.ascontiguousarray(np.concatenate([np.asarray(inp["out_gain_a"])[0], np.asarray(inp["out_gain_b"])[0]]).reshape(8, 128).T)
    m["wpq"] = _kc(np.asarray(inp["peer_w_q"])[0])
    sk = np.asarray(inp["peer_sub_keys"])[0].reshape(16, 128, 128)
    m["skT"] = np.ascontiguousarray(sk.transpose(2, 0, 1))
    m["n2g"] = np.tile(np.asarray(inp["norm2_gain"])[0][None, :], (128, 1))
    m["peer_u"] = np.asarray(inp["peer_u"])[0]
    m["peer_v"] = np.asarray(inp["peer_v"])[0]
    return {k: np.ascontiguousarray(v) for k, v in m.items()}


_CACHE = {}


def run(inputs, stage=3, dbg=False, cut=99, peer_rows=16384):
    x = np.asarray(inputs["x"])
    B, SEQ, _ = x.shape
    NT = SEQ // 128
    L = NT // 4
    key = (NT, L, stage, dbg, cut, peer_rows)
    if key not in _CACHE:
        _CACHE[key] = build(NT, L, stage=stage, dbg=dbg, cut=cut, peer_rows=peer_rows)
    nc, dbg_names = _CACHE[key]
    in_maps = [_prep_core(inputs, c, NT, L) for c in range(8)]
    if peer_rows != 16384:
        for m in in_maps:
            m["peer_u"] = m["peer_u"][:peer_rows]
            m["peer_v"] = m["peer_v"][:peer_rows]
    import os
    ncr = int(os.environ.get("NCORES", "8"))
    res = run_bass_kernel_spmd(nc, in_maps[:ncr], core_ids=list(range(ncr)))
    return res, NT, L


def kernel(**inputs):
    res, NT, L = run(inputs, stage=3, dbg=False)
    x = np.asarray(inputs["x"])
    out = np.zeros(x.shape, np.float32)
    for c in range(8):
        b, j = c // 4, c % 4
        o = res.results[c]["out"]
        for l in range(L):
            tt = 4 * l + j
            out[b, tt * 128:(tt + 1) * 128, :] = o[l]
    return out
```

```python
import math
import types
from contextlib import ExitStack
import numpy as np
import ml_dtypes
import concourse.bass as bass
import concourse.mybir as mybir
from concourse.bass_utils import run_bass_kernel_spmd

F32 = mybir.dt.float32
BF16 = mybir.dt.bfloat16
I32 = mybir.dt.int32
U32 = mybir.dt.uint32
AF = mybir.ActivationFunctionType
ALU = mybir.AluOpType
AX = mybir.AxisListType

EPS = 1e-6
NEGM = -30000.0
TWO_PI = 2.0 * math.pi
CW1 = 6.28125
CW2 = TWO_PI - CW1


class _Op:
    __slots__ = ("eng", "fn", "deps", "signal", "signo", "is_dma", "semkey", "dmaval")

    def __init__(self, eng, fn, is_dma=False, semkey=None):
        self.eng = eng
        self.fn = fn
        self.deps = []
        self.signal = False
        self.signo = None
        self.is_dma = is_dma
        self.semkey = semkey
        self.dmaval = None


def _freeze(fn):
    if fn.__closure__ is None:
        return fn
    cells = []
    for c in fn.__closure__:
        try:
            cells.append(types.CellType(c.cell_contents))
        except ValueError:
            cells.append(c)
    return types.FunctionType(fn.__code__, fn.__globals__, fn.__name__, fn.__defaults__, tuple(cells))


class Sched:
    ENGS = ("pe", "act", "dve", "pool", "sp")

    def __init__(self, nc):
        self.nc = nc
        self.streams = {e: [] for e in self.ENGS}
        self.last_w = {}
        self.readers = {}
        self.dma_cnt = {}
        self.last_dma = {}
        self.bar_deps = []
        self.need_bar = {e: False for e in self.ENGS}

    def barrier(self):
        lasts = []
        for e in self.ENGS:
            for o in reversed(self.streams[e]):
                if not o.is_dma:
                    lasts.append(o)
                    break
        lasts += list(self.last_dma.values())
        self.bar_deps = lasts
        self.need_bar = {e: True for e in self.ENGS}

    def op(self, eng, fn, reads=(), writes=(), dma=False, semkey=None):
        o = _Op(eng, _freeze(fn), is_dma=dma, semkey=semkey)
        if dma:
            c = self.dma_cnt.get(semkey, 0) + 1
            self.dma_cnt[semkey] = c
            o.dmaval = 16 * c
            self.last_dma[semkey] = o
        deps = o.deps
        if self.need_bar[eng]:
            self.need_bar[eng] = False
            for d in self.bar_deps:
                if d.is_dma or d.eng != eng:
                    deps.append(d)
        for r in reads:
            w = self.last_w.get(r)
            if w is not None and w not in deps:
                if not (w.eng == eng and eng == "pe" and not w.is_dma and not dma):
                    deps.append(w)
        for r in writes:
            w = self.last_w.get(r)
            if w is not None and w not in deps:
                if w.is_dma or dma or w.eng != eng or eng != "pe":
                    deps.append(w)
            for rd in self.readers.get(r, ()):
                if (rd.is_dma or dma or rd.eng != eng or eng != "pe") and rd not in deps and rd is not o:
                    deps.append(rd)
        for r in reads:
            self.readers.setdefault(r, []).append(o)
        for r in writes:
            self.last_w[r] = o
            self.readers[r] = []
        self.streams[eng].append(o)
        return o

    def dma(self, eng, fn, reads=(), writes=(), semkey=None):
        if semkey is None:
            semkey = writes[0]
        return self.op(eng, fn, reads, writes, dma=True, semkey=semkey)

    def emit(self, final_wait_keys=()):
        nc = self.nc
        for e in self.ENGS:
            for o in self.streams[e]:
                for d in o.deps:
                    if not d.is_dma:
                        d.signal = True
        for e in self.ENGS:
            n = 0
            for o in self.streams[e]:
                if o.signal and not o.is_dma:
                    n += 1
                    o.signo = n
        sems = {}
        stack = []

        def getsem(key):
            if key not in sems:
                cm = nc.semaphore("s%d" % len(sems))
                sems[key] = cm.__enter__()
                stack.append(cm)
            return sems[key]

        for e in self.ENGS:
            getsem(("eng", e))
        for k in self.dma_cnt:
            getsem(("dma", k))
        streams = self.streams
        final_keys = list(final_wait_keys)
        dma_cnt = self.dma_cnt

        with nc.Block() as block:
            def make(ename):
                def body(eng):
                    waited = {}
                    for o in streams[ename]:
                        for d in o.deps:
                            if d.is_dma:
                                key = ("dma", d.semkey)
                                val = d.dmaval
                            else:
                                key = ("eng", d.eng)
                                val = d.signo
                            if waited.get(key, 0) >= val:
                                continue
                            waited[key] = val
                            eng.wait_ge(sems[key], val)
                        ins = o.fn(eng)
                        if o.is_dma:
                            ins.then_inc(sems[("dma", o.semkey)], 16)
                        elif o.signal:
                            ins.then_inc(sems[("eng", ename)], 1)
                    if ename == "sp":
                        for k in final_keys:
                            if k not in dma_cnt:
                                continue
                            eng.wait_ge(sems[("dma", k)], 16 * dma_cnt[k])
                return body

            block.sync(make("sp"))
            block.tensor(make("pe"))
            block.scalar(make("act"))
            block.vector(make("dve"))
            block.gpsimd(make("pool"))
        for cm in reversed(stack):
            cm.__exit__(None, None, None)
        print("sems used:", len(sems), "ops:", {e: len(streams[e]) for e in self.ENGS})


KV_COLS = 928
Q_COLS = 792


def build(NT, L, stage=3, dbg=False, cut=99, peer_rows=16384):
    SEQ = NT * 128
    NCMP = (SEQ - 32) // 16 + 1
    NCK = (NCMP + 127) // 128
    NCP = NCK * 128
    NSLC = SEQ // 64
    assert NSLC <= 128 and NSLC >= 16
    nc = bass.Bass("TRN2", target_bir_lowering=False)
    D = {}

    def din(name, shape, dt=F32):
        D[name] = nc.dram_tensor(name, list(shape), dt, kind="ExternalInput").ap()
        return D[name]

    x_d = din("x", [NT, 128, 1024])
    xq_d = din("xq", [L, 128, 1024])
    pos_d = din("pos", [128, NT], I32)
    posq_d = din("posq", [128, L], I32)
    posc_d = din("posc", [128, NCK], I32)
    wkv_d = din("wkv", [128, 8, KV_COLS])
    wq_d = din("wq", [128, 8, Q_COLS])
    g1_d = din("g1", [128, 8])
    vec_d = din("vecs", [128, 576])
    invf_d = din("invf", [128, 24])
    cposT_d = din("cposT", [128, 32])
    w1k_d = din("w1k", [128, 32, 256])
    w1v_d = din("w1v", [128, 32, 256])
    w2k_d = din("w2k", [128, 2, 64])
    w2v_d = din("w2v", [128, 2, 64])
    wuq_d = din("wuq", [128, 2, 768])
    gql_d = din("gql", [128, 2])
    wukall_d = din("wukall", [128, 512])
    wukT_d = din("wukT", [128, 8, 128])
    wuv_d = din("wuv", [128, 512])
    wout_d = din("wout", [128, 8, 1024])
    gout_d = din("gout", [128, 8])
    ident_d = din("ident", [128, 128])
    e64_d = din("e64", [128, 2, 2048])
    ovl_d = din("ovl", [128, NCK, 128])
    cmask_d = din("cmask", [L, 128, NCP])
    pbias_d = din("pbias", [L, 128, 128])
    dmask_d = din("dmask", [128, 4, 128])
    wmask_d = din("wmask", [128, 8, 128])
    wpq_d = din("wpq", [128, 8, 2048])
    skT_d = din("skT", [128, 16, 128])
    iota_d = din("iota16", [128, 16])
    pu_d = din("peer_u", [peer_rows, 1024])
    pv_d = din("peer_v", [peer_rows, 1024])
    pub_d = nc.dram_tensor("pu_b16", [peer_rows, 1024], BF16, kind="Internal").ap()
    pvb_d = nc.dram_tensor("pv_b16", [peer_rows, 1024], BF16, kind="Internal").ap()
    out_d = nc.dram_tensor("out", [L, 128, 1024], F32, kind="ExternalOutput").ap()
    hs_d = nc.dram_tensor("hs", [L, 128, 1024], F32, kind="Internal").ap()
    dbg_outs = {}

    def dout(name, shape, dt=F32):
        dbg_outs[name] = nc.dram_tensor(name, list(shape), dt, kind="ExternalOutput").ap()
        return dbg_outs[name]

    VOFF = {}
    off = 0
    for nm, n in (("qa", 64), ("kc", 64), ("ks", 64), ("kw", 64), ("kvl", 128), ("mq", 96), ("mk", 96)):
        VOFF[nm] = (off, n)
        off += n
    n2_d = din("n2g", [128, 1024])

    S = Sched(nc)
    SCALE_A = 64 ** -0.5
    SCALE_B = 96 ** -0.5

    with ExitStack() as top:
        uniq = [0]

        def sb(name, shape, dt=F32, st=top):
            uniq[0] += 1
            return st.enter_context(nc.sbuf_tensor("sb%d_%s" % (uniq[0], name), list(shape), dt))

        def ps(name, shape, dt=F32, st=top):
            uniq[0] += 1
            return st.enter_context(nc.psum_tensor("ps%d_%s" % (uniq[0], name), list(shape), dt))

        ident = sb("ident", [128, 128])
        identb = sb("identb", [128, 128], BF16)
        vecs = sb("vecs", [128, 576])
        invf = sb("invf", [128, 24])
        S.dma("sp", lambda e: e.dma_start(out=ident[:], in_=ident_d), writes=["ident"])
        S.dma("sp", lambda e: e.dma_start(out=vecs[:], in_=vec_d), writes=["vecs"])
        S.dma("sp", lambda e: e.dma_start(out=invf[:], in_=invf_d), writes=["invf"])
        S.op("dve", lambda e: e.tensor_copy(out=identb[:], in_=ident[:]), reads=["ident"], writes=["identb"])

        def gv(nm):
            o, n = VOFF[nm]
            return vecs[:, o:o + n]

        def sincos(st, pos_dram, ncol, F, foff, tag, res_pair):
            pi_ = sb("pi_" + tag, [128, ncol], I32, st)
            pf = sb("pf_" + tag, [128, ncol], F32, st)
            ang = sb("ang_" + tag, [128, ncol, F], F32, st)
            ki = sb("ki_" + tag, [128, ncol, F], I32, st)
            kf = sb("kf_" + tag, [128, ncol, F], F32, st)
            r = sb("r_" + tag, [128, ncol, F], F32, st)
            S.dma("sp", lambda e: e.dma_start(out=pi_[:], in_=pos_dram), writes=["pi_" + tag])
            S.op("dve", lambda e: e.tensor_copy(out=pf[:], in_=pi_[:]), reads=["pi_" + tag], writes=["pf_" + tag])
            S.op("dve", lambda e: e.tensor_tensor(out=ang[:], in0=pf[:].unsqueeze(2).to_broadcast([128, ncol, F]),
                                                  in1=invf[:, foff:foff + F].unsqueeze(1).to_broadcast([128, ncol, F]), op=ALU.mult),
                 reads=["pf_" + tag, "invf"], writes=["ang_" + tag])
            outs = []
            for which, shift in (("c", math.pi / 2), ("s", 0.0)):
                res = res_pair[0 if which == "c" else 1]
                rn = "r_" + tag
                if shift != 0.0:
                    S.op("dve", lambda e: e.tensor_scalar(out=r[:], in0=ang[:], scalar1=shift, scalar2=None, op0=ALU.add),
                         reads=["ang_" + tag], writes=[rn])
                    src, srcn = r, rn
                else:
                    src, srcn = ang, "ang_" + tag
                S.op("dve", lambda e, src=src: e.tensor_scalar(out=ki[:], in0=src[:], scalar1=1.0 / TWO_PI, scalar2=None, op0=ALU.mult),
                     reads=[srcn], writes=["ki_" + tag])
                S.op("dve", lambda e: e.tensor_copy(out=kf[:], in_=ki[:]), reads=["ki_" + tag], writes=["kf_" + tag])
                S.op("dve", lambda e, src=src: e.scalar_tensor_tensor(out=r[:], in0=kf[:], scalar=-CW1, in1=src[:], op0=ALU.mult, op1=ALU.add),
                     reads=["kf_" + tag, srcn], writes=[rn])
                S.op("dve", lambda e: e.scalar_tensor_tensor(out=r[:], in0=kf[:], scalar=-CW2, in1=r[:], op0=ALU.mult, op1=ALU.add),
                     reads=["kf_" + tag, rn], writes=[rn])
                S.op("dve", lambda e: e.tensor_scalar(out=r[:], in0=r[:], scalar1=math.pi, scalar2=-math.pi, op0=ALU.min, op1=ALU.max),
                     reads=[rn], writes=[rn])
                S.op("act", lambda e, res=res: e.activation(out=res[:], in_=r[:], func=AF.Sin), reads=[rn], writes=["%s_%s" % (which, tag)])
                outs.append((res, "%s_%s" % (which, tag)))
            return outs

        rp = {}
        for tag, ncol, F in (("QA", L, 8), ("QB", L, 16)):
            rp[tag] = (sb("c_" + tag, [128, ncol, F]), sb("s_" + tag, [128, ncol, F]))
        kvst = ExitStack()

        def rope(eng_name, xv, cosv, sinv, outv, tmpv, rd, wr, half):
            G = xv.shape[1]
            tn = wr[0] + "_ropetmp"
            if G == 1:
                x1 = xv[:, 0, 0:half]
                x2 = xv[:, 0, half:2 * half]
                t0, t1, t2, t3 = (tmpv[:, i, 0, :] for i in range(4))
                S.op("dve", lambda e: e.tensor_tensor(out=t0, in0=x1, in1=cosv, op=ALU.mult), reads=rd, writes=[tn + "0"])
                S.op("dve", lambda e: e.tensor_tensor(out=t1, in0=x2, in1=sinv, op=ALU.mult), reads=rd, writes=[tn + "1"])
                S.op("dve", lambda e: e.tensor_tensor(out=t2, in0=x2, in1=cosv, op=ALU.mult), reads=rd, writes=[tn + "2"])
                S.op("dve", lambda e: e.tensor_tensor(out=t3, in0=x1, in1=sinv, op=ALU.mult), reads=rd, writes=[tn + "3"])
                S.op("dve", lambda e: e.tensor_tensor(out=outv[:, 0, 0:half], in0=t0, in1=t1, op=ALU.subtract), reads=[tn + "0", tn + "1"], writes=wr)
                S.op("dve", lambda e: e.tensor_tensor(out=outv[:, 0, half:2 * half], in0=t2, in1=t3, op=ALU.add), reads=[tn + "2", tn + "3"], writes=wr)
                return
            cb = cosv.unsqueeze(1).to_broadcast([128, G, half])
            sbb = sinv.unsqueeze(1).to_broadcast([128, G, half])
            x1 = xv[:, :, 0:half]
            x2 = xv[:, :, half:2 * half]
            S.op("dve", lambda e: e.tensor_tensor(out=tmpv[:, 0], in0=x1, in1=cb, op=ALU.mult), reads=rd, writes=[tn + "0"])
            S.op("dve", lambda e: e.tensor_tensor(out=tmpv[:, 1], in0=x2, in1=sbb, op=ALU.mult), reads=rd, writes=[tn + "1"])
            S.op("dve", lambda e: e.tensor_tensor(out=tmpv[:, 2], in0=x2, in1=cb, op=ALU.mult), reads=rd, writes=[tn + "2"])
            S.op("dve", lambda e: e.tensor_tensor(out=tmpv[:, 3], in0=x1, in1=sbb, op=ALU.mult), reads=rd, writes=[tn + "3"])
            S.op("dve", lambda e: e.tensor_tensor(out=outv[:, :, 0:half], in0=tmpv[:, 0], in1=tmpv[:, 1], op=ALU.subtract),
                 reads=[tn + "0", tn + "1"], writes=wr)
            S.op("dve", lambda e: e.tensor_tensor(out=outv[:, :, half:2 * half], in0=tmpv[:, 2], in1=tmpv[:, 3], op=ALU.add),
                 reads=[tn + "2", tn + "3"], writes=wr)

        def load_w(st, dst, dstn, src_d, K, F, gain=None, gainn=None, part=None):
            stg = [sb("stg%s%d" % (dstn, i), [128, F], F32, st) for i in range(2)]
            for k in range(K):
                sg = stg[k % 2]
                sgn = "stg%s%d" % (dstn, k % 2)
                if part is None:
                    S.dma("sp", lambda e, sg=sg, k=k: e.dma_start(out=sg[:], in_=src_d[:, k, :]), writes=[sgn])
                else:
                    S.dma("sp", lambda e, sg=sg, k=k: e.dma_start(out=sg[0:part], in_=src_d[0:part, k, :]), writes=[sgn])
                if gain is not None:
                    S.op("dve", lambda e, sg=sg, k=k: e.tensor_scalar(out=dst[:, k, :], in0=sg[:], scalar1=gain[:, k:k + 1], scalar2=None, op0=ALU.mult),
                         reads=[sgn, gainn], writes=[dstn])
                else:
                    pp = 128 if part is None else part
                    S.op("dve", lambda e, sg=sg, k=k, pp=pp: e.tensor_copy(out=dst[0:pp, k, :], in_=sg[0:pp]), reads=[sgn], writes=[dstn])

        KsT = sb("KsT", [128, SEQ], BF16, kvst)
        Vs = sb("Vs", [128, NT, 2, 65], BF16, kvst)
        ckvT = sb("ckvT", [128, SEQ], BF16, kvst)
        krT = sb("krT", [128, SEQ], BF16, kvst)
        ckva = sb("ckva", [128, NT, 129], BF16, kvst)
        srh = sb("srh", [128, NT, 8], F32, kvst)
        KcT = sb("KcT", [128, NCP], BF16, kvst)
        Vc = sb("Vc", [128, NCK, 2, 64], BF16, kvst)
        kw_d = nc.dram_tensor("kw_scr", [128, SEQ], BF16, kind="Internal").ap()
        vw_d = nc.dram_tensor("vw_scr", [128, NT, 130], BF16, kind="Internal").ap()
        S.op("pool", lambda e: e.memset(Vs[:], 1.0), writes=["Vs"])
        S.op("pool", lambda e: e.memset(ckva[:], 1.0), writes=["ckva"])
        S.op("pool", lambda e: e.memset(KcT[:], 0.0), writes=["KcT"])
        S.op("pool", lambda e: e.memset(krT[:], 0.0), writes=["krT"])
        S.op("pool", lambda e: e.memset(Vc[:], 0.0), writes=["Vc"])

        def rstd_from_ss(ss_ap, ssn, out_ap, outn, n_elems, tmp_ap, tmpn):
            S.op("dve", lambda e: e.tensor_scalar(out=tmp_ap, in0=ss_ap, scalar1=1.0 / n_elems, scalar2=EPS, op0=ALU.mult, op1=ALU.add),
                 reads=[ssn], writes=[tmpn])
            S.op("act", lambda e: e.activation(out=tmp_ap, in_=tmp_ap, func=AF.Sqrt), reads=[tmpn], writes=[tmpn])
            S.op("dve", lambda e: e.reciprocal(out=out_ap, in_=tmp_ap), reads=[tmpn], writes=[outn])

        def xnorm_T(xt, xtn, junk, junkn, st_small, sfx, xnb, xnbn, pT, pTn, xnT, xnTn):
            ss, rs, tm = st_small
            S.op("act", lambda e: e.activation(out=junk[:], in_=xt[:], func=AF.Square, accum_out=ss[:, 0:1]), reads=[xtn], writes=[junkn, "ss" + sfx])
            rstd_from_ss(ss[:, 0:1], "ss" + sfx, rs[:, 0:1], "rs" + sfx, 1024.0, tm[:, 0:1], "tm" + sfx)
            S.op("dve", lambda e: e.tensor_scalar(out=xnb[:], in0=xt[:], scalar1=rs[:, 0:1], scalar2=None, op0=ALU.mult),
                 reads=[xtn, "rs" + sfx], writes=[xnbn])
            for k in range(8):
                S.op("pe", lambda e, k=k: e.transpose(out=pT[:, k * 128:(k + 1) * 128], in_=xnb[:, k * 128:(k + 1) * 128], identity=identb[:]),
                     reads=[xnbn, "identb"], writes=[pTn])
            S.op("act", lambda e: e.copy(out=xnT[:].rearrange("p k t -> p (k t)"), in_=pT[:, 0:1024]), reads=[pTn], writes=[xnTn])

        with ExitStack() as p1:
            for tag, ncol, F in (("A", NT, 8), ("B", NT, 16), ("C", NCK, 8)):
                rp[tag] = (sb("c_" + tag, [128, ncol, F], F32, p1), sb("s_" + tag, [128, ncol, F], F32, p1))
            with ExitStack() as tmp0:
                (cosA, cosA_n), (sinA, sinA_n) = sincos(tmp0, pos_d, NT, 8, 0, "A", rp["A"])
                (cosB, cosB_n), (sinB, sinB_n) = sincos(tmp0, pos_d, NT, 16, 8, "B", rp["B"])
                (cosC, cosC_n), (sinC, sinC_n) = sincos(tmp0, posc_d, NCK, 8, 0, "C", rp["C"])
                (cosQA, cosQA_n), (sinQA, sinQA_n) = sincos(tmp0, posq_d, L, 8, 0, "QA", rp["QA"])
                (cosQB, cosQB_n), (sinQB, sinQB_n) = sincos(tmp0, posq_d, L, 16, 8, "QB", rp["QB"])
                S.barrier()
            KcrT = sb("KcrT", [128, SEQ], BF16, p1)
            VcrT = sb("VcrT", [128, SEQ], BF16, p1)
            if stage in (3, 4):
                cvf = [sb("cvf%d" % i, [128, 1024], F32, p1) for i in range(3)]
                cvb = [sb("cvb%d" % i, [128, 1024], BF16, p1) for i in range(2)]
                blocks = [(src, dst, k) for (src, dst) in ((pu_d, pub_d), (pv_d, pvb_d)) for k in range(peer_rows // 128)]

                def cv_in(i):
                    src, dst, k = blocks[i]
                    S.dma("pool", lambda e, src=src, k=k, i=i: e.dma_start(out=cvf[i % 3][:], in_=src[k * 128:(k + 1) * 128, :]), writes=["cvf%d" % (i % 3)])
                cv_in(0)
                cv_in(1)
                for i in range(len(blocks)):
                    src, dst, k = blocks[i]
                    S.op("pool", lambda e, i=i: e.tensor_copy(out=cvb[i % 2][:], in_=cvf[i % 3][:]), reads=["cvf%d" % (i % 3)], writes=["cvb%d" % (i % 2)])
                    S.dma("pool", lambda e, dst=dst, k=k, i=i: e.dma_start(out=dst[k * 128:(k + 1) * 128, :], in_=cvb[i % 2][:]), reads=["cvb%d" % (i % 2)], writes=["pb16"], semkey="cvo%d" % (i % 2))
                    if i + 2 < len(blocks):
                        cv_in(i + 2)
                S.barrier()
            with ExitStack() as p1a:
                wkv = sb("wkv", [128, 8, KV_COLS], BF16, p1a)
                g1 = sb("g1", [128, 8], F32, p1a)
                wukall = sb("wukall", [128, 512], BF16, p1a)
                S.dma("sp", lambda e: e.dma_start(out=g1[:], in_=g1_d), writes=["g1"])
                with ExitStack() as ld:
                    load_w(ld, wkv, "wkv", wkv_d, 8, KV_COLS, gain=g1, gainn="g1")
                    stg = sb("stg_wuk", [128, 512], F32, ld)
                    S.dma("sp", lambda e: e.dma_start(out=stg[:], in_=wukall_d), writes=["stg_wuk"])
                    S.op("dve", lambda e: e.tensor_copy(out=wukall[:], in_=stg[:]), reads=["stg_wuk"], writes=["wukall"])
                    S.barrier()
                xts = [sb("xt%d" % i, [128, 1024], F32, p1a) for i in range(2)]
                junk = sb("junk", [128, 1024], BF16, p1a)
                xnb = sb("xnb", [128, 1024], BF16, p1a)
                xnTs = [sb("xnT%d" % i, [128, 8, 128], BF16, p1a) for i in range(2)]
                small = [(sb("ss%d" % i, [128, 8], F32, p1a), sb("rs%d" % i, [128, 8], F32, p1a), sb("tm%d" % i, [128, 8], F32, p1a)) for i in range(2)]
                kn4 = sb("kn4", [128, 4, 64], F32, p1a)
                rtmp = sb("rtmp", [128, 4, 4, 16], F32, p1a)
                kwst = [sb("kwst%d" % i, [128, 128], BF16, p1a) for i in range(2)]
                vwst = [sb("vwst%d" % i, [128, 2, 65], BF16, p1a) for i in range(2)]
                for i_ in range(2):
                    S.op("dve", lambda e, i_=i_: e.memset(vwst[i_][:], 1.0), writes=["vwst%d" % i_])
                kb = sb("kb", [128, 7, 128], BF16, p1a)
                ckvn = sb("ckvn", [128, 128], F32, p1a)
                krg = sb("krg", [128, 1, 32], F32, p1a)
                sqj = sb("sqj", [128, 512], F32, p1a)
                ssn8 = sb("ssn8", [128, 8], F32, p1a)
                pTs = [ps("pT%d" % i, [128, 1024], BF16, p1a) for i in range(2)]
                pPs = [ps("pP%d" % i, [128, 1024], F32, p1a) for i in range(2)]
                pK = ps("pK", [128, 512], F32, p1a)
                pT2 = ps("pT2", [128, 1024], BF16, p1a)

                for tt in range(NT if cut > 1 else 0):
                    b = tt % 2
                    xt, xtn = xts[b], "xt%d" % b
                    ss, rs, tm = small[b]
                    sfx = str(b)
                    pP, pPn = pPs[b], "pP%d" % b
                    xnT, xnTn = xnTs[b], "xnT%d" % b
                    S.dma("sp", lambda e, xt=xt, tt=tt: e.dma_start(out=xt[:], in_=x_d[tt]), writes=[xtn])
                    xnorm_T(xt, xtn, junk, "junk", small[b], sfx, xnb, "xnb", pTs[b], "pT%d" % b, xnT, xnTn)
                    for half, (c0, c1) in enumerate(((0, 512), (512, KV_COLS))):
                        for k in range(8):
                            S.op("pe", lambda e, k=k, c0=c0, c1=c1, half=half, pP=pP, xnT=xnT: e.matmul(
                                pP[:, half * 512: half * 512 + (c1 - c0)], lhsT=xnT[:, k, :], rhs=wkv[:, k, c0:c1], start=(k == 0), stop=(k == 7)),
                                reads=[xnTn, "wkv"], writes=[pPn])
                    if cut < 3:
                        continue
                    S.op("act", lambda e, pP=pP: e.copy(out=kb[:, 2:4, :].rearrange("p a b -> p (a b)"), in_=pP[:, 0:256]), reads=[pPn], writes=["kb23"])
                    S.op("act", lambda e, pP=pP, tt=tt: e.copy(out=Vs[:, tt, :, 0:64], in_=pP[:, 384:512].rearrange("p (g d) -> p g d", g=2)),
                         reads=[pPn], writes=["Vs"])
                    S.op("act", lambda e, pP=pP, b=b: e.copy(out=vwst[b][:, :, 0:64], in_=pP[:, 640:768].rearrange("p (g d) -> p g d", g=2)),
                         reads=[pPn], writes=["vwst%d" % b])
                    S.dma("sp", lambda e, tt=tt, b=b: e.dma_start(out=vw_d[:, tt, :], in_=vwst[b][:].rearrange("p g d -> p (g d)")), reads=["vwst%d" % b], writes=["vw_scr"], semkey="vwd%d" % b)
                    if cut < 3.2:
                        continue
                    for i, c0 in enumerate((256, 320, 512, 576)):
                        S.op("act", lambda e, pP=pP, c0=c0, i=i, ss=ss: e.activation(out=sqj[:, 0:64], in_=pP[:, c0:c0 + 64], func=AF.Square, accum_out=ss[:, 1 + i:2 + i]),
                             reads=[pPn], writes=["sqj", "ss4" + sfx])
                    rstd_from_ss(ss[:, 1:5], "ss4" + sfx, rs[:, 1:5], "rs4" + sfx, 64.0, tm[:, 1:5], "tm4" + sfx)
                    for i, c0 in enumerate((256, 320, 512, 576)):
                        gn = gv("ks") if i < 2 else gv("kw")
                        S.op("dve", lambda e, pP=pP, c0=c0, i=i, rs=rs, gn=gn: e.scalar_tensor_tensor(
                            out=kn4[:, i, :], in0=pP[:, c0:c0 + 64], scalar=rs[:, 1 + i:2 + i], in1=gn, op0=ALU.mult, op1=ALU.mult),
                            reads=[pPn, "rs4" + sfx, "vecs"], writes=["kn4"])
                    if cut < 3.4:
                        continue
                    kb01 = kb[:, 0:2, :].rearrange("p a (g d) -> p (a g) d", g=2)
                    S.op("dve", lambda e: e.tensor_copy(out=kb01[:, :, 16:64], in_=kn4[:, :, 16:64]), reads=["kn4"], writes=["kb01"])
                    rope("dve", kn4[:], cosA[:, tt, :], sinA[:, tt, :], kb01, rtmp[:, :, :, 0:8], ["kn4", cosA_n, sinA_n], ["kb01"], 8)
                    if cut < 3.6:
                        continue
                    S.op("act", lambda e, pP=pP, ss=ss: e.activation(out=sqj[:, 0:128], in_=pP[:, 768:896], func=AF.Square, accum_out=ss[:, 5:6]),
                         reads=[pPn], writes=["sqj", "ss5" + sfx])
                    rstd_from_ss(ss[:, 5:6], "ss5" + sfx, rs[:, 5:6], "rs5" + sfx, 128.0, tm[:, 5:6], "tm5" + sfx)
                    S.op("dve", lambda e, pP=pP, rs=rs: e.scalar_tensor_tensor(out=ckvn[:], in0=pP[:, 768:896], scalar=rs[:, 5:6], in1=gv("kvl"), op0=ALU.mult, op1=ALU.mult),
                         reads=[pPn, "rs5" + sfx, "vecs"], writes=["ckvn"])
                    S.op("dve", lambda e: e.tensor_copy(out=kb[:, 4, :], in_=ckvn[:]), reads=["ckvn"], writes=["kb4"])
                    S.op("act", lambda e, tt=tt: e.copy(out=ckva[:, tt, 0:128], in_=ckvn[:]), reads=["ckvn"], writes=["ckva"])
                    if cut < 3.8:
                        continue
                    S.op("act", lambda e, pP=pP, ss=ss: e.activation(out=sqj[:, 0:32], in_=pP[:, 896:928], func=AF.Square, accum_out=ss[:, 6:7]),
                         reads=[pPn], writes=["sqj", "ss6" + sfx])
                    mk_o = VOFF["mk"][0]
                    S.op("dve", lambda e, pP=pP: e.tensor_tensor(out=krg[:, 0, :], in0=pP[:, 896:928], in1=vecs[:, mk_o + 64:mk_o + 96], op=ALU.mult),
                         reads=[pPn, "vecs"], writes=["krg"])
                    if cut < 3.9:
                        continue
                    kb5 = kb[:, 5, 0:32].unsqueeze(1)
                    rope("dve", krg[:], cosB[:, tt, :], sinB[:, tt, :], kb5, rtmp[:, :, 0:1, 0:16], ["krg", cosB_n, sinB_n], ["kb5"], 16)
                    if cut < 4.1:
                        continue
                    for i in range(5):
                        S.op("pe", lambda e, i=i: e.transpose(out=pT2[:, i * 128:(i + 1) * 128], in_=kb[:, i, :], identity=identb[:]),
                             reads=["kb01", "kb23", "kb4", "identb"], writes=["pT2"])
                    S.op("pe", lambda e: e.transpose(out=pT2[0:32, 640:768], in_=kb[:, 5, 0:32], identity=identb[:]),
                         reads=["kb5", "identb"], writes=["pT2"])
                    if cut < 4.2:
                        continue
                    tsl = slice(tt * 128, (tt + 1) * 128)
                    S.op("act", lambda e, tsl=tsl: e.copy(out=KsT[:, tsl], in_=pT2[:, 0:128]), reads=["pT2"], writes=["KsT"])
                    S.op("act", lambda e, b=b: e.copy(out=kwst[b][:], in_=pT2[:, 128:256]), reads=["pT2"], writes=["kwst%d" % b])
                    S.dma("sp", lambda e, tsl=tsl, b=b: e.dma_start(out=kw_d[:, tsl], in_=kwst[b][:]), reads=["kwst%d" % b], writes=["kw_scr"], semkey="kwd%d" % b)
                    S.op("act", lambda e, tsl=tsl: e.copy(out=KcrT[:, tsl], in_=pT2[:, 256:384]), reads=["pT2"], writes=["KcrT"])
                    S.op("act", lambda e, tsl=tsl: e.copy(out=VcrT[:, tsl], in_=pT2[:, 384:512]), reads=["pT2"], writes=["VcrT"])
                    S.op("act", lambda e, tsl=tsl: e.copy(out=ckvT[:, tsl], in_=pT2[:, 512:640]), reads=["pT2"], writes=["ckvT"])
                    if cut < 4.3:
                        continue
                    S.op("act", lambda e, tsl=tsl: e.copy(out=krT[0:32, tsl], in_=pT2[0:32, 640:768]), reads=["pT2"], writes=["krT"])
                    if cut < 4.4:
                        continue
                    S.op("pe", lambda e, tsl=tsl: e.matmul(pK[:], lhsT=ckvT[:, tsl], rhs=wukall[:], start=True, stop=True),
                         reads=["ckvT", "wukall"], writes=["pK"])
                    S.op("act", lambda e: e.activation(out=sqj[:], in_=pK[:], func=AF.Square), reads=["pK"], writes=["sqj"])
                    S.op("dve", lambda e: e.tensor_reduce(out=ssn8[:], in_=sqj[:].rearrange("p (h d) -> p h d", h=8), axis=AX.X, op=ALU.add),
                         reads=["sqj"], writes=["ssn8"])
                    S.op("dve", lambda e, ss=ss: e.tensor_scalar(out=ssn8[:], in0=ssn8[:], scalar1=ss[:, 6:7], scalar2=None, op0=ALU.add),
                         reads=["ssn8", "ss6" + sfx], writes=["ssn8"])
                    S.op("dve", lambda e: e.tensor_scalar(out=ssn8[:], in0=ssn8[:], scalar1=1.0 / 96.0, scalar2=EPS, op0=ALU.mult, op1=ALU.add),
                         reads=["ssn8"], writes=["ssn8"])
                    S.op("act", lambda e: e.activation(out=ssn8[:], in_=ssn8[:], func=AF.Sqrt), reads=["ssn8"], writes=["ssn8"])
                    S.op("dve", lambda e: e.reciprocal(out=ssn8[:], in_=ssn8[:]), reads=["ssn8"], writes=["ssn8"])
                    S.op("dve", lambda e, tt=tt: e.tensor_scalar(out=srh[:, tt, :], in0=ssn8[:], scalar1=SCALE_B, scalar2=None, op0=ALU.mult),
                         reads=["ssn8"], writes=["srh"])
                S.barrier()

            with ExitStack() as p1b:
                W1 = sb("W1", [128, 32, 256], BF16, p1b)
                W2 = sb("W2", [128, 2, 64], BF16, p1b)
                cposT = sb("cposT", [128, 32], F32, p1b)
                cposTb = sb("cposTb", [128, 32], BF16, p1b)
                c1 = sb("c1", [128, 2], F32, p1b)
                GH = sb("GH", [128, 2, NCP], BF16, p1b)
                kc4 = sb("kc4", [128, 2, 64], F32, p1b)
                kcb = sb("kcb", [128, 2, 64], BF16, p1b)
                rtmp2 = sb("rtmp2", [128, 4, 2, 8], F32, p1b)
                ssc = sb("ssc", [128, 2], F32, p1b)
                rsc = sb("rsc", [128, 2], F32, p1b)
                tmc = sb("tmc", [128, 2], F32, p1b)
                sqj2 = sb("sqj2", [128, 64], F32, p1b)
                pH = [ps("pH%d" % i, [128, 512], F32, p1b) for i in range(2)]
                pC = ps("pC", [128, 512], F32, p1b)
                pO = ps("pO1", [128, 512], F32, p1b)
                pTc = ps("pTc", [128, 1024], BF16, p1b)
                S.dma("sp", lambda e: e.dma_start(out=cposT[:], in_=cposT_d), writes=["cposT"])
                S.op("dve", lambda e: e.tensor_copy(out=cposTb[:], in_=cposT[:]), reads=["cposT"], writes=["cposTb"])
                S.op("dve", lambda e: e.memset(GH[:], 0.0), writes=["GH"])
                for which, (w1_d, w2_d, srcT, srcn) in enumerate(((w1k_d, w2k_d, KcrT, "KcrT"), (w1v_d, w2v_d, VcrT, "VcrT")) if cut > 5 else ()):
                    with ExitStack() as ld:
                        load_w(ld, W1, "W1", w1_d, 32, 256)
                        load_w(ld, W2, "W2", w2_d, 2, 64)
                        S.barrier()
                    for hh in range(2):
                        for l in range(32):
                            S.op("pe", lambda e, hh=hh, l=l: e.matmul(pC[:, hh:hh + 1], lhsT=W1[0:64, l, hh * 128:(hh + 1) * 128], rhs=cposTb[0:64, l:l + 1],
                                                                       start=(l == 0), stop=(l == 31)), reads=["W1", "cposTb"], writes=["pC"])
                    S.op("dve", lambda e: e.tensor_copy(out=c1[:], in_=pC[:, 0:2]), reads=["pC"], writes=["c1"])
                    for g in range(2):
                        for hh in range(2):
                            ph, phn = pH[hh], "pH%d" % hh
                            for l in range(32):
                                sv = srcT[64 * g:64 * g + 64, :].rearrange("p (n l) -> p n l", l=16)
                                rhs = sv[:, 0:NCMP, l] if l < 16 else sv[:, 1:NCMP + 1, l - 16]
                                S.op("pe", lambda e, l=l, hh=hh, g=g, rhs=rhs, ph=ph: e.matmul(ph[:, 0:NCMP], lhsT=W1[64 * g:64 * g + 64, l, hh * 128:(hh + 1) * 128], rhs=rhs,
                                                                                         start=(l == 0), stop=(l == 31)), reads=[srcn, "W1"], writes=[phn])
                            S.op("act", lambda e, hh=hh, ph=ph: e.activation(out=GH[:, hh, 0:NCMP], in_=ph[:, 0:NCMP], func=AF.Gelu_apprx_tanh, bias=c1[:, hh:hh + 1]),
                                 reads=[phn, "c1"], writes=["GH"])
                        for k in range(NCK):
                            for hh in range(2):
                                S.op("pe", lambda e, k=k, hh=hh, g=g: e.matmul(pO[:, (k * 2 + g) * 64:(k * 2 + g) * 64 + 64], lhsT=GH[:, hh, k * 128:(k + 1) * 128], rhs=W2[:, hh, :],
                                                                           start=(hh == 0), stop=(hh == 1)), reads=["GH", "W2"], writes=["pO1"])
                    for k in range(NCK):
                        pv = pO[:, k * 128:(k + 1) * 128].rearrange("p (g d) -> p g d", g=2)
                        if which == 1:
                            S.op("act", lambda e, k=k, pv=pv: e.copy(out=Vc[:, k, :, :], in_=pv), reads=["pO1"], writes=["Vc"])
                            continue
                        for g in range(2):
                            S.op("act", lambda e, g=g, pv=pv: e.activation(out=sqj2[:], in_=pv[:, g, :], func=AF.Square, accum_out=ssc[:, g:g + 1]),
                                 reads=["pO1"], writes=["sqj2", "ssc"])
                        rstd_from_ss(ssc[:], "ssc", rsc[:], "rsc", 64.0, tmc[:], "tmc")
                        for g in range(2):
                            S.op("dve", lambda e, g=g, pv=pv: e.scalar_tensor_tensor(out=kc4[:, g, :], in0=pv[:, g, :], scalar=rsc[:, g:g + 1], in1=gv("kc"), op0=ALU.mult, op1=ALU.mult),
                                 reads=["pO1", "rsc", "vecs"], writes=["kc4"])
                        S.op("dve", lambda e: e.tensor_copy(out=kcb[:, :, 16:64], in_=kc4[:, :, 16:64]), reads=["kc4"], writes=["kcb"])
                        rope("dve", kc4[:], cosC[:, k, :], sinC[:, k, :], kcb[:], rtmp2[:], ["kc4", cosC_n, sinC_n], ["kcb"], 8)
                        S.op("pe", lambda e: e.transpose(out=pTc[:, 0:128], in_=kcb[:].rearrange("p g d -> p (g d)"), identity=identb[:]), reads=["kcb", "identb"], writes=["pTc"])
                        nval = min(128, NCMP - k * 128)
                        S.op("act", lambda e, k=k, nval=nval: e.copy(out=KcT[:, k * 128:k * 128 + nval], in_=pTc[:, 0:nval]), reads=["pTc"], writes=["KcT"])
                S.barrier()

        if dbg and cut > 6:
            for nm, t, shp, dt in (("d_KsT", KsT, [128, SEQ], BF16), ("d_ckvT", ckvT, [128, SEQ], BF16),
                                   ("d_KcT", KcT, [128, NCP], BF16), ("d_srh", srh, [128, NT, 8], F32), ("d_Vc", Vc, [128, NCK, 2, 64], BF16),
                                   ("d_Vs", Vs, [128, NT, 2, 65], BF16), ("d_ckva", ckva, [128, NT, 129], BF16)):
                o = dout(nm, shp, dt)
                S.dma("sp", lambda e, o=o, t=t: e.dma_start(out=o, in_=t[:]), reads=[nm[2:]], writes=[nm])
            o = dout("d_krT", [32, SEQ], BF16)
            S.dma("sp", lambda e, o=o: e.dma_start(out=o, in_=krT[0:32, :]), reads=["krT"], writes=["d_krT"])

        PHASE2 = stage in (2, 3)
        PHASE3 = stage in (3, 4)
        if PHASE2:
            with ExitStack() as p2:
                wq = sb("wq", [128, 8, Q_COLS], BF16, p2)
                wuq = sb("wuq", [128, 2, 768], BF16, p2)
                wukT = sb("wukT", [128, 8, 128], BF16, p2)
                wuv = sb("wuv", [128, 512], BF16, p2)
                wout = sb("wout", [128, 8, 1024], BF16, p2)
                e64 = sb("e64", [128, 32 * 128], BF16, p2)
                ovl = sb("ovl", [128, NCK, 128], F32, p2)
                dmask = sb("dmask", [128, 4, 128], BF16, p2)
                wmask = sb("wmask", [128, 8, 128], BF16, p2)
                g1b = sb("g1b", [128, 8], F32, p2)
                gql = sb("gql", [128, 2], F32, p2)
                gout = sb("gout", [128, 8], F32, p2)
                for t_, d_, n_ in ((g1b, g1_d, "g1b"), (gql, gql_d, "gql"), (gout, gout_d, "gout"), (ovl, ovl_d, "ovl")):
                    S.dma("sp", lambda e, t_=t_, d_=d_: e.dma_start(out=t_[:], in_=d_), writes=[n_])
                with ExitStack() as ld:
                    load_w(ld, wq, "wq", wq_d, 8, Q_COLS, gain=g1b, gainn="g1b")
                    load_w(ld, e64[:].rearrange("p (k f) -> p k f", k=2), "e64", e64_d, 2, 2048)
                    load_w(ld, dmask, "dmask", dmask_d, 4, 128)
                    load_w(ld, wmask, "wmask", wmask_d, 8, 128)
                    load_w(ld, wuq, "wuq", wuq_d, 2, 768, gain=gql, gainn="gql")
                    load_w(ld, wukT, "wukT", wukT_d, 8, 128)
                    load_w(ld, wout, "wout", wout_d, 8, 1024, gain=gout, gainn="gout")
                    stg = sb("stg_wuv", [128, 512], F32, ld)
                    S.dma("sp", lambda e: e.dma_start(out=stg[:], in_=wuv_d), writes=["stg_wuv"])
                    S.op("dve", lambda e: e.tensor_copy(out=wuv[:], in_=stg[:]), reads=["stg_wuv"], writes=["wuv"])
                    S.barrier()
                xt = sb("xt", [128, 1024], F32, p2)
                xnb = sb("xnb", [128, 1024], BF16, p2)
                junk = xnb
                xnT = sb("xnT", [128, 8, 128], BF16, p2)
                small = (sb("ss", [128, 8], F32, p2), sb("rs", [128, 8], F32, p2), sb("tm", [128, 8], F32, p2))
                sqj = sb("sqj", [128, 768], F32, p2)
                s8 = sb("s8", [128, 8], F32, p2)
                r8 = sb("r8", [128, 8], F32, p2)
                t8 = sb("t8", [128, 8], F32, p2)
                qn = sb("qn", [128, 8, 64], F32, p2)
                qab = sb("qab", [128, 4, 2, 64], BF16, p2)
                rtmp = sb("rtmp", [128, 4, 8, 16], F32, p2)
                QaTz = [sb("QaTz%d" % i, [128, 4, 128], BF16, p2) for i in range(2)]
                gat = sb("gat", [128, 24], F32, p2)
                cqn = sb("cqn", [128, 256], BF16, p2)
                cqT = sb("cqT", [128, 2, 128], BF16, p2)
                qm = sb("qm", [128, 8, 96], F32, p2)
                qmn = sb("qmn", [128, 8, 64], BF16, p2)
                qmr = sb("qmr", [128, 8, 32], BF16, p2)
                qnT = sb("qnT", [128, 4, 128], BF16, p2)
                QlatT = sb("QlatT", [128, 8, 128], BF16, p2)
                QropeT = sb("QropeT", [128, 8, 128], BF16, p2)
                cmask = sb("cmask", [128, NCP], F32, p2)
                pbias = sb("pbias", [128, 128], F32, p2)
                Pc = sb("Pc", [128, 4, NCP], F32, p2)
                Pcb = sb("Pcb", [128, NCP], BF16, p2)
                Psm = sb("Psm", [128, NCP], F32, p2)
                PsT = sb("PsT", [128, NCK, 128], F32, p2)
                PcT = sb("PcT", [128, NCK, 128], BF16, p2)
                z4 = sb("z4", [128, 4], F32, p2)
                f4 = sb("f4", [128, 4], F32, p2)
                impb = sb("impb", [128, 128], F32, p2)
                wk = sb("wk", [128, 128], F32, p2)
                m8 = sb("m8", [128, 16], F32, p2)
                nsl = sb("nsl", [128, 128], F32, p2)
                nslb = sb("nslb", [128, 128], BF16, p2)
                nselZ = [sb("nselZ%d" % i, [128, 4, 128], BF16, p2) for i in range(2)]
                PTs = [sb("PT%d" % i, [128, 512], BF16, p2) for i in range(3)]
                KwTw = sb("KwTw", [128, 8 * 128], BF16, p2)
                Vww = sb("Vww", [128, 8, 2, 65], BF16, p2)
                oa = sb("oa", [128, 8, 64], F32, p2)
                olb = sb("olb", [128, 128], BF16, p2)
                OlatT = sb("OlatT", [128, 128], BF16, p2)
                onb = xnb
                onT = xnT
                ht = xt
                pT0 = ps("pT0", [128, 1024], BF16, p2)
                pT1 = ps("pT1", [128, 1024], BF16, p2)
                pP0 = ps("pP0", [128, 512], F32, p2)
                pP1 = ps("pP1", [128, 512], F32, p2)
                pSs = [ps("pS%d" % i, [128, 512], F32, p2) for i in range(2)]
                pO = ps("pO", [128, 512], F32, p2)
                pX = ps("pX", [128, 512], F32, p2)
                S.op("pool", lambda e: e.memset(nsl[:], NEGM), writes=["nsl"])
                S.op("pool", lambda e: e.memset(QropeT[:], 0.0), writes=["QropeT"])
                for i_ in range(2):
                    S.op("pool", lambda e, i_=i_: e.memset(QaTz[i_][:], 0.0), writes=["QaT"])
                    S.op("pool", lambda e, i_=i_: e.memset(nselZ[i_][:], 0.0), writes=["nselT4"])
                S.op("pool", lambda e: e.memset(impb[:], 0.0), writes=["impb"])
                mq_o = VOFF["mq"][0]
                mk_o = VOFF["mk"][0]
                scnt = [0]

                def attn_chunks(lhsK, lhsKn, rhsQ, rhsQn, N, extra, scale, scalen, pv_list, chunks, first_last):
                    nch = len(chunks)
                    st = []
                    for idx, c in enumerate(chunks):
                        sl = scnt[0] % 2
                        pl = scnt[0] % 3
                        scnt[0] += 1
                        st.append((c, pSs[sl], "pS%d" % sl, PTs[pl], "PT%d" % pl))

                    def qk(idx):
                        c, pSx, pSn, PT, PTn = st[idx]
                        ex = extra(c)
                        for kk, (lk, lkn, rq, rqn) in enumerate(zip(lhsK, lhsKn, rhsQ, rhsQn)):
                            last = (kk == len(lhsK) - 1) and not ex
                            S.op("pe", lambda e, lk=lk, rq=rq, c=c, kk=kk, last=last, pSx=pSx: e.matmul(pSx[:, 0:N], lhsT=lk[:, c * 128:(c + 1) * 128], rhs=rq, start=(kk == 0), stop=last),
                                 reads=[lkn, rqn], writes=[pSn])
                        for i, (cols, lT, lTn, rh, rhn) in enumerate(ex):
                            S.op("pe", lambda e, cols=cols, lT=lT, rh=rh, i=i, pSx=pSx, nex=len(ex): e.matmul(pSx[:, cols[0]:cols[1]], lhsT=lT, rhs=rh, start=False, stop=(i == nex - 1)),
                                 reads=[lTn, rhn], writes=[pSn])

                    def ex_pv(idx):
                        c, pSx, pSn, PT, PTn = st[idx]
                        for (cols, sc_ap) in scale(c):
                            S.op("act", lambda e, cols=cols, sc_ap=sc_ap, PT=PT, pSx=pSx: e.activation(out=PT[:, cols[0]:cols[1]], in_=pSx[:, cols[0]:cols[1]], func=AF.Exp, scale=sc_ap),
                                 reads=[pSn, scalen], writes=[PTn])
                        seen = set()
                        for (ocols, otile, otn, qcols, vfn, vn) in pv_list:
                            first = (idx == 0) and (otn not in seen)
                            seen.add(otn)
                            lastr = [p[2] for p in pv_list if p[2] == otn][-1] is otn and all(not (p[2] == otn) for p in pv_list[pv_list.index((ocols, otile, otn, qcols, vfn, vn)) + 1:])
                            S.op("pe", lambda e, ocols=ocols, otile=otile, qcols=qcols, vfn=vfn, c=c, PT=PT, first=first, lastm=(idx == nch - 1 and lastr): e.matmul(
                                otile[:, ocols[0]:ocols[1]], lhsT=PT[:, qcols[0]:qcols[1]], rhs=vfn(c), start=first, stop=lastm),
                                reads=[PTn, vn], writes=[otn])

                    qk(0)
                    for idx in range(nch):
                        if idx + 1 < nch:
                            qk(idx + 1)
                        ex_pv(idx)

                for l in range(L):
                    cmax = min(4 * l + 3, NT - 1)
                    S.dma("sp", lambda e, l=l: e.dma_start(out=xt[:], in_=xq_d[l]), writes=["xt"])
                    S.dma("sp", lambda e, l=l: e.dma_start(out=cmask[:], in_=cmask_d[l]), writes=["cmask"])
                    S.dma("sp", lambda e, l=l: e.dma_start(out=pbias[:], in_=pbias_d[l]), writes=["pbias"])
                    xnorm_T(xt, "xt", junk, "xnb", small, "", xnb, "xnb", pT0, "pT0", xnT, "xnT")
                    for (pp, ppn, c0, c1) in ((pP0, "pP0", 0, 512), (pP1, "pP1", 512, Q_COLS)):
                        for k in range(8):
                            S.op("pe", lambda e, k=k, pp=pp, c0=c0, c1=c1: e.matmul(pp[:, 0:c1 - c0], lhsT=xnT[:, k, :], rhs=wq[:, k, c0:c1], start=(k == 0), stop=(k == 7)),
                                 reads=["xnT", "wq"], writes=[ppn])
                    if cut < 10:
                        continue
                    S.op("act", lambda e: e.activation(out=sqj[:, 0:512], in_=pP0[:], func=AF.Square), reads=["pP0"], writes=["sqj"])
                    S.op("dve", lambda e: e.tensor_reduce(out=s8[:], in_=sqj[:, 0:512].rearrange("p (h d) -> p h d", h=8), axis=AX.X, op=ALU.add), reads=["sqj"], writes=["s8"])
                    rstd_from_ss(s8[:], "s8", r8[:], "r8", 64.0, t8[:], "t8")
                    S.op("act", lambda e: e.copy(out=qn[:].rearrange("p h d -> p (h d)"), in_=pP0[:]), reads=["pP0"], writes=["qn"])
                    S.op("dve", lambda e: e.tensor_tensor(out=qn[:], in0=qn[:], in1=r8[:].unsqueeze(2).to_broadcast([128, 8, 64]), op=ALU.mult),
                         reads=["qn", "r8"], writes=["qn"])
                    S.op("dve", lambda e: e.tensor_tensor(out=qn[:], in0=qn[:], in1=gv("qa").unsqueeze(1).to_broadcast([128, 8, 64]), op=ALU.mult),
                         reads=["qn", "vecs"], writes=["qn"])
                    if cut < 10.2:
                        continue
                    for g in range(2):
                        S.op("dve", lambda e, g=g: e.tensor_copy(out=qab[:, :, g, 16:64], in_=qn[:, 4 * g:4 * g + 4, 16:64]), reads=["qn"], writes=["qab"])
                        rope("dve", qn[:, 4 * g:4 * g + 4, :], cosQA[:, l, :], sinQA[:, l, :], qab[:, :, g, :], rtmp[:, :, 0:4, 0:8], ["qn", cosQA_n, sinQA_n], ["qab"], 8)
                    if cut < 10.4:
                        continue
                    for r in range(4):
                        S.op("pe", lambda e, r=r: e.transpose(out=pT1[:, r * 128:(r + 1) * 128], in_=qab[:, r, :, :].rearrange("p g d -> p (g d)"), identity=identb[:]),
                             reads=["qab", "identb"], writes=["pT1"])
                    for g_ in range(2):
                        S.op("act", lambda e, g_=g_: e.copy(out=QaTz[g_][64 * g_:64 * g_ + 64, :, :].rearrange("p r t -> p (r t)"), in_=pT1[64 * g_:64 * g_ + 64, 0:512]), reads=["pT1"], writes=["QaT"])
                    if cut < 10.6:
                        continue
                    S.op("act", lambda e: e.activation(out=gat[:], in_=pP1[:, 0:24], func=AF.Sigmoid), reads=["pP1"], writes=["gat"])
                    ss, rs, tm = small
                    S.op("act", lambda e: e.activation(out=sqj[:, 0:256], in_=pP1[:, 24:280], func=AF.Square, accum_out=ss[:, 1:2]), reads=["pP1"], writes=["sqj", "ss1"])
                    rstd_from_ss(ss[:, 1:2], "ss1", rs[:, 1:2], "rs1", 256.0, tm[:, 1:2], "tm1")
                    S.op("act", lambda e: e.activation(out=cqn[:], in_=pP1[:, 24:280], func=AF.Copy, scale=rs[:, 1:2]), reads=["pP1", "rs1"], writes=["cqn"])
                    if cut < 10.8:
                        continue
                    for k in range(2):
                        S.op("pe", lambda e, k=k: e.transpose(out=pT1[:, 512 + k * 128:512 + (k + 1) * 128], in_=cqn[:, k * 128:(k + 1) * 128], identity=identb[:]),
                             reads=["cqn", "identb"], writes=["pT1b"])
                    S.op("act", lambda e: e.copy(out=cqT[:].rearrange("p k t -> p (k t)"), in_=pT1[:, 512:768]), reads=["pT1b"], writes=["cqT"])
                    for hb in range(2):
                        for k in range(2):
                            S.op("pe", lambda e, hb=hb, k=k: e.matmul(pSs[hb][:, 0:384], lhsT=cqT[:, k, :], rhs=wuq[:, k, hb * 384:(hb + 1) * 384], start=(k == 0), stop=(k == 1)),
                                 reads=["cqT", "wuq"], writes=["pS%d" % hb])
                        S.op("act", lambda e, hb=hb: e.activation(out=sqj[:, hb * 384:(hb + 1) * 384], in_=pSs[hb][:, 0:384], func=AF.Square), reads=["pS%d" % hb], writes=["sqj"])
                    S.op("dve", lambda e: e.tensor_reduce(out=s8[:], in_=sqj[:, 0:768].rearrange("p (h d) -> p h d", h=8), axis=AX.X, op=ALU.add), reads=["sqj"], writes=["s8"])
                    rstd_from_ss(s8[:], "s8", r8[:], "r8", 96.0, t8[:], "t8")
                    for hb in range(2):
                        S.op("act", lambda e, hb=hb: e.copy(out=qm[:, 4 * hb:4 * hb + 4, :].rearrange("p h d -> p (h d)"), in_=pSs[hb][:, 0:384]), reads=["pS%d" % hb], writes=["qm"])
                        S.op("dve", lambda e, hb=hb: e.tensor_tensor(out=qm[:, 4 * hb:4 * hb + 4, :], in0=qm[:, 4 * hb:4 * hb + 4, :],
                                                                   in1=r8[:, 4 * hb:4 * hb + 4].unsqueeze(2).to_broadcast([128, 4, 96]), op=ALU.mult),
                             reads=["qm", "r8"], writes=["qm"])
                    S.op("dve", lambda e: e.tensor_tensor(out=qm[:], in0=qm[:], in1=vecs[:, mq_o:mq_o + 96].unsqueeze(1).to_broadcast([128, 8, 96]), op=ALU.mult),
                         reads=["qm", "vecs"], writes=["qm"])
                    if cut < 10.9:
                        continue
                    S.op("dve", lambda e: e.tensor_tensor(out=qmn[:], in0=qm[:, :, 0:64], in1=vecs[:, mk_o:mk_o + 64].unsqueeze(1).to_broadcast([128, 8, 64]), op=ALU.mult),
                         reads=["qm", "vecs"], writes=["qmn"])
                    if cut < 11:
                        continue
                    rope("dve", qm[:, :, 64:96], cosQB[:, l, :], sinQB[:, l, :], qmr[:], rtmp[:, :, :, :], ["qm", cosQB_n, sinQB_n], ["qmr"], 16)
                    if cut < 11.2:
                        continue
                    for i in range(4):
                        S.op("pe", lambda e, i=i: e.transpose(out=pT0[:, i * 128:(i + 1) * 128], in_=qmn[:, 2 * i:2 * i + 2, :].rearrange("p a d -> p (a d)"), identity=identb[:]),
                             reads=["qmn", "identb"], writes=["pT0"])
                    S.op("act", lambda e: e.copy(out=qnT[:].rearrange("p i t -> p (i t)"), in_=pT0[:, 0:512]), reads=["pT0"], writes=["qnT"])
                    for h in range(8):
                        S.op("pe", lambda e, h=h: e.matmul(pSs[h // 4][:, (h % 4) * 128:(h % 4 + 1) * 128], lhsT=wukT[:, h, :],
                                                          rhs=qnT[:, h // 2, :], start=True, stop=True),
                             reads=["wukT", "qnT"], writes=["pS%d" % (h // 4)])
                    for hb in range(2):
                        S.op("act", lambda e, hb=hb: e.copy(out=QlatT[:, 4 * hb:4 * hb + 4, :].rearrange("p h t -> p (h t)"), in_=pSs[hb][:, 0:512]), reads=["pS%d" % hb], writes=["QlatT"])
                    if cut < 11.5:
                        continue
                    for h in range(8):
                        S.op("pe", lambda e, h=h: e.transpose(out=pT1[0:32, h * 128:(h + 1) * 128], in_=qmr[:, h, :], identity=identb[:]), reads=["qmr", "identb"], writes=["pT1", "pT1b"])
                    S.op("act", lambda e: e.copy(out=QropeT[0:32, :, :].rearrange("p h t -> p (h t)"), in_=pT1[0:32, 0:1024]), reads=["pT1", "pT1b"], writes=["QropeT"])

                    if cut < 12:
                        continue
                    nvmax = min(8 * (4 * l + 3) + 7, NCMP)
                    nvk = (nvmax + 127) // 128
                    nvc = nvk * 128
                    wc0 = max(0, 4 * l - 4)
                    nwc = cmax - wc0 + 1
                    S.dma("sp", lambda e, wc0=wc0, nwc=nwc: e.dma_start(out=KwTw[:, 0:nwc * 128], in_=kw_d[:, wc0 * 128:(wc0 + nwc) * 128]), reads=["kw_scr"], writes=["KwTw"])
                    S.dma("sp", lambda e, wc0=wc0, nwc=nwc: e.dma_start(out=Vww[:, 0:nwc, :, :].rearrange("p c g d -> p c (g d)"), in_=vw_d[:, wc0:wc0 + nwc, :]), reads=["vw_scr"], writes=["Vww"])
                    for g in range(2):
                        for r in range(4):
                            pSx, pSn = pSs[r % 2], "pS%d" % (r % 2)
                            S.op("pe", lambda e, r=r, g=g, pSx=pSx: e.matmul(pSx[:, 0:nvc], lhsT=QaTz[g][:, r, :], rhs=KcT[:, 0:nvc], start=True, stop=True),
                                 reads=["QaT", "KcT"], writes=[pSn])
                            S.op("act", lambda e, r=r, pSx=pSx: e.activation(out=Pc[:, r, 0:nvc], in_=pSx[:, 0:nvc], func=AF.Exp, scale=SCALE_A), reads=[pSn], writes=["Pc"])
                        if cut < 13:
                            continue
                        S.op("dve", lambda e: e.tensor_tensor(out=Pc[:, :, 0:nvc], in0=Pc[:, :, 0:nvc], in1=cmask[:, 0:nvc].unsqueeze(1).to_broadcast([128, 4, nvc]), op=ALU.mult),
                             reads=["Pc", "cmask"], writes=["Pc"])
                        S.op("dve", lambda e: e.tensor_reduce(out=z4[:], in_=Pc[:, :, 0:nvc], axis=AX.X, op=ALU.add), reads=["Pc"], writes=["z4"])
                        S.op("dve", lambda e: e.tensor_scalar(out=z4[:], in0=z4[:], scalar1=1e-30, scalar2=None, op0=ALU.max), reads=["z4"], writes=["z4"])
                        S.op("dve", lambda e: e.reciprocal(out=z4[:], in_=z4[:]), reads=["z4"], writes=["z4"])
                        S.op("dve", lambda e: e.tensor_tensor(out=Pc[:, :, 0:nvc], in0=Pc[:, :, 0:nvc], in1=z4[:].unsqueeze(2).to_broadcast([128, 4, nvc]), op=ALU.mult),
                             reads=["Pc", "z4"], writes=["Pc"])
                        S.op("dve", lambda e: e.tensor_tensor(out=Psm[:, 0:nvc], in0=Pc[:, 0, 0:nvc], in1=Pc[:, 1, 0:nvc], op=ALU.add), reads=["Pc"], writes=["Psm"])
                        S.op("dve", lambda e: e.tensor_tensor(out=Psm[:, 0:nvc], in0=Psm[:, 0:nvc], in1=Pc[:, 2, 0:nvc], op=ALU.add), reads=["Pc", "Psm"], writes=["Psm"])
                        S.op("dve", lambda e: e.tensor_tensor(out=Psm[:, 0:nvc], in0=Psm[:, 0:nvc], in1=Pc[:, 3, 0:nvc], op=ALU.add), reads=["Pc", "Psm"], writes=["Psm"])
                        for k in range(nvk):
                            S.op("pe", lambda e, k=k: e.transpose(out=pX[:, k * 128:(k + 1) * 128], in_=Psm[:, k * 128:(k + 1) * 128], identity=ident[:]), reads=["Psm", "ident"], writes=["pX"])
                        S.op("act", lambda e: e.copy(out=PsT[:, 0:nvk, :].rearrange("p k t -> p (k t)"), in_=pX[:, 0:nvc]), reads=["pX"], writes=["PsT"])
                        if cut < 14:
                            continue
                        for k in range(nvk):
                            S.op("pe", lambda e, k=k: e.matmul(pP0[:, 0:NSLC], lhsT=PsT[:, k, :], rhs=ovl[:, k, 0:NSLC], start=(k == 0), stop=(k == nvk - 1)), reads=["PsT", "ovl"], writes=["pP0"])
                        S.op("dve", lambda e: e.tensor_tensor(out=impb[:, 0:NSLC], in0=pP0[:, 0:NSLC], in1=pbias[:, 0:NSLC], op=ALU.add), reads=["pP0", "pbias"], writes=["impb"])
                        if cut < 14.3:
                            continue
                        S.op("dve", lambda e: e.max(out=m8[:, 0:8], in_=impb[:, 0:NSLC]), reads=["impb"], writes=["m8a"])
                        S.op("dve", lambda e: e.match_replace(out=wk[:, 0:NSLC], in_to_replace=m8[:, 0:8], in_values=impb[:, 0:NSLC], imm_value=-3e38), reads=["impb", "m8a"], writes=["wk"])
                        S.op("dve", lambda e: e.max(out=m8[:, 8:16], in_=wk[:, 0:NSLC]), reads=["wk"], writes=["m8b"])
                        S.op("dve", lambda e: e.tensor_scalar(out=nsl[:, 0:NSLC], in0=impb[:, 0:NSLC], scalar1=m8[:, 15:16], scalar2=1.0, op0=ALU.is_ge, op1=ALU.subtract),
                             reads=["impb", "m8b"], writes=["nsl"])
                        S.op("dve", lambda e: e.tensor_scalar(out=nslb[:], in0=nsl[:], scalar1=-NEGM, scalar2=None, op0=ALU.mult), reads=["nsl"], writes=["nslb"])
                        if cut < 14.6:
                            continue
                        S.op("pe", lambda e: e.transpose(out=pT0[:, 0:128], in_=nslb[:], identity=identb[:]), reads=["nslb", "identb"], writes=["pT0"])
                        for hv in range(2):
                            S.op("act", lambda e, hv=hv: e.copy(out=nselZ[hv][64 * hv:64 * hv + 64, :, :], in_=pT0[64 * hv:64 * hv + 64, 0:128].unsqueeze(1).to_broadcast([64, 4, 128])), reads=["pT0"], writes=["nselT4"])
                        if cut < 15:
                            continue
                        if dbg and l == L - 1 and g == 0:
                            o = dout("d_impb", [128, 128]); S.dma("sp", lambda e, o=o: e.dma_start(out=o, in_=impb[:]), reads=["impb"], writes=["d_impb"])
                            o = dout("d_nsl", [128, 128]); S.dma("sp", lambda e, o=o: e.dma_start(out=o, in_=nsl[:]), reads=["nsl"], writes=["d_nsl"])
                        for r in range(4):
                            S.op("dve", lambda e, r=r: e.tensor_copy(out=Pcb[:, 0:nvc], in_=Pc[:, r, 0:nvc]), reads=["Pc"], writes=["Pcb"])
                            for k in range(nvk):
                                S.op("pe", lambda e, r=r, k=k: e.transpose(out=pT1[:, k * 128:(k + 1) * 128], in_=Pcb[:, k * 128:(k + 1) * 128], identity=identb[:]),
                                     reads=["Pcb", "identb"], writes=["pT1"])
                            S.op("act", lambda e: e.copy(out=PcT[:, 0:nvk, :].rearrange("p k t -> p (k t)"), in_=pT1[:, 0:nvc]), reads=["pT1"], writes=["PcT"])
                            for k in range(nvk):
                                S.op("pe", lambda e, r=r, k=k, g=g: e.matmul(pO[:, r * 64:(r + 1) * 64], lhsT=PcT[:, k, :], rhs=Vc[:, k, g, :], start=(k == 0 and r == 0), stop=(k == nvk - 1 and r == 3)),
                                     reads=["PcT", "Vc"], writes=["pO"])
                        for r in range(4):
                            h = 4 * g + r
                            S.op("dve", lambda e, r=r, h=h: e.tensor_scalar(out=oa[:, h, :], in0=pO[:, r * 64:(r + 1) * 64], scalar1=gat[:, 3 * h:3 * h + 1], scalar2=None, op0=ALU.mult),
                                 reads=["pO", "gat"], writes=["oa"])
                        if cut < 16:
                            continue
                        QaTg = QaTz[g][:, :, :].rearrange("p r t -> p (r t)")
                        for br in range(2):
                            if br == 1 and cut < 17:
                                continue
                            if br == 0:
                                chunks = list(range(0, cmax + 1))
                                Kt, Ktn, Vt, Vtn = KsT, "KsT", Vs, "Vs"

                                def extra(c, l=l, g=g):
                                    ex = [((0, 512), e64[:, (c % 32) * 128:(c % 32 + 1) * 128], "e64",
                                           nselZ[c // 32][:, :, :].rearrange("p r t -> p (r t)"), "nselT4")]
                                    if c >= 4 * l:
                                        for r in range(4):
                                            ex.append(((r * 128, (r + 1) * 128), identb[:], "identb", dmask[:, c - 4 * l, :], "dmask"))
                                    return ex
                            else:
                                chunks = list(range(0, cmax - wc0 + 1))
                                Kt, Ktn, Vt, Vtn = KwTw, "KwTw", Vww, "Vww"

                                def extra(c, l=l, g=g, wc0=wc0):
                                    return [((r * 128, (r + 1) * 128), identb[:], "identb", wmask[:, c + wc0 - (4 * l - 4), :], "wmask") for r in range(4)]
                            pv_list = [((r * 65, (r + 1) * 65), pO, "pO", (r * 128, (r + 1) * 128), (lambda c, Vt=Vt, g=g: Vt[:, c, g, :]), Vtn) for r in range(4)]
                            attn_chunks([Kt[:, :]], [Ktn], [QaTg], ["QaT"], 512, extra, (lambda c: [((0, 512), SCALE_A)]), "vecs", pv_list, chunks, None)
                            S.op("dve", lambda e: e.tensor_scalar(out=z4[:], in0=pO[:, 0:260].rearrange("p (r d) -> p r d", r=4)[:, :, 64], scalar1=1e-30, scalar2=None, op0=ALU.max), reads=["pO"], writes=["z4"])
                            S.op("dve", lambda e: e.reciprocal(out=z4[:], in_=z4[:]), reads=["z4"], writes=["z4"])
                            gsl = gat[:, 12 * g:12 * g + 12].rearrange("p (r t) -> p r t", t=3)[:, :, 1 + br]
                            S.op("dve", lambda e, gsl=gsl: e.tensor_tensor(out=f4[:], in0=z4[:], in1=gsl, op=ALU.mult), reads=["z4", "gat"], writes=["f4"])
                            for r in range(4):
                                h = 4 * g + r
                                S.op("dve", lambda e, r=r, h=h: e.scalar_tensor_tensor(out=oa[:, h, :], in0=pO[:, r * 65:r * 65 + 64], scalar=f4[:, r:r + 1], in1=oa[:, h, :], op0=ALU.mult, op1=ALU.add),
                                     reads=["pO", "f4", "oa"], writes=["oa"])
                    if cut < 18:
                        continue
                    chunks = list(range(0, cmax + 1))
                    for hb in range(2):
                        def extra(c, l=l):
                            if c >= 4 * l:
                                return [((r * 128, (r + 1) * 128), identb[:], "identb", dmask[:, c - 4 * l, :], "dmask") for r in range(4)]
                            return []
                        pv_list = []
                        for r in range(4):
                            ot, otn = (pO, "pO") if r < 2 else (pX, "pX")
                            pv_list.append((((r % 2) * 129, (r % 2 + 1) * 129), ot, otn, (r * 128, (r + 1) * 128), (lambda c: ckva[:, c, :]), "ckva"))
                        attn_chunks([ckvT[:, :], krT[:, :]], ["ckvT", "krT"],
                                    [QlatT[:, 4 * hb:4 * hb + 4, :].rearrange("p h t -> p (h t)"), QropeT[:, 4 * hb:4 * hb + 4, :].rearrange("p h t -> p (h t)")], ["QlatT", "QropeT"],
                                    512, extra, (lambda c, hb=hb: [((r * 128, (r + 1) * 128), srh[:, c, 4 * hb + r:4 * hb + r + 1]) for r in range(4)]), "srh", pv_list, chunks, None)
                        for r in range(4):
                            h = 4 * hb + r
                            ot, otn = (pO, "pO") if r < 2 else (pX, "pX")
                            b0 = (r % 2) * 129
                            S.op("dve", lambda e, ot=ot, b0=b0: e.reciprocal(out=z4[:, 0:1], in_=ot[:, b0 + 128:b0 + 129]), reads=[otn], writes=["z4"])
                            S.op("act", lambda e, ot=ot, b0=b0: e.activation(out=olb[:], in_=ot[:, b0:b0 + 128], func=AF.Copy, scale=z4[:, 0:1]), reads=[otn, "z4"], writes=["olb"])
                            S.op("pe", lambda e: e.transpose(out=pT0[:, 0:128], in_=olb[:], identity=identb[:]), reads=["olb", "identb"], writes=["pT0"])
                            S.op("act", lambda e: e.copy(out=OlatT[:], in_=pT0[:, 0:128]), reads=["pT0"], writes=["OlatT"])
                            S.op("pe", lambda e, h=h: e.matmul(pP0[:, h * 64:(h + 1) * 64], lhsT=OlatT[:], rhs=wuv[:, h * 64:(h + 1) * 64], start=True, stop=True), reads=["OlatT", "wuv"], writes=["pP0"])
                    if cut < 19:
                        continue
                    S.op("act", lambda e: e.activation(out=sqj[:, 0:512], in_=oa[:].rearrange("p h d -> p (h d)"), func=AF.Square, accum_out=ss[:, 2:3]), reads=["oa"], writes=["sqj", "ss2"])
                    S.op("act", lambda e: e.activation(out=sqj[:, 0:512], in_=pP0[:], func=AF.Square, accum_out=ss[:, 3:4]), reads=["pP0"], writes=["sqj", "ss2"])
                    rstd_from_ss(ss[:, 2:4], "ss2", rs[:, 2:4], "rs2", 512.0, tm[:, 2:4], "tm2")
                    S.op("dve", lambda e: e.tensor_scalar(out=onb[:, 0:512], in0=oa[:].rearrange("p h d -> p (h d)"), scalar1=rs[:, 2:3], scalar2=None, op0=ALU.mult), reads=["oa", "rs2"], writes=["xnb"])
                    S.op("act", lambda e: e.activation(out=onb[:, 512:1024], in_=pP0[:], func=AF.Copy, scale=rs[:, 3:4]), reads=["pP0", "rs2"], writes=["xnb"])
                    if dbg:
                        o = dout("d_oa%d" % l, [128, 512]); S.dma("sp", lambda e, o=o: e.dma_start(out=o, in_=oa[:].rearrange("p h d -> p (h d)")), reads=["oa"], writes=["d_oa%d" % l])
                        o = dout("d_on%d" % l, [128, 1024], BF16); S.dma("sp", lambda e, o=o: e.dma_start(out=o, in_=onb[:]), reads=["xnb"], writes=["d_on%d" % l])
                    for k in range(8):
                        S.op("pe", lambda e, k=k: e.transpose(out=pT1[:, k * 128:(k + 1) * 128], in_=onb[:, k * 128:(k + 1) * 128], identity=identb[:]), reads=["xnb", "identb"], writes=["pT1", "pT1b"])
                    S.op("act", lambda e: e.copy(out=onT[:].rearrange("p k t -> p (k t)"), in_=pT1[:, 0:1024]), reads=["pT1", "pT1b"], writes=["xnT"])
                    for hf in range(2):
                        for k in range(8):
                            S.op("pe", lambda e, hf=hf, k=k: e.matmul(pSs[hf][:, 0:512], lhsT=onT[:, k, :], rhs=wout[:, k, hf * 512:(hf + 1) * 512], start=(k == 0), stop=(k == 7)),
                                 reads=["xnT", "wout"], writes=["pS%d" % hf])
                        S.op("dve", lambda e, hf=hf: e.tensor_tensor(out=ht[:, hf * 512:(hf + 1) * 512], in0=pSs[hf][:, 0:512], in1=xt[:, hf * 512:(hf + 1) * 512], op=ALU.add),
                             reads=["pS%d" % hf, "xt"], writes=["xt"])
                    S.dma("sp", lambda e, l=l: e.dma_start(out=hs_d[l], in_=ht[:]), reads=["xt"], writes=["hs"])
                    if dbg:
                        o = dout("d_h%d" % l, [128, 1024]); S.dma("sp", lambda e, o=o: e.dma_start(out=o, in_=ht[:]), reads=["xt"], writes=["d_h%d" % l])
                S.barrier()

        S.barrier()
        kvst.close()
        if PHASE3:
            with ExitStack() as p3:
                wpq = sb("wpq", [128, 8, 2048], BF16, p3)
                skT = sb("skT", [128, 16, 128], BF16, p3)
                n2g = sb("n2g", [128, 1024], F32, p3)
                iota16 = sb("iota16", [128, 16], F32, p3)
                S.dma("sp", lambda e: e.dma_start(out=n2g[:], in_=n2_d), writes=["n2g"])
                S.dma("sp", lambda e: e.dma_start(out=iota16[:], in_=iota_d), writes=["iota16"])
                with ExitStack() as ld:
                    load_w(ld, wpq, "wpq", wpq_d, 8, 2048)
                    load_w(ld, skT, "skT", skT_d, 16, 128)
                    S.barrier()
                ht = sb("ht3", [128, 1024], F32, p3)
                hn = sb("hn", [128, 1024], F32, p3)
                hnb = sb("hnb", [128, 1024], BF16, p3)
                junk = sb("junk3", [128, 1024], BF16, p3)
                junkf = sb("junkf", [128, 1024], F32, p3)
                hnT = sb("hnT", [128, 8, 128], BF16, p3)
                small = (sb("ss_3", [128, 8], F32, p3), sb("rs_3", [128, 8], F32, p3), sb("tm_3", [128, 8], F32, p3))
                sq2 = sb("sq2", [128, 2048], F32, p3)
                s8 = sb("s8_3", [128, 8], F32, p3)
                r8 = sb("r8_3", [128, 8], F32, p3)
                t8 = sb("t8_3", [128, 8], F32, p3)
                qpb = sb("qpb", [128, 2048], BF16, p3)
                qT = sb("qT", [128, 16, 128], BF16, p3)
                sc = sb("sc", [128, 16, 128], F32, p3)
                wk3 = sb("wk3", [128, 256], F32, p3)
                v16 = sb("v16", [128, 16, 16], F32, p3)
                i16 = sb("i16", [128, 16, 16], U32, p3)
                i16f = sb("i16f", [128, 16, 16], F32, p3)
                cand = sb("cand", [128, 8, 16, 16], F32, p3)
                vals = sb("vals", [128, 8, 16], F32, p3)
                ci = sb("ci", [128, 8, 16], U32, p3)
                ca = sb("ca", [128, 8, 16], I32, p3)
                cb = sb("cb", [128, 8, 16], I32, p3)
                caf = sb("caf", [128, 8, 16], F32, p3)
                cbf = sb("cbf", [128, 8, 16], F32, p3)
                oh = sb("oh", [128, 8, 16, 16], F32, p3)
                e1 = sb("e1", [128, 8, 16], F32, p3)
                e2 = sb("e2", [128, 8, 16], F32, p3)
                ei = sb("ei", [128, 128], I32, p3)
                gts = sb("gts", [128, 8, 16], F32, p3)
                gs8 = sb("gs8", [128, 8], F32, p3)
                av = sb("av", [128, 128], F32, p3)
                wv = sb("wv", [128, 128], F32, p3)
                NB = 4
                ugs = [sb("ug%d" % i, [128, 1024], BF16, p3) for i in range(NB)]
                vgs = [sb("vg%d" % i, [128, 1024], BF16, p3) for i in range(NB)]
                dgs = [sb("dg%d" % i, [128, 128], BF16, p3) for i in range(NB)]
                ot = sb("ot", [128, 1024], F32, p3)
                pT3 = ps("pT3", [128, 1024], BF16, p3)
                pT4 = ps("pT4", [128, 1024], BF16, p3)
                pQ = [ps("pQ%d" % i, [128, 512], F32, p3) for i in range(4)]
                pY = [ps("pY%d" % i, [128, 512], F32, p3) for i in range(2)]
                for l in range(L):
                    S.dma("sp", lambda e, l=l: e.dma_start(out=ht[:], in_=hs_d[l]), reads=["hs"], writes=["ht3"])
                    ss, rs, tm = small
                    S.op("act", lambda e: e.activation(out=junk[:], in_=ht[:], func=AF.Square, accum_out=ss[:, 0:1]), reads=["ht3"], writes=["junk3", "ss_3"])
                    rstd_from_ss(ss[:, 0:1], "ss_3", rs[:, 0:1], "rs_3", 1024.0, tm[:, 0:1], "tm_3")
                    S.op("dve", lambda e: e.scalar_tensor_tensor(out=hn[:], in0=ht[:], scalar=rs[:, 0:1], in1=n2g[:], op0=ALU.mult, op1=ALU.mult), reads=["ht3", "rs_3", "n2g"], writes=["hn"])
                    S.op("dve", lambda e: e.tensor_copy(out=hnb[:], in_=hn[:]), reads=["hn"], writes=["hnb"])
                    for k in range(8):
                        S.op("pe", lambda e, k=k: e.transpose(out=pT3[:, k * 128:(k + 1) * 128], in_=hnb[:, k * 128:(k + 1) * 128], identity=identb[:]), reads=["hnb", "identb"], writes=["pT3"])
                    S.op("act", lambda e: e.copy(out=hnT[:].rearrange("p k t -> p (k t)"), in_=pT3[:, 0:1024]), reads=["pT3"], writes=["hnT"])
                    for q4 in range(4):
                        for k in range(8):
                            S.op("pe", lambda e, q4=q4, k=k: e.matmul(pQ[q4][:, 0:512], lhsT=hnT[:, k, :], rhs=wpq[:, k, q4 * 512:(q4 + 1) * 512], start=(k == 0), stop=(k == 7)),
                                 reads=["hnT", "wpq"], writes=["pQ%d" % q4])
                        S.op("act", lambda e, q4=q4: e.activation(out=sq2[:, q4 * 512:(q4 + 1) * 512], in_=pQ[q4][:, 0:512], func=AF.Square), reads=["pQ%d" % q4], writes=["sq2"])
                    S.op("dve", lambda e: e.tensor_reduce(out=s8[:], in_=sq2[:].rearrange("p (h d) -> p h d", h=8), axis=AX.X, op=ALU.add), reads=["sq2"], writes=["s8_3"])
                    rstd_from_ss(s8[:], "s8_3", r8[:], "r8_3", 256.0, t8[:], "t8_3")
                    for q4 in range(4):
                        S.op("act", lambda e, q4=q4: e.copy(out=sq2[:, q4 * 512:(q4 + 1) * 512], in_=pQ[q4][:, 0:512]), reads=["pQ%d" % q4, "s8_3"], writes=["sq2"])
                        S.op("dve", lambda e, q4=q4: e.tensor_tensor(out=qpb[:, q4 * 512:(q4 + 1) * 512].rearrange("p (h d) -> p h d", h=2), in0=sq2[:, q4 * 512:(q4 + 1) * 512].rearrange("p (h d) -> p h d", h=2),
                                                                   in1=r8[:, 2 * q4:2 * q4 + 2].unsqueeze(2).to_broadcast([128, 2, 256]), op=ALU.mult), reads=["sq2", "r8_3"], writes=["qpb"])
                    for i in range(16):
                        pt, ptn = (pT3, "pT3") if i < 8 else (pT4, "pT4")
                        S.op("pe", lambda e, i=i, pt=pt: e.transpose(out=pt[:, (i % 8) * 128:(i % 8 + 1) * 128], in_=qpb[:, i * 128:(i + 1) * 128], identity=identb[:]), reads=["qpb", "identb"], writes=[ptn])
                    S.op("act", lambda e: e.copy(out=qT[:, 0:8, :].rearrange("p k t -> p (k t)"), in_=pT3[:, 0:1024]), reads=["pT3"], writes=["qT"])
                    S.op("act", lambda e: e.copy(out=qT[:, 8:16, :].rearrange("p k t -> p (k t)"), in_=pT4[:, 0:1024]), reads=["pT4"], writes=["qT"])
                    for i in range(16):
                        S.op("pe", lambda e, i=i: e.matmul(pQ[i // 4][:, (i % 4) * 128:(i % 4 + 1) * 128], lhsT=qT[:, i, :], rhs=skT[:, i, :], start=True, stop=True), reads=["qT", "skT"], writes=["pQ%d" % (i // 4)])
                    for q4 in range(4):
                        S.op("act", lambda e, q4=q4: e.copy(out=sc[:, 4 * q4:4 * q4 + 4, :].rearrange("p a n -> p (a n)"), in_=pQ[q4][:, 0:512]), reads=["pQ%d" % q4], writes=["sc"])
                    for i in range(16):
                        S.op("dve", lambda e, i=i: e.max(out=v16[:, i, 0:8], in_=sc[:, i, :]), reads=["sc"], writes=["v16"])
                        S.op("dve", lambda e, i=i: e.max_index(out=i16[:, i, 0:8], in_max=v16[:, i, 0:8], in_values=sc[:, i, :]), reads=["sc", "v16"], writes=["i16"])
                        S.op("dve", lambda e, i=i: e.match_replace(out=wk3[:, 0:128], in_to_replace=v16[:, i, 0:8], in_values=sc[:, i, :], imm_value=-3e38), reads=["sc", "v16"], writes=["wk3"])
                        S.op("dve", lambda e, i=i: e.max(out=v16[:, i, 8:16], in_=wk3[:, 0:128]), reads=["wk3"], writes=["v16"])
                        S.op("dve", lambda e, i=i: e.max_index(out=i16[:, i, 8:16], in_max=v16[:, i, 8:16], in_values=wk3[:, 0:128]), reads=["wk3", "v16"], writes=["i16"])
                    S.op("dve", lambda e: e.tensor_copy(out=i16f[:], in_=i16[:]), reads=["i16"], writes=["i16f"])
                    v4 = v16[:].rearrange("p (h two) k -> p h two k", two=2)
                    i4 = i16f[:].rearrange("p (h two) k -> p h two k", two=2)
                    for h in range(8):
                        S.op("dve", lambda e, h=h: e.tensor_tensor(out=cand[:, h, :, :], in0=v4[:, h, 0, :].unsqueeze(2).to_broadcast([128, 16, 16]),
                                                                 in1=v4[:, h, 1, :].unsqueeze(1).to_broadcast([128, 16, 16]), op=ALU.add), reads=["v16"], writes=["cand"])
                    for h in range(8):
                        cf = cand[:, h, :, :].rearrange("p a b -> p (a b)")
                        S.op("dve", lambda e, h=h, cf=cf: e.max(out=vals[:, h, 0:8], in_=cf), reads=["cand"], writes=["vals"])
                        S.op("dve", lambda e, h=h, cf=cf: e.max_index(out=ci[:, h, 0:8], in_max=vals[:, h, 0:8], in_values=cf), reads=["cand", "vals"], writes=["ci"])
                        S.op("dve", lambda e, h=h, cf=cf: e.match_replace(out=wk3[:], in_to_replace=vals[:, h, 0:8], in_values=cf, imm_value=-3e38), reads=["cand", "vals"], writes=["wk3"])
                        S.op("dve", lambda e, h=h: e.max(out=vals[:, h, 8:16], in_=wk3[:]), reads=["wk3"], writes=["vals"])
                        S.op("dve", lambda e, h=h: e.max_index(out=ci[:, h, 8:16], in_max=vals[:, h, 8:16], in_values=wk3[:]), reads=["wk3", "vals"], writes=["ci"])
                    S.op("dve", lambda e: e.tensor_tensor(out=gts[:], in0=vals[:], in1=vals[:, :, 0:1].to_broadcast([128, 8, 16]), op=ALU.subtract), reads=["vals"], writes=["gts"])
                    S.op("act", lambda e: e.activation(out=gts[:], in_=gts[:], func=AF.Exp), reads=["gts"], writes=["gts"])
                    S.op("dve", lambda e: e.tensor_reduce(out=gs8[:], in_=gts[:], axis=AX.X, op=ALU.add), reads=["gts"], writes=["gs8"])
                    S.op("dve", lambda e: e.reciprocal(out=gs8[:], in_=gs8[:]), reads=["gs8"], writes=["gs8"])
                    S.op("dve", lambda e: e.tensor_tensor(out=gts[:], in0=gts[:], in1=gs8[:].unsqueeze(2).to_broadcast([128, 8, 16]), op=ALU.mult), reads=["gts", "gs8"], writes=["gts"])
                    cii = ci[:].bitcast(I32)
                    S.op("dve", lambda e: e.tensor_single_scalar(out=ca[:], in_=cii, scalar=4, op=ALU.arith_shift_right), reads=["ci"], writes=["ca"])
                    S.op("dve", lambda e: e.tensor_single_scalar(out=cb[:], in_=cii, scalar=15, op=ALU.bitwise_and), reads=["ci"], writes=["cb"])
                    S.op("dve", lambda e: e.tensor_copy(out=caf[:], in_=ca[:]), reads=["ca"], writes=["caf"])
                    S.op("dve", lambda e: e.tensor_copy(out=cbf[:], in_=cb[:]), reads=["cb"], writes=["cbf"])
                    for (cf_, cfn, half, eo, eon) in ((caf, "caf", 0, e1, "e1"), (cbf, "cbf", 1, e2, "e2")):
                        for h in range(8):
                            S.op("dve", lambda e, h=h, cf_=cf_: e.tensor_tensor(out=oh[:, h, :, :], in0=cf_[:, h, :].unsqueeze(2).to_broadcast([128, 16, 16]),
                                                                             in1=iota16[:].unsqueeze(1).to_broadcast([128, 16, 16]), op=ALU.is_equal), reads=[cfn, "iota16"], writes=["oh"])
                            S.op("dve", lambda e, h=h, half=half: e.tensor_tensor(out=oh[:, h, :, :], in0=oh[:, h, :, :], in1=i4[:, h, half, :].unsqueeze(1).to_broadcast([128, 16, 16]), op=ALU.mult),
                                 reads=["oh", "i16f"], writes=["oh"])
                        S.op("dve", lambda e, eo=eo: e.tensor_reduce(out=eo[:].rearrange("p h k -> p (h k)"), in_=oh[:].rearrange("p h k a -> p (h k) a"), axis=AX.X, op=ALU.add), reads=["oh"], writes=[eon])
                    S.op("dve", lambda e: e.scalar_tensor_tensor(out=e1[:], in0=e1[:], scalar=128.0, in1=e2[:], op0=ALU.mult, op1=ALU.add), reads=["e1", "e2"], writes=["e1"])
                    S.op("dve", lambda e: e.tensor_copy(out=ei[:], in_=e1[:].rearrange("p h k -> p (h k)")), reads=["e1"], writes=["ei"])
                    if dbg and l == 0:
                        o = dout("d_ei", [128, 128], I32); S.dma("sp", lambda e, o=o: e.dma_start(out=o, in_=ei[:]), reads=["ei"], writes=["d_ei"])
                        o = dout("d_gts", [128, 128]); S.dma("sp", lambda e, o=o: e.dma_start(out=o, in_=gts[:].rearrange("p h k -> p (h k)")), reads=["gts"], writes=["d_gts"])
                    for s_ in range(128):
                        b_ = s_ % NB
                        S.dma("pool", lambda e, s_=s_, b_=b_: e.indirect_dma_start(out=ugs[b_][:], out_offset=None, in_=pub_d, in_offset=bass.IndirectOffsetOnAxis(ap=ei[:, s_:s_ + 1], axis=0)),
                              reads=["ei", "pb16"], writes=["ug%d" % b_])
                        S.op("dve", lambda e, s_=s_, b_=b_: e.scalar_tensor_tensor(out=junkf[:], in0=ugs[b_][:], scalar=1.0, in1=hnb[:], op0=ALU.mult, op1=ALU.mult, accum_out=av[:, s_:s_ + 1]),
                             reads=["ug%d" % b_, "hnb"], writes=["junkf", "av"])
                    S.op("act", lambda e: e.activation(out=wv[:], in_=av[:], func=AF.Gelu_apprx_tanh), reads=["av"], writes=["wv"])
                    S.op("dve", lambda e: e.tensor_tensor(out=wv[:], in0=wv[:], in1=gts[:].rearrange("p h k -> p (h k)"), op=ALU.mult), reads=["wv", "gts"], writes=["wv"])
                    for s_ in range(128):
                        b_ = s_ % NB
                        S.dma("pool", lambda e, s_=s_, b_=b_: e.indirect_dma_start(out=vgs[b_][:], out_offset=None, in_=pvb_d, in_offset=bass.IndirectOffsetOnAxis(ap=ei[:, s_:s_ + 1], axis=0)),
                              reads=["ei", "pb16"], writes=["vg%d" % b_])
                        S.op("act", lambda e, s_=s_, b_=b_: e.activation(out=dgs[b_][:], in_=identb[:], func=AF.Copy, scale=wv[:, s_:s_ + 1]), reads=["identb", "wv"], writes=["dg%d" % b_])
                        for hf in range(2):
                            S.op("pe", lambda e, s_=s_, b_=b_, hf=hf: e.matmul(pY[hf][:, 0:512], lhsT=dgs[b_][:], rhs=vgs[b_][:, hf * 512:(hf + 1) * 512], start=(s_ == 0), stop=(s_ == 127)),
                                 reads=["dg%d" % b_, "vg%d" % b_], writes=["pY%d" % hf])
                    for hf in range(2):
                        S.op("dve", lambda e, hf=hf: e.tensor_tensor(out=ot[:, hf * 512:(hf + 1) * 512], in0=pY[hf][:, 0:512], in1=ht[:, hf * 512:(hf + 1) * 512], op=ALU.add), reads=["pY%d" % hf, "ht3"], writes=["ot"])
                    S.dma("sp", lambda e, l=l: e.dma_start(out=out_d[l], in_=ot[:]), reads=["ot"], writes=["out"])
                S.barrier()

        finals = list(dbg_outs.keys())
        if PHASE3:
            finals.append("out")
        elif PHASE2:
            finals.append("hs")
        S.emit(final_wait_keys=finals)
    return nc, list(dbg_outs.keys())


def _kc(w):
    K = w.shape[0] // 128
    return np.ascontiguousarray(w.reshape(K, 128, -1).transpose(1, 0, 2))


def _consts(NT, L, j):
    SEQ = NT * 128
    NCMP = (SEQ - 32) // 16 + 1
    NCK = (NCMP + 127) // 128
    NCP = NCK * 128
    NSLC = SEQ // 64
    bf = ml_dtypes.bfloat16
    c = {}
    c["ident"] = np.eye(128, dtype=np.float32)
    jj = np.arange(64)[:, None, None]
    cc = np.arange(32)[None, :, None]
    s = np.arange(128)[None, None, :]
    e = (jj == 2 * cc + s // 64).astype(np.float32).reshape(64, 32 * 128)
    c["e64"] = np.concatenate([e, e], 0).astype(np.float32).reshape(128, 2, 2048)
    n = np.arange(NCP)
    cs, ce = 16 * n, 16 * n + 31
    sl = 64 * np.arange(128)
    ov = ((cs[:, None] < sl[None, :] + 64) & (ce[:, None] >= sl[None, :]) & (n[:, None] < NCMP) & (np.arange(128)[None, :] < NSLC)).astype(np.float32)
    c["ovl"] = np.ascontiguousarray(ov.reshape(NCK, 128, 128).transpose(1, 0, 2))
    qi = np.arange(128)
    cm = np.zeros((L, 128, NCP), np.float32)
    pb = np.zeros((L, 128, 128), np.float32)
    for l in range(L):
        t = 128 * (4 * l + j) + qi
        cm[l] = ((ce[None, :] <= t[:, None]) & (n[None, :] < NCMP)).astype(np.float32)
        jb = np.arange(128)[None, :]
        causal = (64 * jb <= t[:, None]) & (jb < NSLC)
        b = np.where(causal, 0.0, -1e30)
        b = np.where(jb == (t // 64)[:, None], 1e9, b)
        b = np.where(jb == 0, 2e9, b)
        pb[l] = b
    c["cmask"] = cm
    c["pbias"] = pb
    sq = np.arange(128)
    tri_d = np.where(sq[:, None] <= sq[None, :], 0.0, NEGM)
    tri_w = np.where(sq[:, None] > sq[None, :], 0.0, NEGM)
    full0 = np.zeros((128, 128))
    fulln = np.full((128, 128), NEGM)
    dm = np.stack([full0 if k < j else (tri_d if k == j else fulln) for k in range(4)], 1)
    c["dmask"] = dm.astype(np.float32)
    wm = []
    for k in range(8):
        d = k - 4 - j
        wm.append(fulln if d < -4 else tri_w if d == -4 else full0 if d < 0 else tri_d if d == 0 else fulln)
    c["wmask"] = np.stack(wm, 1).astype(np.float32)
    c["iota16"] = np.tile(np.arange(16, dtype=np.float32)[None, :], (128, 1))
    invA = (500000.0 ** (-np.arange(0, 16, 2, dtype=np.float32) / np.float32(16))).astype(np.float32)
    invB = (500000.0 ** (-np.arange(0, 32, 2, dtype=np.float32) / np.float32(32))).astype(np.float32)
    c["invf"] = np.tile(np.concatenate([invA, invB])[None, :], (128, 1)).astype(np.float32)
    return c


def _prep_core(inp, c, NT, L):
    b, j = c // 4, c % 4
    SEQ = NT * 128
    NCMP = (SEQ - 32) // 16 + 1
    NCK = (NCMP + 127) // 128
    NCP = NCK * 128
    f = np.float32
    m = dict(_consts(NT, L, j))
    x = np.asarray(inp["x"])[b].reshape(NT, 128, 1024)
    own = [4 * l + j for l in range(L)]
    m["x"] = np.ascontiguousarray(x)
    m["xq"] = np.ascontiguousarray(x[own])
    pos = np.asarray(inp["positions"])[b].astype(np.int32)
    pt = pos.reshape(NT, 128).T
    m["pos"] = np.ascontiguousarray(pt)
    m["posq"] = np.ascontiguousarray(pt[:, own])
    pc = np.zeros(NCP, np.int32)
    pc[:NCMP] = pos[16 * np.arange(NCMP) + 31]
    m["posc"] = np.ascontiguousarray(pc.reshape(NCK, 128).T)
    w_in = np.asarray(inp["w_in"])[0]
    m["wkv"] = _kc(np.concatenate([w_in[:, 512:1280], w_in[:, 1560:1720]], 1))
    m["wq"] = _kc(np.concatenate([w_in[:, 0:512], w_in[:, 1280:1304], w_in[:, 1304:1560]], 1))
    m["g1"] = np.ascontiguousarray(np.asarray(inp["norm1_gain"])[0].reshape(8, 128).T)
    vec = np.zeros(576, f)
    off = 0
    for nm in ("nsa_q_gain", "nsa_kc_gain", "nsa_ks_gain", "nsa_kw_gain", "mla_kv_lora_gain", "mla_q_gain", "mla_k_gain"):
        v = np.asarray(inp[nm])[0]
        vec[off:off + v.size] = v
        off += v.size
    m["vecs"] = np.tile(vec[None, :], (128, 1))
    cp = np.asarray(inp["cmp_pos"])[0]
    m["cposT"] = np.ascontiguousarray(np.concatenate([cp.T, cp.T], 0))
    for nm, src in (("w1k", "cmp_k_w1"), ("w1v", "cmp_v_w1")):
        w = np.asarray(inp[src])[0].reshape(32, 64, 256).transpose(1, 0, 2)
        m[nm] = np.ascontiguousarray(np.concatenate([w, w], 0))
    for nm, src in (("w2k", "cmp_k_w2"), ("w2v", "cmp_v_w2")):
        m[nm] = _kc(np.asarray(inp[src])[0])
    m["wuq"] = _kc(np.asarray(inp["mla_w_uq"])[0])
    m["gql"] = np.ascontiguousarray(np.asarray(inp["mla_q_lora_gain"])[0].reshape(2, 128).T)
    wukv = np.asarray(inp["mla_w_ukv"])[0].reshape(128, 8, 128)
    knope, vpart = wukv[:, :, 0:64], wukv[:, :, 64:128]
    m["wukall"] = np.ascontiguousarray(knope.reshape(128, 512))
    wt = np.zeros((128, 8, 128), f)
    for h in range(8):
        wt[(h % 2) * 64:(h % 2) * 64 + 64, h, :] = knope[:, h, :].T
    m["wukT"] = wt
    m["wuv"] = np.ascontiguousarray(vpart.reshape(128, 512))
    m["wout"] = _kc(np.asarray(inp["w_out"])[0])
    m["gout"] = np.ascontiguousarray(np.concatenate([np.asarray(inp["out_gain_a"])[0], np.asarray(inp["out_gain_b"])[0]]).reshape(8, 128).T)
    m["wpq"] = _kc(np.asarray(inp["peer_w_q"])[0])
    sk = np.asarray(inp["peer_sub_keys"])[0].reshape(16, 128, 128)
    m["skT"] = np.ascontiguousarray(sk.transpose(2, 0, 1))
    m["n2g"] = np.tile(np.asarray(inp["norm2_gain"])[0][None, :], (128, 1))
    m["peer_u"] = np.asarray(inp["peer_u"])[0]
    m["peer_v"] = np.asarray(inp["peer_v"])[0]
    return {k: np.ascontiguousarray(v) for k, v in m.items()}


_CACHE = {}


def run(inputs, stage=3, dbg=False, cut=99, peer_rows=16384):
    x = np.asarray(inputs["x"])
    B, SEQ, _ = x.shape
    NT = SEQ // 128
    L = NT // 4
    key = (NT, L, stage, dbg, cut, peer_rows)
    if key not in _CACHE:
        _CACHE[key] = build(NT, L, stage=stage, dbg=dbg, cut=cut, peer_rows=peer_rows)
    nc, dbg_names = _CACHE[key]
    in_maps = [_prep_core(inputs, c, NT, L) for c in range(8)]
    if peer_rows != 16384:
        for m in in_maps:
            m["peer_u"] = m["peer_u"][:peer_rows]
            m["peer_v"] = m["peer_v"][:peer_rows]
    import os
    ncr = int(os.environ.get("NCORES", "8"))
    res = run_bass_kernel_spmd(nc, in_maps[:ncr], core_ids=list(range(ncr)))
    return res, NT, L


def kernel(**inputs):
    res, NT, L = run(inputs, stage=3, dbg=False)
    x = np.asarray(inputs["x"])
    out = np.zeros(x.shape, np.float32)
    for c in range(8):
        b, j = c // 4, c % 4
        o = res.results[c]["out"]
        for l in range(L):
            tt = 4 * l + j
            out[b, tt * 128:(tt + 1) * 128, :] = o[l]
    return out
```
